# Optimizing a Trainium2 kernel written in Bass

```python
import math
import jax, jax.numpy as jnp
from jax import lax
import numpy as np

D_MODEL = 1024
BATCH = 8
SEQ = 4096
DEPTH = 4

CHUNK = 64
Q_BLOCK = 128
CONV_WIDTH = 3
A_GROUPS = 8
A_GROUP_DIM = D_MODEL // 16
A_DIM = A_GROUPS * A_GROUP_DIM
SGU_BLOCK = 128
B_GROUPS = 8
B_GROUP_DIM = D_MODEL // 16
B_DIM = B_GROUPS * B_GROUP_DIM
EVEN_IN = 3 * A_DIM + 2 * B_DIM
EVEN_MIX = A_DIM + B_DIM
MLA_HEADS = 8
MLA_NOPE_DIM = 64
MLA_ROPE_DIM = 32
MLA_V_DIM = 64
MLA_Q_RANK = 256
MLA_KV_RANK = 128
ROPE_THETA = 10000.0
FOX_HEADS = 8
FOX_HEAD_DIM = 64
FOX_DIM = FOX_HEADS * FOX_HEAD_DIM
FOX_BIAS_INIT = 3.0
ODD_IN = MLA_Q_RANK + MLA_KV_RANK + MLA_ROPE_DIM + 3 * FOX_DIM + FOX_HEADS
ODD_MIX = MLA_HEADS * MLA_V_DIM + FOX_DIM
N_EXPERTS = 32
TOP_K = 4
D_FF = D_MODEL
SWIGLU_ALPHA = 1.702
SWIGLU_LIMIT = 7.0
DN_ALPHA = (2 * DEPTH) ** 0.25
DN_BETA = (8 * DEPTH) ** -0.25
N_EVEN = (DEPTH + 1) // 2
N_ODD = DEPTH // 2
NORM_EPS = 1e-5
NEG_INF = -1e30

kernel_name = "hybrid_conv_sgu_mla_fox_moe_deepnorm"


def _layernorm(x, g, b):
    xf = x.astype(jnp.float32)
    mu = jnp.mean(xf, axis=-1, keepdims=True)
    var = jnp.mean(jnp.square(xf - mu), axis=-1, keepdims=True)
    return ((xf - mu) * lax.rsqrt(var + NORM_EPS) * g + b).astype(x.dtype)


def _rmsnorm(x, g):
    xf = x.astype(jnp.float32)
    ms = jnp.mean(jnp.square(xf), axis=-1, keepdims=True)
    return (xf * lax.rsqrt(ms + NORM_EPS) * g).astype(x.dtype)


def _rope(x):
    s, d = x.shape[1], x.shape[-1]
    inv_freq = ROPE_THETA ** (-jnp.arange(0, d, 2, dtype=jnp.float32) / d)
    ang = jnp.arange(s, dtype=jnp.float32)[:, None] * inv_freq[None, :]
    cos = jnp.cos(ang)[None, :, None, :]
    sin = jnp.sin(ang)[None, :, None, :]
    xf = x.astype(jnp.float32)
    x1, x2 = xf[..., : d // 2], xf[..., d // 2:]
    return jnp.concatenate([x1 * cos - x2 * sin, x2 * cos + x1 * sin], axis=-1).astype(x.dtype)


def _heads(t, h):
    b, s, _ = t.shape
    return t.reshape(b, s, h, -1).transpose(0, 2, 1, 3)


def _merge(t):
    b, h, s, d = t.shape
    return t.transpose(0, 2, 1, 3).reshape(b, s, h * d)


def _block_attention(q, k, v, scale, per_frame, log_f_cum=None):
    b, h, s, dq = q.shape
    dv = v.shape[-1]
    nb = s // Q_BLOCK
    k_pos = jnp.arange(s)
    q_blocks = jnp.moveaxis(q.reshape(b, h, nb, Q_BLOCK, dq), 2, 0)
    idx = jnp.arange(nb)

    def attend(qb, i, fq):
        q_pos = i * Q_BLOCK + jnp.arange(Q_BLOCK)
        logits = jnp.einsum('bhqd,bhkd->bhqk', qb, k).astype(jnp.float32) * scale
        if per_frame:
            allowed = k_pos[None, :] <= q_pos[:, None]
        else:
            allowed = (k_pos // CHUNK)[None, :] <= (q_pos // CHUNK)[:, None]
        if fq is not None:
            logits = logits + (fq[..., :, None] - log_f_cum[..., None, :])
        logits = jnp.where(allowed, logits, NEG_INF)
        p = jax.nn.softmax(logits, axis=-1).astype(v.dtype)
        return jnp.einsum('bhqk,bhkd->bhqd', p, v)

    if log_f_cum is None:
        out = lax.map(lambda xs: attend(xs[0], xs[1], None), (q_blocks, idx))
    else:
        f_blocks = jnp.moveaxis(log_f_cum.reshape(b, h, nb, Q_BLOCK), 2, 0)
        out = lax.map(lambda xs: attend(xs[0], xs[1], xs[2]), (q_blocks, idx, f_blocks))
    return jnp.moveaxis(out, 0, 2).reshape(b, h, s, dv)


def _even_mixer(x, w_in, conv_w, sgu_ln_g, sgu_ln_b, sgu_w, sgu_b, w_out):
    b, s, _ = x.shape
    proj = x @ w_in
    a_c, a_b, a_v, b_uv = jnp.split(proj, [A_DIM, 2 * A_DIM, 3 * A_DIM], axis=-1)
    h = a_c * a_v
    hp = jnp.pad(h, ((0, 0), (CONV_WIDTH - 1, 0), (0, 0)))
    conv = hp[:, 0:s] * conv_w[0] + hp[:, 1:s + 1] * conv_w[1] + hp[:, 2:s + 2] * conv_w[2]
    y_a = a_b * conv
    z = jax.nn.gelu(b_uv)
    u, v = jnp.split(z, 2, axis=-1)
    v = v.reshape(b, s // SGU_BLOCK, SGU_BLOCK, B_GROUPS, B_GROUP_DIM)
    v = _layernorm(v, sgu_ln_g.reshape(B_GROUPS, B_GROUP_DIM), sgu_ln_b.reshape(B_GROUPS, B_GROUP_DIM))
    pos = jnp.arange(SGU_BLOCK)
    mask = (pos[None, :] // CHUNK) <= (pos[:, None] // CHUNK)
    w_s = jnp.where(mask, sgu_w, 0.0)
    v = jnp.einsum('gij,bnjgc->bnigc', w_s, v) + sgu_b.T[:, :, None]
    y_b = u * v.reshape(b, s, B_DIM)
    return jnp.concatenate([y_a, y_b], axis=-1) @ w_out


def _odd_mixer(x, w_in, q_norm_g, kv_norm_g, w_uq, w_ukv, f_bias, w_out):
    b, s, _ = x.shape
    proj = x @ w_in
    splits = np.cumsum([MLA_Q_RANK, MLA_KV_RANK, MLA_ROPE_DIM, FOX_DIM, FOX_DIM, FOX_DIM]).tolist()
    c_q, c_kv, k_r, f_q, f_k, f_v, f_z = jnp.split(proj, splits, axis=-1)
    q = (_rmsnorm(c_q, q_norm_g) @ w_uq).reshape(b, s, MLA_HEADS, MLA_NOPE_DIM + MLA_ROPE_DIM)
    q = jnp.concatenate([q[..., :MLA_NOPE_DIM], _rope(q[..., MLA_NOPE_DIM:])], axis=-1)
    kv = (_rmsnorm(c_kv, kv_norm_g) @ w_ukv).reshape(b, s, MLA_HEADS, MLA_NOPE_DIM + MLA_V_DIM)
    k_nope, v_c = kv[..., :MLA_NOPE_DIM], kv[..., MLA_NOPE_DIM:]
    k_rope = jnp.broadcast_to(_rope(k_r[:, :, None, :]), (b, s, MLA_HEADS, MLA_ROPE_DIM))
    k = jnp.concatenate([k_nope, k_rope], axis=-1)
    y_c = _block_attention(q.transpose(0, 2, 1, 3), k.transpose(0, 2, 1, 3), v_c.transpose(0, 2, 1, 3),
                           1.0 / math.sqrt(MLA_NOPE_DIM + MLA_ROPE_DIM), per_frame=False)
    log_f = jax.nn.log_sigmoid((f_z + f_bias).astype(jnp.float32))
    log_f_cum = jnp.cumsum(log_f, axis=1).transpose(0, 2, 1)
    y_d = _block_attention(_heads(f_q, FOX_HEADS), _heads(f_k, FOX_HEADS), _heads(f_v, FOX_HEADS),
                           1.0 / math.sqrt(FOX_HEAD_DIM), per_frame=True, log_f_cum=log_f_cum)
    return jnp.concatenate([_merge(y_c), _merge(y_d)], axis=-1) @ w_out


def _clamped_swiglu(h):
    glu, lin = h[..., ::2], h[..., 1::2]
    glu = jnp.minimum(glu, SWIGLU_LIMIT)
    lin = jnp.clip(lin, -SWIGLU_LIMIT, SWIGLU_LIMIT)
    return glu * jax.nn.sigmoid(SWIGLU_ALPHA * glu) * (lin + 1.0)


def _moe(x, w_router, b_router, w1, b1, w2, b2):
    b, s, d = x.shape
    xt = x.reshape(b * s, d)
    logits = (xt @ w_router + b_router).astype(jnp.float32)
    top_val, top_idx = lax.top_k(logits, TOP_K)
    top_w = jax.nn.softmax(top_val, axis=-1)
    gates = jnp.einsum('nk,nke->ne', top_w, jax.nn.one_hot(top_idx, N_EXPERTS, dtype=jnp.float32)).astype(x.dtype)
    out = jnp.zeros_like(xt)
    for e in range(N_EXPERTS):
        h = _clamped_swiglu(xt @ w1[e] + b1[e])
        out = out + gates[:, e:e + 1] * (h @ w2[e] + b2[e])
    return out.reshape(b, s, d)


def setup_inputs(seed: int = 0) -> dict:
    key = jax.random.key(seed)
    ks = jax.random.split(key, 32)
    nrm = jax.random.normal
    f32 = jnp.float32
    return {
        "x": nrm(ks[0], (BATCH, SEQ, D_MODEL), f32),
        "ev_w_in": nrm(ks[1], (N_EVEN, D_MODEL, EVEN_IN), f32) * D_MODEL ** -0.5,
        "ev_conv_w": nrm(ks[2], (N_EVEN, CONV_WIDTH, A_DIM), f32) * CONV_WIDTH ** -0.5,
        "ev_sgu_ln_g": 1.0 + 0.1 * nrm(ks[3], (N_EVEN, B_DIM), f32),
        "ev_sgu_ln_b": 0.1 * nrm(ks[4], (N_EVEN, B_DIM), f32),
        "ev_sgu_w": nrm(ks[5], (N_EVEN, B_GROUPS, SGU_BLOCK, SGU_BLOCK), f32) * SGU_BLOCK ** -0.5,
        "ev_sgu_b": 1.0 + 0.1 * nrm(ks[6], (N_EVEN, B_GROUPS, SGU_BLOCK), f32),
        "ev_w_out": nrm(ks[7], (N_EVEN, EVEN_MIX, D_MODEL), f32) * EVEN_MIX ** -0.5 * DN_BETA,
        "od_w_in": nrm(ks[8], (N_ODD, D_MODEL, ODD_IN), f32) * D_MODEL ** -0.5,
        "od_q_norm_g": 1.0 + 0.1 * nrm(ks[9], (N_ODD, MLA_Q_RANK), f32),
        "od_kv_norm_g": 1.0 + 0.1 * nrm(ks[10], (N_ODD, MLA_KV_RANK), f32),
        "od_w_uq": nrm(ks[11], (N_ODD, MLA_Q_RANK, MLA_HEADS * (MLA_NOPE_DIM + MLA_ROPE_DIM)), f32) * MLA_Q_RANK ** -0.5,
        "od_w_ukv": nrm(ks[12], (N_ODD, MLA_KV_RANK, MLA_HEADS * (MLA_NOPE_DIM + MLA_V_DIM)), f32) * MLA_KV_RANK ** -0.5,
        "od_f_bias": FOX_BIAS_INIT + 0.5 * nrm(ks[13], (N_ODD, FOX_HEADS), f32),
        "od_w_out": nrm(ks[14], (N_ODD, ODD_MIX, D_MODEL), f32) * ODD_MIX ** -0.5 * DN_BETA,
        "ln_mix_g": 1.0 + 0.1 * nrm(ks[15], (DEPTH, D_MODEL), f32),
        "ln_mix_b": 0.1 * nrm(ks[16], (DEPTH, D_MODEL), f32),
        "ln_ffn_g": 1.0 + 0.1 * nrm(ks[17], (DEPTH, D_MODEL), f32),
        "ln_ffn_b": 0.1 * nrm(ks[18], (DEPTH, D_MODEL), f32),
        "moe_w_router": nrm(ks[19], (DEPTH, D_MODEL, N_EXPERTS), f32) * D_MODEL ** -0.5,
        "moe_b_router": 0.01 * nrm(ks[20], (DEPTH, N_EXPERTS), f32),
        "moe_w1": nrm(ks[21], (DEPTH, N_EXPERTS, D_MODEL, 2 * D_FF), f32) * D_MODEL ** -0.5,
        "moe_b1": 0.01 * nrm(ks[22], (DEPTH, N_EXPERTS, 2 * D_FF), f32),
        "moe_w2": nrm(ks[23], (DEPTH, N_EXPERTS, D_FF, D_MODEL), f32) * D_FF ** -0.5 * DN_BETA,
        "moe_b2": 0.01 * nrm(ks[24], (DEPTH, N_EXPERTS, D_MODEL), f32),
    }


def reference(x, ev_w_in, ev_conv_w, ev_sgu_ln_g, ev_sgu_ln_b, ev_sgu_w, ev_sgu_b, ev_w_out,
              od_w_in, od_q_norm_g, od_kv_norm_g, od_w_uq, od_w_ukv, od_f_bias, od_w_out,
              ln_mix_g, ln_mix_b, ln_ffn_g, ln_ffn_b,
              moe_w_router, moe_b_router, moe_w1, moe_b1, moe_w2, moe_b2):
    for layer in range(DEPTH):
        i = layer // 2
        if layer % 2 == 0:
            mix = _even_mixer(x, ev_w_in[i], ev_conv_w[i], ev_sgu_ln_g[i], ev_sgu_ln_b[i],
                              ev_sgu_w[i], ev_sgu_b[i], ev_w_out[i])
        else:
            mix = _odd_mixer(x, od_w_in[i], od_q_norm_g[i], od_kv_norm_g[i], od_w_uq[i],
                             od_w_ukv[i], od_f_bias[i], od_w_out[i])
        x = _layernorm(DN_ALPHA * x + mix, ln_mix_g[layer], ln_mix_b[layer])
        ffn = _moe(x, moe_w_router[layer], moe_b_router[layer], moe_w1[layer], moe_b1[layer],
                   moe_w2[layer], moe_b2[layer])
        x = _layernorm(DN_ALPHA * x + ffn, ln_ffn_g[layer], ln_ffn_b[layer])
    return x
```

```python
import numpy as np
import concourse.bass as bass
import concourse.mybir as mybir
from concourse.bass_utils import run_bass_kernel_spmd
from contextlib import ExitStack

F32 = mybir.dt.float32
BF16 = mybir.dt.bfloat16
ALU = mybir.AluOpType
AF = mybir.ActivationFunctionType
AX = mybir.AxisListType


class _Op:
    __slots__ = ("eng", "fn", "deps", "signal", "sigval", "dma_key", "dma_val")

    def __init__(self, eng, fn):
        self.eng = eng
        self.fn = fn
        self.deps = []
        self.signal = False
        self.sigval = 0
        self.dma_key = None
        self.dma_val = 0


class Sched:
    ENGS = ("pe", "dve", "act", "pool", "sp")

    def __init__(self, nc, es):
        self.nc = nc
        self.es = es
        self.h = {"pe": nc.tensor, "dve": nc.vector, "act": nc.scalar,
                  "pool": nc.gpsimd, "sp": nc.sync}
        self.sem = {e: es.enter_context(nc.semaphore("s_" + e)) for e in self.ENGS}
        self.dsem = {}
        self.reset()

    def sbuf(self, name, shape, dt, es=None):
        return (es or self.es).enter_context(self.nc.sbuf_tensor(name, list(shape), dt))

    def psum(self, name, shape, dt=F32, es=None):
        return (es or self.es).enter_context(self.nc.psum_tensor(name, list(shape), dt))

    def reset(self):
        self.ops = {e: [] for e in self.ENGS}
        self.buf = {}
        self.dcount = {}

    def _track(self, op, reads, writes):
        deps = []
        for b in reads:
            st = self.buf.get(b)
            if st is None:
                st = self.buf[b] = [None, []]
            if st[0] is not None:
                deps.append(st[0])
        for b in writes:
            st = self.buf.get(b)
            if st is None:
                st = self.buf[b] = [None, []]
            if st[0] is not None:
                deps.append(st[0])
            deps.extend(st[1])
        for b in reads:
            self.buf[b][1].append(op)
        for b in writes:
            self.buf[b][0] = op
            self.buf[b][1] = []
        seen = set()
        for d in deps:
            if d is op or id(d) in seen:
                continue
            seen.add(id(d))
            if d.dma_key is None and d.eng == "pe" and op.eng == "pe" and op.dma_key is None:
                continue
            op.deps.append(d)
            if d.dma_key is None:
                d.signal = True

    def op(self, eng, fn, reads=(), writes=()):
        o = _Op(eng, fn)
        self._track(o, reads, writes)
        self.ops[eng].append(o)
        return o

    def dma(self, eng, out, in_, key, reads=(), writes=(), **kw):
        h = self.h[eng]
        o = _Op(eng, lambda: h.dma_start(out=out, in_=in_, **kw))
        o.dma_key = key
        if key not in self.dsem:
            self.dsem[key] = self.es.enter_context(self.nc.semaphore("d_" + str(key)))
        self.dcount[key] = self.dcount.get(key, 0) + 1
        o.dma_val = 16 * self.dcount[key]
        self._track(o, reads, writes)
        self.ops[eng].append(o)
        return o

    def flush(self, final=False):
        nc = self.nc
        for e in self.ENGS:
            c = 0
            for o in self.ops[e]:
                if o.dma_key is None and o.signal:
                    c += 1
                    o.sigval = c
        used_keys = {e: {} for e in self.ENGS}

        def emit(e, h):
            waited = {}
            for o in self.ops[e]:
                need = {}
                for d in o.deps:
                    if d.dma_key is not None:
                        k = ("d", d.dma_key)
                        v = d.dma_val
                    else:
                        k = ("e", d.eng)
                        v = d.sigval
                    if v > need.get(k, 0):
                        need[k] = v
                for k, v in need.items():
                    if waited.get(k, 0) >= v:
                        continue
                    waited[k] = v
                    s = self.dsem[k[1]] if k[0] == "d" else self.sem[k[1]]
                    h.wait_ge(s, v)
                ins = o.fn()
                if o.dma_key is not None:
                    ins.then_inc(self.dsem[o.dma_key], 16)
                    used_keys[e][o.dma_key] = o.dma_val
                elif o.signal:
                    ins.then_inc(self.sem[o.eng], 1)
            for k, v in used_keys[e].items():
                if waited.get(("d", k), 0) < v:
                    h.wait_ge(self.dsem[k], v)

        with nc.Block() as block:
            @block.tensor
            def _(h):
                emit("pe", h)

            @block.vector
            def _(h):
                emit("dve", h)

            @block.scalar
            def _(h):
                emit("act", h)

            @block.gpsimd
            def _(h):
                emit("pool", h)

            @block.sync
            def _(h):
                emit("sp", h)
        if not final:
            nc.all_engine_barrier()
            for e in self.ENGS:
                self.h[e].sem_clear(self.sem[e])
            ks = list(self.dcount.keys())
            for i, k in enumerate(ks):
                self.h["sp"].sem_clear(self.dsem[k])
            nc.all_engine_barrier()
        self.reset()


import os
S_TOK = int(os.environ.get('K_STOK', '4096'))
NT = S_TOK // 128
D = 1024
KC = 8
DEPTH = 4
NE = 32
DN_ALPHA = float((2 * DEPTH) ** 0.25)
EPS = 1e-5
GELU_C = 1.5957691216057308


class Prog:
    def __init__(self, stop_after=None, single=None):
        self.stop_after = stop_after
        self.single = single
        nc = self.nc = bass.Bass("TRN2", target_bir_lowering=False)
        self.es = ExitStack()
        self.S = Sched(nc, self.es)
        self.d = {}

    def din(self, name, shape, dt=F32):
        t = self.nc.dram_tensor(name, list(shape), dt, kind="ExternalInput")
        self.d[name] = t.ap()
        return self.d[name]

    def dscratch(self, name, shape, dt):
        t = self.nc.dram_tensor(name, list(shape), dt, kind="Internal")
        self.d[name] = t.ap()
        return self.d[name]

    def PI(self, layer):
        return 0 if self.single is not None else layer // 2

    def LI(self, layer):
        return 0 if self.single is not None else layer

    def declare(self):
        d0 = self.din
        one = self.single is not None

        def d(name, shape, dt=F32):
            if one and not name.startswith("c_") and name != "x":
                shape = [1] + list(shape[1:])
            return d0(name, shape, dt)
        d("x", [S_TOK, D])
        if self.single in (None, 0):
            d("ev_w_in", [2, D, 2560]); d("ev_conv_w", [2, 3, 512]); d("ev_sgu_ln_g", [2, 512]); d("ev_sgu_ln_b", [2, 512])
            d("ev_sgu_w", [2, 8, 128, 128]); d("ev_sgu_b", [2, 8, 128]); d("ev_w_out", [2, D, D])
        if self.single in (None, 1):
            d("od_w_in", [2, D, 1960]); d("od_q_norm_g", [2, 256]); d("od_kv_norm_g", [2, 128])
            d("od_w_uq", [2, 256, 768]); d("od_w_ukv", [2, 128, 1024]); d("od_f_bias", [2, 8]); d("od_w_out", [2, D, D])
        d("ln_mix_g", [4, D]); d("ln_mix_b", [4, D]); d("ln_ffn_g", [4, D]); d("ln_ffn_b", [4, D])
        d("moe_w_router", [4, D, NE]); d("moe_b_router", [4, NE])
        d("moe_w1p", [4, NE, 8, D, 256]); d("moe_b1p", [4, NE * 16, 128])
        d("moe_w2", [4, NE, D, D]); d("moe_b2", [4, NE, D])
        d("c_ident", [128, 128])
        if self.single in (None, 1):
            d("c_tri", [128, 128]); d("c_maskc", [128, 128]); d("c_maskf", [128, 128])
            d("c_cos", [S_TOK, 16]); d("c_sin", [S_TOK, 16])
        self.out = self.nc.dram_tensor("out", [S_TOK, D], F32, kind="ExternalOutput").ap()
        self.dscratch("xres", [S_TOK, D], F32)
        self.dscratch("xTd", [128, KC, S_TOK], BF16)
        self.dscratch("ffn", [S_TOK, D], F32)

    def mm_group(self, out, pairs):
        nc = self.nc
        n = len(pairs)

        def f():
            ins = None
            for i, (l, r) in enumerate(pairs):
                ins = nc.tensor.matmul(out, l, r, start=(i == 0), stop=(i == n - 1))
            return ins
        return f

    def V(self, eng):
        return {"dve": self.nc.vector, "pool": self.nc.gpsimd}[eng]

    def tt(self, eng, out, a, b, op, reads, writes):
        h = self.V(eng)
        self.S.op(eng, lambda: h.tensor_tensor(out, a, b, op), reads, writes)

    def ts(self, eng, out, a, s1, s2, op0, op1, reads, writes):
        h = self.V(eng)
        if op1 is None:
            self.S.op(eng, lambda: h.tensor_scalar(out, a, s1, None, op0), reads, writes)
        else:
            self.S.op(eng, lambda: h.tensor_scalar(out, a, s1, s2, op0, op1), reads, writes)

    def stt(self, eng, out, a, sc, b, op0, op1, reads, writes):
        h = self.V(eng)
        self.S.op(eng, lambda: h.scalar_tensor_tensor(out, a, sc, b, op0, op1), reads, writes)

    def act(self, out, in_, func, reads, writes, bias=None, scale=None):
        nc = self.nc
        kw = {}
        if bias is not None:
            kw["bias"] = bias
        if scale is not None:
            kw["scale"] = scale
        self.S.op("act", lambda: nc.scalar.activation(out, in_, func, **kw), reads, writes)

    def rsum(self, eng, out, in_, reads, writes):
        h = self.V(eng)
        self.S.op(eng, lambda: h.reduce_sum(out, in_, AX.X), reads, writes)

    def begin_phase(self):
        self.pes = ExitStack()
        S = self.S
        self.phase_id = getattr(self, "phase_id", 0) + 1
        self.pb = [S.psum(f"pb{i}_p{self.phase_id}", [128, 512], F32, es=self.pes) for i in range(8)]
        self._uid = 0

    def end_phase(self, final=False):
        self.S.flush(final=final)
        self.pes.close()

    def sb(self, name, shape, dt):
        return self.S.sbuf(f"{name}_p{self.phase_id}", shape, dt, es=self.pes)

    def bfv(self, i):
        return self.pb[i][:].bitcast(BF16)

    def load_consts(self):
        S, nc, d = self.S, self.nc, self.d
        self.ident_b = S.sbuf("ident_b", [128, 128], BF16)
        self.ident_f = S.sbuf("ident_f", [128, 128], F32)
        self.gates = S.sbuf("gates", [128, NT, NE], F32)
        self.gT = S.sbuf("gT", [NE, S_TOK], BF16)
        S.dma("pool", self.ident_b[:], d["c_ident"], key="cid_b", writes=["ident_b"])
        S.dma("sp", self.ident_f[:], d["c_ident"], key="cid_f", writes=["ident_f"])

    def emit_xT(self, xb, xbname, stage, stname, j, bank):
        S, nc = self.S, self.nc
        pv = self.bfv(bank)
        pn = f"pb{bank}"

        def f():
            ins = None
            for k in range(KC):
                ins = nc.tensor.transpose(pv[:, k * 128:(k + 1) * 128], xb[:, k * 128:(k + 1) * 128], self.ident_b[:])
            return ins
        S.op("pe", f, reads=[xbname, "ident_b"], writes=[pn])
        pv3 = pv.rearrange("p (k t) -> p k t", k=KC)
        S.op("dve", lambda: nc.vector.tensor_copy(stage[:, :, j * 128:(j + 1) * 128], pv3), reads=[pn], writes=[stname + str(j)])

    def alloc_finish(self):
        sb = self.sb
        self.f_sq = sb("f_sq", [128, D], F32)
        self.f_xn = sb("f_xn", [128, D], F32)
        self.f_xb = sb("f_xb", [128, D], BF16)
        self.f_st = sb("f_st", [128, 16], F32)
        self.f_g = sb("f_g", [128, D], F32)
        self.f_b = sb("f_b", [128, D], F32)
        self.f_stage = sb("f_stage", [128, KC, 512], BF16)
        self.r_x32 = sb("r_x32", [128, KC, 128], F32)
        self.r_w = sb("r_w", [128, KC, NE], F32)
        self.r_bb = sb("r_bb", [128, NE], F32)
        self.r_lg = sb("r_lg", [128, NE], F32)
        self.r_t8 = sb("r_t8", [128, 8], F32)
        self.r_ex = sb("r_ex", [128, NE], F32)
        self.r_sm = sb("r_sm", [128, 4], F32)
        self.r_gb = sb("r_gb", [128, NE], BF16)

    def load_ln_params(self, gname, bname, layer, router_layer=None):
        S, d = self.S, self.d
        layer = self.LI(layer)
        if router_layer is not None:
            router_layer = layer
        S.dma("sp", self.f_g[:], d[gname][layer:layer + 1, :].to_broadcast([128, D]), key="f_g", writes=["f_g"])
        S.dma("sp", self.f_b[:], d[bname][layer:layer + 1, :].to_broadcast([128, D]), key="f_b", writes=["f_b"])
        if router_layer is not None:
            S.dma("sp", self.r_w[:], d["moe_w_router"][router_layer].rearrange("(k p) e -> p k e", p=128), key="r_w", writes=["r_w"])
            S.dma("sp", self.r_bb[:], d["moe_b_router"][router_layer:router_layer + 1, :].to_broadcast([128, NE]), key="r_bb", writes=["r_bb"])

    def finish_tile(self, z, zname, t, dst, router, banks):
        S, nc = self.S, self.nc
        st = self.f_st
        j = t % 4
        self.rsum("dve", st[:, 0:1], z[:], [zname], ["f_st0"])
        self.ts("dve", st[:, 1:2], st[:, 0:1], -1.0 / D, None, ALU.mult, None, ["f_st0"], ["f_st1"])
        self.act(self.f_sq[:], z[:], AF.Square, [zname, "f_st1"], ["f_sq"], bias=st[:, 1:2], scale=1.0)
        self.rsum("dve", st[:, 2:3], self.f_sq[:], ["f_sq"], ["f_st2"])
        self.ts("dve", st[:, 3:4], st[:, 2:3], 1.0 / D, EPS, ALU.mult, ALU.add, ["f_st2"], ["f_st3"])
        self.act(st[:, 4:5], st[:, 3:4], AF.Sqrt, ["f_st3"], ["f_st4"])
        S.op("dve", lambda: nc.vector.reciprocal(st[:, 4:5], st[:, 4:5]), ["f_st4"], ["f_st4"])
        self.ts("dve", self.f_xn[:], z[:], st[:, 1:2], st[:, 4:5], ALU.add, ALU.mult, [zname, "f_st1", "f_st4"], ["f_xn"])
        self.tt("pool", self.f_xn[:], self.f_xn[:], self.f_g[:], ALU.mult, ["f_xn", "f_g"], ["f_xn"])
        self.tt("pool", z[:], self.f_xn[:], self.f_b[:], ALU.add, ["f_xn", "f_b"], [zname])
        S.dma("sp", dst[t * 128:(t + 1) * 128, :], z[:], key="f_out_" + zname, reads=[zname], writes=[("res", t)])
        self.S.op("act", lambda: nc.scalar.copy(self.f_xb[:], z[:]), [zname], ["f_xb"])
        self.emit_xT(self.f_xb, "f_xb", self.f_stage, "f_stage", j, banks[0])
        if j == 3:
            blk = t // 4
            S.dma("sp", self.d["xTd"][:, :, blk * 512:(blk + 1) * 512], self.f_stage[:], key="f_xTd",
                  reads=["f_stage0", "f_stage1", "f_stage2", "f_stage3"], writes=[("xTd", blk)])
        if not router:
            return
        b1, b2, b3 = banks[1], banks[2], banks[3]

        def ftr():
            ins = None
            for k in range(KC):
                pbk = self.pb[b1] if k < 4 else self.pb[b2]
                ins = nc.tensor.transpose(pbk[:, (k % 4) * 128:(k % 4 + 1) * 128], z[:, k * 128:(k + 1) * 128], self.ident_f[:])
            return ins
        S.op("pe", ftr, reads=[zname, "ident_f"], writes=[f"pb{b1}", f"pb{b2}"])
        x32 = self.r_x32
        S.op("act", lambda: nc.scalar.copy(x32[:, 0:4, :], self.pb[b1][:].rearrange("p (k t) -> p k t", k=4)), [f"pb{b1}"], ["r_x32a"])
        S.op("act", lambda: nc.scalar.copy(x32[:, 4:8, :], self.pb[b2][:].rearrange("p (k t) -> p k t", k=4)), [f"pb{b2}"], ["r_x32b"])
        lgp = self.pb[b3][:, 0:NE]
        S.op("pe", self.mm_group(lgp, [(x32[:, k, :], self.r_w[:, k, :]) for k in range(KC)]),
             reads=["r_x32a", "r_x32b", "r_w"], writes=[f"pb{b3}"])
        lg, t8, ex, sm = self.r_lg, self.r_t8, self.r_ex, self.r_sm
        self.tt("dve", lg[:], lgp, self.r_bb[:], ALU.add, [f"pb{b3}", "r_bb"], ["r_lg"])
        S.op("dve", lambda: nc.vector.max(out=t8[:], in_=lg[:]), ["r_lg"], ["r_t8"])
        self.ts("dve", sm[:, 0:1], t8[:, 0:1], -1.0, None, ALU.mult, None, ["r_t8"], ["r_sm0"])
        self.act(ex[:], lg[:], AF.Exp, ["r_lg", "r_sm0"], ["r_ex"], bias=sm[:, 0:1], scale=1.0)
        self.stt("dve", ex[:], lg[:], t8[:, 3:4], ex[:], ALU.is_ge, ALU.mult, ["r_lg", "r_t8", "r_ex"], ["r_ex"])
        self.rsum("dve", sm[:, 1:2], ex[:], ["r_ex"], ["r_sm1"])
        S.op("dve", lambda: nc.vector.reciprocal(sm[:, 2:3], sm[:, 1:2]), ["r_sm1"], ["r_sm2"])
        self.ts("dve", self.gates[:, t, :], ex[:], sm[:, 2:3], None, ALU.mult, None, ["r_ex", "r_sm2"], [("gates", t)])
        S.op("act", lambda: nc.scalar.copy(self.r_gb[:], self.gates[:, t, :]), [("gates", t)], ["r_gb"])
        gtp = self.bfv(b3)[0:NE, 512:640]
        S.op("pe", lambda: nc.tensor.transpose(gtp, self.r_gb[:], self.ident_b[:]), ["r_gb", "ident_b"], [f"pb{b3}"])
        S.op("act", lambda: nc.scalar.copy(self.gT[:, t * 128:(t + 1) * 128], gtp), [f"pb{b3}"], [("gT", t)])

    def phase_init(self):
        S, nc, d = self.S, self.nc, self.d
        self.begin_phase()
        self.load_consts()
        xin = [self.sb(f"i_x{i}", [128, D], F32) for i in range(2)]
        xb = [self.sb(f"i_xb{i}", [128, D], BF16) for i in range(2)]
        stage = [self.sb(f"i_st{i}", [128, KC, 512], BF16) for i in range(2)]
        for t in range(NT):
            b = t % 2
            S.dma("sp", xin[b][:], d["x"][t * 128:(t + 1) * 128, :], key=f"i_x{b}", writes=[f"i_x{b}"])
            S.op("act", lambda b=b: nc.scalar.copy(xb[b][:], xin[b][:]), [f"i_x{b}"], [f"i_xb{b}"])
            sg = (t // 4) % 2
            self.emit_xT(xb[b], f"i_xb{b}", stage[sg], f"i_st{sg}_", t % 4, t % 2)
            if t % 4 == 3:
                blk = t // 4
                S.dma("sp", d["xTd"][:, :, blk * 512:(blk + 1) * 512], stage[sg][:], key=f"i_xTd{sg}",
                      reads=[f"i_st{sg}_{j}" for j in range(4)], writes=[("xTd", blk)])
        self.end_phase()

    def load_T(self, dst, dstname, src, R, tmp, tmpname, bank):
        S, nc = self.S, self.nc
        S.dma("sp", tmp[0:R, :], src, key=tmpname, writes=[tmpname])
        pv = self.pb[bank][:, 0:R]
        S.op("pe", lambda: nc.tensor.transpose(pv, tmp[0:R, :], self.ident_f[0:R, 0:R]), [tmpname, "ident_f"], [f"pb{bank}"])
        S.op("dve", lambda: nc.vector.tensor_copy(dst, pv), [f"pb{bank}"], [dstname])

    def phase_even(self, layer):
        S, nc, d = self.S, self.nc, self.d
        i = self.PI(layer)
        self.begin_phase()
        sb = self.sb
        self.alloc_finish()
        self.load_ln_params("ln_mix_g", "ln_mix_b", layer, router_layer=layer)
        w_in = sb("e_win", [128, KC, 2560], BF16)
        w_out = sb("e_wout", [128, KC, D], BF16)
        for k in range(KC):
            S.dma("pool", w_in[:, k, :], d["ev_w_in"][i, k * 128:(k + 1) * 128, :], key="e_win", writes=["e_win"])
        for k in range(KC):
            S.dma("pool", w_out[:, k, :], d["ev_w_out"][i, k * 128:(k + 1) * 128, :], key="e_wout", writes=["e_wout"])
        tmpT = sb("e_tmpT", [128, 128], F32)
        cw = sb("e_cw", [128, 12], F32)
        self.load_T(cw[:, :], "e_cw", d["ev_conv_w"][i].rearrange("k (c p) -> (k c) p", p=128), 12, tmpT, "e_tmpT", 0)
        lg = sb("e_lg", [128, 512], F32)
        lb = sb("e_lb", [128, 512], F32)
        S.dma("sp", lg[:], d["ev_sgu_ln_g"][i:i + 1, :].to_broadcast([128, 512]), key="e_lg", writes=["e_lg"])
        S.dma("sp", lb[:], d["ev_sgu_ln_b"][i:i + 1, :].to_broadcast([128, 512]), key="e_lb", writes=["e_lb"])
        wraw = sb("e_wraw", [128, 8, 128], F32)
        wrb = sb("e_wrb", [128, 8, 128], BF16)
        wsT = sb("e_wsT", [128, 8, 128], BF16)
        S.dma("sp", wraw[:], d["ev_sgu_w"][i].rearrange("g i j -> i g j"), key="e_wraw", writes=["e_wraw"])
        S.op("pool", lambda: nc.gpsimd.memset(wraw[0:64, :, 64:128], 0.0), ["e_wraw"], ["e_wraw"])
        S.op("dve", lambda: nc.vector.tensor_copy(wrb[:], wraw[:]), ["e_wraw"], ["e_wrb"])
        pv = self.bfv(1)

        def ftr():
            ins = None
            for g in range(8):
                ins = nc.tensor.transpose(pv[:, g * 128:(g + 1) * 128], wrb[:, g, :], self.ident_b[:])
            return ins
        S.op("pe", ftr, ["e_wrb", "ident_b"], ["pb1"])
        S.op("dve", lambda: nc.vector.tensor_copy(wsT[:], pv.rearrange("p (g i) -> p g i", g=8)), ["pb1"], ["e_wsT"])
        sgub = sb("e_sgub", [128, 4, 4, 128], F32)
        for g in range(8):
            k, hh = g // 2, g % 2
            for tt in range(4):
                S.dma("sp", sgub[hh * 64:(hh + 1) * 64, k, tt, :], d["ev_sgu_b"][i, g:g + 1, :].to_broadcast([64, 128]),
                      key="e_sgub", writes=["e_sgub"])
        xblk = [sb(f"e_xblk{b}", [128, KC, 512], BF16) for b in range(2)]
        hbuf = sb("e_h", [128, 4, 514], F32)
        S.op("pool", lambda: nc.gpsimd.memset(hbuf[:, :, 0:2], 0.0), [], ["e_h0", "e_h1", "e_h2", "e_h3"])
        mix = sb("e_mix", [128, KC, 512], BF16)
        ug = sb("e_ug", [128, 4, 512], F32)
        t1 = sb("e_t1", [128, 512], F32)
        t2 = sb("e_t2", [128, 512], F32)
        t3 = sb("e_t3", [128, 512], F32)
        t4 = sb("e_t4", [128, 512], F32)
        vz = sb("e_vz", [128, 512], F32)
        vc = sb("e_vc", [128, 512], F32)
        vs = sb("e_vs", [128, 32], F32)
        vnb = sb("e_vnb", [128, 4, 512], BF16)
        z = sb("e_z", [128, D], F32)
        xsrc = d["x"] if (layer == 0 or self.single is not None) else d["xres"]
        PB = self.pb

        def gelu(src_ps, srcname, dst, dstname):
            self.act(dst, src_ps, AF.Gelu_apprx_tanh, [srcname], [dstname])

        for tb in range(NT // 4):
            xb = xblk[tb % 2]
            xbn = f"e_xblk{tb % 2}"
            S.dma("sp", xb[:], d["xTd"][:, :, tb * 512:(tb + 1) * 512], key=xbn, reads=[("xTd", tb)], writes=[xbn])

            def proj_fm(bank, col0):
                S.op("pe", self.mm_group(PB[bank][:], [(w_in[:, k, col0:col0 + 128], xb[:, k, :]) for k in range(KC)]),
                     reads=["e_win", xbn], writes=[f"pb{bank}"])
            for c in range(4):
                proj_fm(0, c * 128)
                proj_fm(1, 1024 + c * 128)
                proj_fm(2, 512 + c * 128)
                S.op("act", lambda: nc.scalar.copy(t2[:], PB[0][:]), ["pb0"], ["e_t2"])
                hn = f"e_h{c}"
                self.tt("dve", hbuf[:, c, 2:514], t2[:], PB[1][:], ALU.mult, ["e_t2", "pb1"], [hn])
                self.ts("pool", t3[:], hbuf[:, c, 2:514], cw[:, 8 + c:9 + c], None, ALU.mult, None, [hn, "e_cw"], ["e_t3"])
                self.stt("dve", t3[:], hbuf[:, c, 1:513], cw[:, 4 + c:5 + c], t3[:], ALU.mult, ALU.add, [hn, "e_cw", "e_t3"], ["e_t3"])
                self.stt("dve", t3[:], hbuf[:, c, 0:512], cw[:, c:c + 1], t3[:], ALU.mult, ALU.add, [hn, "e_cw", "e_t3"], ["e_t3"])
                self.tt("dve", mix[:, c, :], t3[:], PB[2][:], ALU.mult, ["e_t3", "pb2"], ["e_mix"])
                S.op("pool", lambda c=c: nc.gpsimd.tensor_copy(hbuf[:, c, 0:2], hbuf[:, c, 512:514]), [hn], [hn])
            for c in range(4):
                proj_fm(3, 1536 + c * 128)
                gelu(PB[3][:], "pb3", ug[:, c, :], f"e_ug{c}")
            for tt in range(4):
                tok = slice(tt * 128, (tt + 1) * 128)
                S.op("pe", self.mm_group(PB[4][:], [(xb[:, k, tok], w_in[:, k, 2048:2560]) for k in range(KC)]),
                     reads=["e_win", xbn], writes=["pb4"])
                gelu(PB[4][:], "pb4", vz[:], "e_vz")
                vz3 = vz[:].rearrange("p (g c) -> p g c", g=8)
                vc3 = vc[:].rearrange("p (g c) -> p g c", g=8)
                self.rsum("dve", vs[:, 0:8], vz3, ["e_vz"], ["e_vs0"])
                self.ts("dve", vs[:, 8:16], vs[:, 0:8], 1.0 / 64, None, ALU.mult, None, ["e_vs0"], ["e_vs1"])
                self.tt("dve", vc3, vz3, vs[:, 8:16].unsqueeze(2).to_broadcast([128, 8, 64]), ALU.subtract, ["e_vz", "e_vs1"], ["e_vc"])
                self.act(vz[:], vc[:], AF.Square, ["e_vc"], ["e_vz"])
                self.rsum("dve", vs[:, 16:24], vz3, ["e_vz"], ["e_vs2"])
                self.ts("dve", vs[:, 16:24], vs[:, 16:24], 1.0 / 64, EPS, ALU.mult, ALU.add, ["e_vs2"], ["e_vs2"])
                self.act(vs[:, 24:32], vs[:, 16:24], AF.Sqrt, ["e_vs2"], ["e_vs3"])
                S.op("dve", lambda: nc.vector.reciprocal(vs[:, 24:32], vs[:, 24:32]), ["e_vs3"], ["e_vs3"])
                self.tt("dve", vc3, vc3, vs[:, 24:32].unsqueeze(2).to_broadcast([128, 8, 64]), ALU.mult, ["e_vc", "e_vs3"], ["e_vc"])
                self.tt("pool", vc[:], vc[:], lg[:], ALU.mult, ["e_vc", "e_lg"], ["e_vc"])
                self.tt("pool", vnb[:, tt, :], vc[:], lb[:], ALU.add, ["e_vc", "e_lb"], [f"e_vnb{tt}"])
            for k in range(4):
                def fsg(k=k):
                    ins = None
                    for tt in range(4):
                        nc.tensor.matmul(PB[5][:, tt * 128:(tt + 1) * 128], vnb[:, tt, k * 128:(k + 1) * 128], wsT[:, 2 * k, :], start=True, stop=True)
                        ins = nc.tensor.matmul(PB[6][:, tt * 128:(tt + 1) * 128], vnb[:, tt, k * 128:(k + 1) * 128], wsT[:, 2 * k + 1, :], start=True, stop=True)
                    return ins
                S.op("pe", fsg, reads=[f"e_vnb{tt}" for tt in range(4)] + ["e_wsT"], writes=["pb5", "pb6"])
                sg = sgub[:, k, :, :].rearrange("p t i -> p (t i)")
                self.tt("dve", t4[0:64, :], PB[5][0:64, :], sg[0:64, :], ALU.add, ["pb5", "e_sgub"], ["e_t4a"])
                self.tt("dve", t4[64:128, :], PB[6][64:128, :], sg[64:128, :], ALU.add, ["pb6", "e_sgub"], ["e_t4b"])
                self.tt("pool", mix[:, 4 + k, :], t4[:], ug[:, k, :], ALU.mult, ["e_t4a", "e_t4b", f"e_ug{k}"], ["e_mix"])
            for tt in range(4):
                t = tb * 4 + tt
                tok = slice(tt * 128, (tt + 1) * 128)
                S.dma("sp", z[:], xsrc[t * 128:(t + 1) * 128, :], key="e_z", reads=[("res", t)], writes=["e_z"])
                for nb in range(2):
                    bank = 5 + nb
                    S.op("pe", self.mm_group(PB[bank][:], [(mix[:, k, tok], w_out[:, k, nb * 512:(nb + 1) * 512]) for k in range(KC)]),
                         reads=["e_mix", "e_wout"], writes=[f"pb{bank}"])
                    self.stt("dve", z[:, nb * 512:(nb + 1) * 512], z[:, nb * 512:(nb + 1) * 512], DN_ALPHA, PB[bank][:],
                             ALU.mult, ALU.add, ["e_z", f"pb{bank}"], ["e_z"])
                self.finish_tile(z, "e_z", t, d["xres"], True, [7, 0, 1, 2])
        self.end_phase()

    def phase_copy_out(self):
        S, d = self.S, self.d
        self.begin_phase()
        for q in range(S_TOK // 512):
            S.dma("sp", self.out[q * 512:(q + 1) * 512, :], d["xres"][q * 512:(q + 1) * 512, :], key="cp_out", writes=[("o", q)])
        self.end_phase(final=True)

    def build(self):
        self.declare()
        if self.single is not None:
            layer = self.single
            self.phase_init()
            if layer % 2 == 0:
                self.phase_even(layer)
            else:
                self.phase_odd(layer)
            self.phase_moe(layer, final=True)
            return self.nc
        stop = self.stop_after
        self.phase_init()
        for layer in range(DEPTH):
            if layer % 2 == 0:
                self.phase_even(layer)
            else:
                self.phase_odd(layer)
            if stop == f"mix{layer}":
                self.phase_copy_out()
                return self.nc
            self.phase_moe(layer, final=(layer == DEPTH - 1))
            if stop == f"ffn{layer}":
                if layer != DEPTH - 1:
                    self.phase_copy_out()
                return self.nc
        return self.nc


def _consts():
    c = {}
    c["c_ident"] = np.eye(128, dtype=np.float32)
    i = np.arange(128)
    c["c_tri"] = (i[:, None] <= i[None, :]).astype(np.float32)
    c["c_maskc"] = ((i[:, None] // 64) <= (i[None, :] // 64)).astype(np.float32)
    c["c_maskf"] = (i[:, None] <= i[None, :]).astype(np.float32)
    inv = (10000.0 ** (-np.arange(0, 32, 2, dtype=np.float32) / 32)).astype(np.float32)
    ang = (np.arange(S_TOK, dtype=np.float32)[:, None] * inv[None, :]).astype(np.float32)
    c["c_cos"] = np.cos(ang).astype(np.float32)
    c["c_sin"] = np.sin(ang).astype(np.float32)
    return c


def _prep_weights(inputs):
    w = {}
    for k, v in inputs.items():
        if k in ("x", "moe_w1", "moe_b1"):
            continue
        w[k] = np.ascontiguousarray(np.asarray(v, dtype=np.float32))
    w1 = np.asarray(inputs["moe_w1"], dtype=np.float32)
    L, E = w1.shape[0], w1.shape[1]
    w1v = w1.reshape(L, E, D, 8, 128, 2)
    w1p = np.ascontiguousarray(w1v.transpose(0, 1, 3, 2, 5, 4)).reshape(L, E, 8, D, 256)
    w["moe_w1p"] = w1p
    b1 = np.asarray(inputs["moe_b1"], dtype=np.float32).reshape(L, E, 8, 128, 2)
    b1p = np.ascontiguousarray(b1.transpose(0, 1, 2, 4, 3)).reshape(L, E * 16, 128)
    w["moe_b1p"] = b1p
    w.update(_consts())
    return w


_CACHE = {}


def _get_prog(stop_after=None, single=None):
    key = (stop_after, single)
    if key not in _CACHE:
        p = Prog(stop_after, single)
        p.build()
        _CACHE[key] = p
    return _CACHE[key]


_EV = ("ev_w_in", "ev_conv_w", "ev_sgu_ln_g", "ev_sgu_ln_b", "ev_sgu_w", "ev_sgu_b", "ev_w_out")
_OD = ("od_w_in", "od_q_norm_g", "od_kv_norm_g", "od_w_uq", "od_w_ukv", "od_f_bias", "od_w_out")
_LY = ("ln_mix_g", "ln_mix_b", "ln_ffn_g", "ln_ffn_b", "moe_w_router", "moe_b_router", "moe_w1p", "moe_b1p", "moe_w2", "moe_b2")
_CO = ("c_tri", "c_maskc", "c_maskf", "c_cos", "c_sin")


def run(inputs, stop_after=None, cores=None, trace=False):
    x = np.asarray(inputs["x"], dtype=np.float32)
    n = x.shape[0]
    cores = list(range(n)) if cores is None else cores
    w = _prep_weights(inputs)
    p = _get_prog(stop_after)
    in_maps = []
    for c in cores:
        m = dict(w)
        m["x"] = np.ascontiguousarray(x[c])
        in_maps.append(m)
    res = run_bass_kernel_spmd(p.nc, in_maps, core_ids=list(range(len(cores))), trace=trace)
    return np.stack([r["out"] for r in res.results], axis=0), res


def run_layers(inputs, cores=None, nlayers=DEPTH):
    x = np.asarray(inputs["x"], dtype=np.float32)
    n = x.shape[0]
    cores = list(range(n)) if cores is None else cores
    w = _prep_weights(inputs)
    cur = [np.ascontiguousarray(x[c]) for c in cores]
    for L in range(nlayers):
        par = L % 2
        p = _get_prog(None, single=par)
        base = {"c_ident": w["c_ident"]}
        fam = _EV if par == 0 else _OD
        for k in fam:
            base[k] = w[k][L // 2:L // 2 + 1]
        for k in _LY:
            base[k] = w[k][L:L + 1]
        if par == 1:
            for k in _CO:
                base[k] = w[k]
        in_maps = []
        for ci in range(len(cores)):
            m = dict(base)
            m["x"] = cur[ci]
            in_maps.append(m)
        res = run_bass_kernel_spmd(p.nc, in_maps, core_ids=list(range(len(cores))))
        cur = [np.ascontiguousarray(r["out"]) for r in res.results]
    return np.stack(cur, axis=0)


def kernel(**inputs):
    out = run_layers(inputs)
    return out.astype(np.float32)


def _phase_moe(self, layer, final):
    S, nc, d = self.S, self.nc, self.d
    layer_true = layer
    layer = self.LI(layer)
    self.begin_phase()
    sb = self.sb
    PB = self.pb
    NH = S_TOK // 2
    NTH = NH // 128
    NBH = NH // 512
    tmpT = sb("m_tmpT", [128, 128], F32)
    b1T = sb("m_b1T", [128, NE * 16], F32)
    for q in range(4):
        self.load_T(b1T[:, q * 128:(q + 1) * 128], "m_b1T", d["moe_b1p"][layer, q * 128:(q + 1) * 128, :], 128, tmpT, "m_tmpT", q % 2)
    b2 = sb("m_b2", [NE, D], BF16)
    S.dma("pool", b2[:], d["moe_b2"][layer], key="m_b2", writes=["m_b2"])
    xh = sb("m_xh", [128, KC, NH], BF16)
    acc = sb("m_acc", [128, NTH, D], F32)
    actb = sb("m_act", [128, 8, NH], BF16)
    NR = 3
    w1r = [sb(f"m_w1_{i}", [128, KC, 256], BF16) for i in range(NR)]
    w2b = sb("m_w2", [128, KC, D], BF16)
    gt = [sb(f"m_gt{i}", [128, 512], F32) for i in range(2)]
    st = [sb(f"m_st{i}", [128, 512], F32) for i in range(2)]
    lt = [sb(f"m_lt{i}", [128, 512], F32) for i in range(2)]

    def w1_dma(hf, e, fc):
        n = (hf * NE + e) * 8 + fc
        r = n % NR
        S.dma("pool", w1r[r][:], d["moe_w1p"][layer, e, fc].rearrange("(k p) c -> p k c", p=128),
              key=f"m_w1_{r}", writes=[f"m_w1_{r}"])

    for hf in range(2):
        S.dma("sp", xh[:], d["xTd"][:, :, hf * NH:(hf + 1) * NH], key="m_xh",
              reads=[("xTd", b) for b in range(hf * NBH, hf * NBH + NBH)], writes=["m_xh"])
        for tt in range(NTH):
            t = hf * NTH + tt
            for nb in range(2):
                bank = 4 + 2 * (tt % 2) + nb
                S.op("pe", self.mm_group(PB[bank][:], [(self.gT[:, t * 128:(t + 1) * 128], b2[:, nb * 512:(nb + 1) * 512])]),
                     reads=[("gT", t), "m_b2"], writes=[f"pb{bank}"])
                S.op("act", lambda tt=tt, nb=nb, bank=bank: nc.scalar.copy(acc[:, tt, nb * 512:(nb + 1) * 512], PB[bank][:]),
                     [f"pb{bank}"], [("m_acc", tt, nb)])
        w1_dma(hf, 0, 0)
        w1_dma(hf, 0, 1)
        unit = 0
        for e in range(NE):
            S.dma("pool", w2b[:], d["moe_w2"][layer, e].rearrange("(k p) n -> p k n", p=128), key="m_w2", writes=["m_w2"])
            for fc in range(8):
                nxt = e * 8 + fc + 2
                if nxt < NE * 8:
                    w1_dma(hf, nxt // 8, nxt % 8)
                r = ((hf * NE + e) * 8 + fc) % NR
                wp = w1r[r]
                wn = f"m_w1_{r}"
                cg = e * 16 + fc * 2
                for tb in range(NBH):
                    s = unit % 2
                    unit += 1
                    bg, bl = 2 * s, 2 * s + 1
                    tok = slice(tb * 512, (tb + 1) * 512)
                    S.op("pe", self.mm_group(PB[bg][:], [(wp[:, k, 0:128], xh[:, k, tok]) for k in range(KC)]),
                         reads=[wn, "m_xh"], writes=[f"pb{bg}"])
                    S.op("pe", self.mm_group(PB[bl][:], [(wp[:, k, 128:256], xh[:, k, tok]) for k in range(KC)]),
                         reads=[wn, "m_xh"], writes=[f"pb{bl}"])
                    self.ts("dve", gt[s][:], PB[bg][:], b1T[:, cg:cg + 1], 7.0, ALU.add, ALU.min, [f"pb{bg}", "m_b1T"], [f"m_gt{s}"])
                    self.act(st[s][:], gt[s][:], AF.Sigmoid, [f"m_gt{s}"], [f"m_st{s}"], scale=1.702)
                    self.act(lt[s][:], PB[bl][:], AF.Identity, [f"pb{bl}", "m_b1T"], [f"m_lt{s}"], bias=b1T[:, cg + 1:cg + 2], scale=1.0)
                    self.ts("pool", lt[s][:], lt[s][:], -7.0, 7.0, ALU.max, ALU.min, [f"m_lt{s}"], [f"m_lt{s}"])
                    self.tt("dve", gt[s][:], gt[s][:], st[s][:], ALU.mult, [f"m_gt{s}", f"m_st{s}"], [f"m_gt{s}"])
                    self.stt("dve", actb[:, fc, tok], lt[s][:], 1.0, gt[s][:], ALU.add, ALU.mult,
                             [f"m_lt{s}", f"m_gt{s}"], [("m_act", fc, tb)])
            for tt in range(NTH):
                t = hf * NTH + tt
                tk = slice(tt * 128, (tt + 1) * 128)
                for nb in range(2):
                    bank = 4 + 2 * (tt % 2) + nb
                    S.op("pe", self.mm_group(PB[bank][:], [(actb[:, k, tk], w2b[:, k, nb * 512:(nb + 1) * 512]) for k in range(KC)]),
                         reads=[("m_act", k, tt // 4) for k in range(KC)] + ["m_w2"], writes=[f"pb{bank}"])
                    a = acc[:, tt, nb * 512:(nb + 1) * 512]
                    self.stt("dve", a, PB[bank][:], self.gates[:, t, e:e + 1], a, ALU.mult, ALU.add,
                             [f"pb{bank}", ("gates", t), ("m_acc", tt, nb)], [("m_acc", tt, nb)])
        for tt in range(NTH):
            t = hf * NTH + tt
            S.dma("sp", d["ffn"][t * 128:(t + 1) * 128, :], acc[:, tt, :], key=f"m_ffn{tt}",
                  reads=[("m_acc", tt, 0), ("m_acc", tt, 1)], writes=[("ffn", t)])
    self.end_phase()
    self.phase_ffn_ln(layer_true, final)


def _phase_ffn_ln(self, layer, final):
    S, nc, d = self.S, self.nc, self.d
    self.begin_phase()
    self.alloc_finish()
    self.load_ln_params("ln_ffn_g", "ln_ffn_b", layer, router_layer=None)
    z = [self.sb(f"l_z{i}", [128, D], F32) for i in range(2)]
    f = [self.sb(f"l_f{i}", [128, D], F32) for i in range(2)]
    dst = self.out if final else d["xres"]
    for t in range(NT):
        b = t % 2
        S.dma("sp", z[b][:], d["xres"][t * 128:(t + 1) * 128, :], key=f"l_z{b}", reads=[("res", t)], writes=[f"l_z{b}"])
        S.dma("sp", f[b][:], d["ffn"][t * 128:(t + 1) * 128, :], key=f"l_f{b}", reads=[("ffn", t)], writes=[f"l_f{b}"])
        self.stt("dve", z[b][:], z[b][:], DN_ALPHA, f[b][:], ALU.mult, ALU.add, [f"l_z{b}", f"l_f{b}"], [f"l_z{b}"])
        self.finish_tile(z[b], f"l_z{b}", t, dst, False, [t % 2, 2, 3, 4])
    self.end_phase(final=final)


Prog.phase_moe = _phase_moe
Prog.phase_ffn_ln = _phase_ffn_ln


SC_C = float(1.0 / np.sqrt(96.0))
SC_F = 0.125


def _phase_odd(self, layer):
    self.phase_odd_a(layer)
    self.phase_odd_b(layer)
    self.phase_odd_c(layer)


def _phase_odd_a(self, layer):
    S, nc, d = self.S, self.nc, self.d
    i = self.PI(layer)
    self.begin_phase()
    sb = self.sb
    PB = self.pb
    if not hasattr(self, "kb_all"):
        self.kb_all = S.sbuf("kb_all", [128, NT, 16], F32)
        self.ef_all = S.sbuf("ef_all", [128, NT, 16], F32)
        self.dscratch("QTd", [16, 128, S_TOK], BF16)
        self.dscratch("KTd", [16, 128, S_TOK], BF16)
        self.dscratch("VPd", [S_TOK, 16, 64], BF16)
        self.dscratch("mixTd", [128, KC, S_TOK], BF16)
    kb_all, ef_all = self.kb_all, self.ef_all
    w_in = sb("a_win", [128, KC, 1960], BF16)
    for k in range(KC):
        S.dma("pool", w_in[:, k, :], d["od_w_in"][i, k * 128:(k + 1) * 128, :], key="a_win", writes=["a_win"])
    w_uq = sb("a_wuq", [128, 2, 768], BF16)
    S.dma("pool", w_uq[:], d["od_w_uq"][i].rearrange("(c p) n -> p c n", p=128), key="a_wuq", writes=["a_wuq"])
    w_ukv = sb("a_wukv", [128, 1024], BF16)
    S.dma("pool", w_ukv[:], d["od_w_ukv"][i], key="a_wukv", writes=["a_wukv"])
    gq = sb("a_gq", [128, 256], F32)
    gkv = sb("a_gkv", [128, 128], F32)
    fb = sb("a_fb", [128, 8], F32)
    S.dma("sp", gq[:], d["od_q_norm_g"][i:i + 1, :].to_broadcast([128, 256]), key="a_gq", writes=["a_gq"])
    S.dma("sp", gkv[:], d["od_kv_norm_g"][i:i + 1, :].to_broadcast([128, 128]), key="a_gkv", writes=["a_gkv"])
    S.dma("sp", fb[:], d["od_f_bias"][i:i + 1, :].to_broadcast([128, 8]), key="a_fb", writes=["a_fb"])
    cos = sb("a_cos", [128, NT, 16], F32)
    sin = sb("a_sin", [128, NT, 16], F32)
    S.dma("sp", cos[:], d["c_cos"].rearrange("(t p) f -> p t f", p=128), key="a_cos", writes=["a_cos"])
    S.dma("sp", sin[:], d["c_sin"].rearrange("(t p) f -> p t f", p=128), key="a_sin", writes=["a_sin"])
    tri = sb("a_tri", [128, 128], F32)
    ones = sb("a_ones", [128, 128], F32)
    S.dma("sp", tri[:], d["c_tri"], key="a_tri", writes=["a_tri"])
    S.op("pool", lambda: nc.gpsimd.memset(ones[:], 1.0), [], ["a_ones"])
    carry = sb("a_carry", [128, 8], F32)
    S.op("pool", lambda: nc.gpsimd.memset(carry[:], 0.0), [], ["a_carry"])
    xblk = [sb(f"a_xblk{b}", [128, KC, 512], BF16) for b in range(2)]
    sq = sb("a_sq", [128, 1024], F32)
    sm = sb("a_sm", [128, 64], F32)
    cqn = sb("a_cqn", [128, 256], F32)
    cqb = sb("a_cqb", [128, 384], BF16)
    cT = sb("a_cT", [128, 3, 128], BF16)
    qf = sb("a_qf", [128, 768], F32)
    kvf = sb("a_kvf", [128, 1024], F32)
    krf = sb("a_krf", [128, 32], F32)
    kro = sb("a_kro", [128, 32], F32)
    rt = sb("a_rt", [128, 8, 32], F32)
    krt = sb("a_krt", [128, 16], F32)
    kro2 = sb("a_kro2", [128, 32], F32)
    qaug = sb("a_qaug", [128, 8, 98], BF16)
    kaug = sb("a_kaug", [128, 8, 98], BF16)
    vp = sb("a_vp", [128, 16, 64], BF16)
    fqf = sb("a_fqf", [128, 512], F32)
    fkf = sb("a_fkf", [128, 512], F32)
    qaugf = sb("a_qaugf", [128, 8, 66], BF16)
    kaugf = sb("a_kaugf", [128, 8, 66], BF16)
    Ft = sb("a_Ft", [128, 8], F32)
    hib = sb("a_hib", [128, 16], BF16)
    hif = sb("a_hif", [128, 16], F32)
    av = sb("a_av", [128, 16], F32)
    stq = sb("a_stq", [128, 8, 128], BF16)
    stk = sb("a_stk", [128, 8, 128], BF16)
    stqf = sb("a_stqf", [128, 8, 128], BF16)
    stkf = sb("a_stkf", [128, 8, 128], BF16)
    S.op("pool", lambda: nc.gpsimd.memset(kaug[:, :, 96:98], 1.0), [], ["a_kaug_one"])
    S.op("pool", lambda: nc.gpsimd.memset(kaugf[:, :, 64:66], 1.0), [], ["a_kaugf_one"])

    def rstd(dst, src, n, name):
        self.ts("dve", dst, src, 1.0 / n, EPS, ALU.mult, ALU.add, [name + "_s"], [name])
        self.act(dst, dst, AF.Sqrt, [name], [name])
        S.op("dve", lambda: nc.vector.reciprocal(dst, dst), [name], [name])

    def hilo(a, n, dst_hi, dst_lo, aname, outname):
        S.op("dve", lambda: nc.vector.tensor_copy(hib[:, 0:n], a), [aname], ["a_hib"])
        S.op("dve", lambda: nc.vector.tensor_copy(hif[:, 0:n], hib[:, 0:n]), ["a_hib"], ["a_hif"])
        S.op("dve", lambda: nc.vector.tensor_copy(dst_hi, hib[:, 0:n]), ["a_hib"], [outname + "_hi"])
        self.tt("dve", dst_lo, a, hif[:, 0:n], ALU.subtract, [aname, "a_hif"], [outname + "_lo"])

    def transposes(src, nh, da, bank, stage, stname, srcnames):
        pv = self.bfv(bank)

        def f():
            ins = None
            for h in range(nh):
                ins = nc.tensor.transpose(pv[0:da, h * 128:(h + 1) * 128], src[:, h, :], self.ident_b[:])
            return ins
        S.op("pe", f, srcnames + ["ident_b"], [f"pb{bank}"])
        S.op("act", lambda: nc.scalar.copy(stage[0:da, :, :], pv[0:da, :].rearrange("p (h t) -> p h t", h=nh)), [f"pb{bank}"], [stname])

    for t in range(NT):
        tb, tt = t // 4, t % 4
        xb = xblk[tb % 2]
        xbn = f"a_xblk{tb % 2}"
        if tt == 0:
            S.dma("sp", xb[:], d["xTd"][:, :, tb * 512:(tb + 1) * 512], key=xbn, reads=[("xTd", tb)], writes=[xbn])
        tok = slice(tt * 128, (tt + 1) * 128)

        def fA(xb=xb, tok=tok):
            ins = None
            for k in range(KC):
                nc.tensor.matmul(PB[0][:, 0:416], xb[:, k, tok], w_in[:, k, 0:416], start=(k == 0), stop=(k == KC - 1))
            for k in range(KC):
                ins = nc.tensor.matmul(PB[0][:, 416:424], xb[:, k, tok], w_in[:, k, 1952:1960], start=(k == 0), stop=(k == KC - 1))
            return ins
        S.op("pe", fA, ["a_win", xbn], ["pb0"])
        for bi, c0 in ((1, 416), (2, 928), (3, 1440)):
            S.op("pe", self.mm_group(PB[bi][:], [(xb[:, k, tok], w_in[:, k, c0:c0 + 512]) for k in range(KC)]),
                 ["a_win", xbn], [f"pb{bi}"])
        self.act(sq[:, 0:384], PB[0][:, 0:384], AF.Square, ["pb0"], ["a_sq"])
        self.rsum("dve", sm[:, 0:1], sq[:, 0:256], ["a_sq"], ["a_sm0_s"])
        self.rsum("dve", sm[:, 1:2], sq[:, 256:384], ["a_sq"], ["a_sm1_s"])
        rstd(sm[:, 0:1], sm[:, 0:1], 256, "a_sm0")
        rstd(sm[:, 1:2], sm[:, 1:2], 128, "a_sm1")
        self.stt("dve", cqn[:, 0:256], PB[0][:, 0:256], sm[:, 0:1], gq[:], ALU.mult, ALU.mult, ["pb0", "a_sm0", "a_gq"], ["a_cqn"])
        S.op("act", lambda: nc.scalar.copy(cqb[:, 0:256], cqn[:, 0:256]), ["a_cqn"], ["a_cqb0"])
        self.stt("dve", cqb[:, 256:384], PB[0][:, 256:384], sm[:, 1:2], gkv[:], ALU.mult, ALU.mult, ["pb0", "a_sm1", "a_gkv"], ["a_cqb1"])
        S.op("act", lambda: nc.scalar.copy(krf[:], PB[0][:, 384:416]), ["pb0"], ["a_krf"])
        self.tt("dve", sm[:, 8:16], PB[0][:, 416:424], fb[:], ALU.add, ["pb0", "a_fb"], ["a_zb"])
        self.act(sm[:, 8:16], sm[:, 8:16], AF.Exp, ["a_zb"], ["a_zb"], scale=-1.0)
        self.act(sm[:, 8:16], sm[:, 8:16], AF.Ln, ["a_zb"], ["a_zb"], bias=1.0, scale=1.0)
        pv4 = self.bfv(4)

        def fT():
            ins = None
            for c in range(3):
                ins = nc.tensor.transpose(pv4[:, c * 128:(c + 1) * 128], cqb[:, c * 128:(c + 1) * 128], self.ident_b[:])
            return ins
        S.op("pe", fT, ["a_cqb0", "a_cqb1", "ident_b"], ["pb4"])
        S.op("act", lambda: nc.scalar.copy(cT[:], pv4[:, 0:384].rearrange("p (c t) -> p c t", c=3)), ["pb4"], ["a_cT"])
        S.op("pe", self.mm_group(PB[5][:], [(cT[:, c, :], w_uq[:, c, 0:512]) for c in range(2)]), ["a_cT", "a_wuq"], ["pb5"])
        S.op("pe", self.mm_group(PB[6][:, 0:256], [(cT[:, c, :], w_uq[:, c, 512:768]) for c in range(2)]), ["a_cT", "a_wuq"], ["pb6"])
        S.op("act", lambda: nc.scalar.copy(qf[:, 0:512], PB[5][:]), ["pb5"], ["a_qf0"])
        S.op("act", lambda: nc.scalar.copy(qf[:, 512:768], PB[6][:, 0:256]), ["pb6"], ["a_qf1"])
        S.op("pe", self.mm_group(PB[7][:], [(cT[:, 2, :], w_ukv[:, 0:512])]), ["a_cT", "a_wukv"], ["pb7"])
        S.op("pe", self.mm_group(PB[4][:], [(cT[:, 2, :], w_ukv[:, 512:1024])]), ["a_cT", "a_wukv"], ["pb4"])
        S.op("act", lambda: nc.scalar.copy(kvf[:, 0:512], PB[7][:]), ["pb7"], ["a_kvf0"])
        S.op("act", lambda: nc.scalar.copy(kvf[:, 512:1024], PB[4][:]), ["pb4"], ["a_kvf1"])
        q3 = qf[:].rearrange("p (h c) -> p h c", h=8)
        kv3 = kvf[:].rearrange("p (h c) -> p h c", h=8)
        QN = ["a_qf0", "a_qf1"]
        KN = ["a_kvf0", "a_kvf1"]
        sq3 = sq[:, 0:768].rearrange("p (h c) -> p h c", h=8)
        self.tt("pool", sq[:, 0:768], qf[:], qf[:], ALU.mult, QN, ["a_sq"])
        self.rsum("dve", av[:, 0:8], sq3, ["a_sq"], ["a_av_q"])
        self.ts("dve", av[:, 0:8], av[:, 0:8], -0.5 * SC_C, None, ALU.mult, None, ["a_av_q"], ["a_av_q"])
        hilo(av[:, 0:8], 8, qaug[:, :, 96], qaug[:, :, 97], "a_av_q", "a_qaug")
        self.ts("dve", qaug[:, :, 0:64], q3[:, :, 0:64], SC_C, None, ALU.mult, None, QN, ["a_qaug_n"])
        cosb = cos[:, t, :].unsqueeze(1).to_broadcast([128, 8, 16])
        sinb = sin[:, t, :].unsqueeze(1).to_broadcast([128, 8, 16])
        x1, x2 = q3[:, :, 64:80], q3[:, :, 80:96]
        self.tt("dve", rt[:, :, 0:16], x1, cosb, ALU.mult, QN + ["a_cos"], ["a_rt0"])
        self.tt("dve", rt[:, :, 16:32], x2, sinb, ALU.mult, QN + ["a_sin"], ["a_rt1"])
        self.tt("dve", rt[:, :, 0:16], rt[:, :, 0:16], rt[:, :, 16:32], ALU.subtract, ["a_rt0", "a_rt1"], ["a_rt0"])
        self.ts("dve", qaug[:, :, 64:80], rt[:, :, 0:16], SC_C, None, ALU.mult, None, ["a_rt0"], ["a_qaug_r1"])
        self.tt("dve", rt[:, :, 0:16], x2, cosb, ALU.mult, QN + ["a_cos"], ["a_rt0"])
        self.tt("dve", rt[:, :, 16:32], x1, sinb, ALU.mult, QN + ["a_sin"], ["a_rt1"])
        self.tt("dve", rt[:, :, 0:16], rt[:, :, 0:16], rt[:, :, 16:32], ALU.add, ["a_rt0", "a_rt1"], ["a_rt0"])
        self.ts("dve", qaug[:, :, 80:96], rt[:, :, 0:16], SC_C, None, ALU.mult, None, ["a_rt0"], ["a_qaug_r2"])
        c1, s1 = cos[:, t, :], sin[:, t, :]
        self.tt("pool", kro[:, 0:16], krf[:, 0:16], c1, ALU.mult, ["a_krf", "a_cos"], ["a_kro0"])
        self.tt("pool", krt[:], krf[:, 16:32], s1, ALU.mult, ["a_krf", "a_sin"], ["a_krt"])
        self.tt("pool", kro[:, 0:16], kro[:, 0:16], krt[:], ALU.subtract, ["a_kro0", "a_krt"], ["a_kro0"])
        self.tt("pool", kro[:, 16:32], krf[:, 16:32], c1, ALU.mult, ["a_krf", "a_cos"], ["a_kro1"])
        self.tt("pool", krt[:], krf[:, 0:16], s1, ALU.mult, ["a_krf", "a_sin", "a_krt"], ["a_krt"])
        self.tt("pool", kro[:, 16:32], kro[:, 16:32], krt[:], ALU.add, ["a_kro1", "a_krt"], ["a_kro1"])
        S.op("act", lambda: nc.scalar.copy(kaug[:, :, 0:64], kv3[:, :, 0:64]), KN, ["a_kaug_n"])
        S.op("dve", lambda: nc.vector.tensor_copy(kaug[:, :, 64:96], kro[:].unsqueeze(1).to_broadcast([128, 8, 32])), ["a_kro0", "a_kro1"], ["a_kaug_r"])
        sqk = sq[:].rearrange("p (h c) -> p h c", h=8)
        self.tt("pool", sq[:], kvf[:], kvf[:], ALU.mult, KN + ["a_sq"], ["a_sq"])
        self.rsum("dve", av[:, 8:16], sqk[:, :, 0:64], ["a_sq"], ["a_av_k"])
        self.tt("pool", kro2[:], kro[:], kro[:], ALU.mult, ["a_kro0", "a_kro1"], ["a_kro2"])
        self.rsum("dve", sm[:, 2:3], kro2[:], ["a_kro2"], ["a_sm2"])
        self.ts("dve", av[:, 8:16], av[:, 8:16], sm[:, 2:3], 0.5 * SC_C, ALU.add, ALU.mult, ["a_av_k", "a_sm2"], ["a_av_k"])
        self.ts("dve", kb_all[:, t, 0:8], av[:, 8:16], -1.0, None, ALU.mult, None, ["a_av_k"], [("kb", t, 0)])
        self.act(ef_all[:, t, 0:8], av[:, 8:16], AF.Exp, ["a_av_k"], [("ef", t, 0)])
        self.tt("dve", vp[:, 0:8, :], kv3[:, :, 64:128], ef_all[:, t, 0:8].unsqueeze(2).to_broadcast([128, 8, 64]), ALU.mult,
                KN + [("ef", t, 0)], ["a_vp0"])
        S.op("pe", self.mm_group(PB[5][:, 0:8], [(tri[:], sm[:, 8:16])]), ["a_tri", "a_zb"], ["pb5"])
        S.op("pe", self.mm_group(PB[6][:, 0:8], [(ones[:], sm[:, 8:16])]), ["a_ones", "a_zb"], ["pb6"])
        self.tt("dve", Ft[:], PB[5][:, 0:8], carry[:], ALU.add, ["pb5", "a_carry"], ["a_Ft"])
        self.tt("dve", carry[:], carry[:], PB[6][:, 0:8], ALU.add, ["pb6", "a_carry"], ["a_carry"])
        S.op("act", lambda: nc.scalar.copy(fqf[:], PB[1][:]), ["pb1"], ["a_fqf"])
        S.op("act", lambda: nc.scalar.copy(fkf[:], PB[2][:]), ["pb2"], ["a_fkf"])
        fq3 = fqf[:].rearrange("p (h c) -> p h c", h=8)
        fk3 = fkf[:].rearrange("p (h c) -> p h c", h=8)
        sqf = sq[:, 0:512].rearrange("p (h c) -> p h c", h=8)
        self.tt("pool", sq[:, 0:512], fqf[:], fqf[:], ALU.mult, ["a_fqf", "a_sq"], ["a_sq"])
        self.rsum("dve", av[:, 0:8], sqf, ["a_sq"], ["a_av_fq"])
        self.stt("dve", av[:, 0:8], av[:, 0:8], 0.5 * SC_F, Ft[:], ALU.mult, ALU.add, ["a_av_fq", "a_Ft"], ["a_av_fq"])
        self.ts("dve", av[:, 0:8], av[:, 0:8], -1.0, None, ALU.mult, None, ["a_av_fq"], ["a_av_fq"])
        hilo(av[:, 0:8], 8, qaugf[:, :, 64], qaugf[:, :, 65], "a_av_fq", "a_qaugf")
        self.ts("dve", qaugf[:, :, 0:64], fq3, SC_F, None, ALU.mult, None, ["a_fqf"], ["a_qaugf_n"])
        S.op("act", lambda: nc.scalar.copy(kaugf[:, :, 0:64], fk3), ["a_fkf"], ["a_kaugf_n"])
        self.tt("pool", sq[:, 0:512], fkf[:], fkf[:], ALU.mult, ["a_fkf", "a_sq"], ["a_sq"])
        self.rsum("dve", av[:, 8:16], sqf, ["a_sq"], ["a_av_fk"])
        self.ts("dve", av[:, 8:16], av[:, 8:16], 0.5 * SC_F, None, ALU.mult, None, ["a_av_fk"], ["a_av_fk"])
        self.tt("dve", kb_all[:, t, 8:16], Ft[:], av[:, 8:16], ALU.subtract, ["a_Ft", "a_av_fk"], [("kb", t, 1)])
        self.act(ef_all[:, t, 8:16], av[:, 8:16], AF.Exp, ["a_av_fk"], [("ef", t, 1)])
        self.tt("dve", vp[:, 8:16, :], PB[3][:].rearrange("p (h c) -> p h c", h=8),
                ef_all[:, t, 8:16].unsqueeze(2).to_broadcast([128, 8, 64]), ALU.mult, ["pb3", ("ef", t, 1)], ["a_vp1"])
        S.dma("sp", d["VPd"][t * 128:(t + 1) * 128, :, :], vp[:], key="a_vpd", reads=["a_vp0", "a_vp1"], writes=[("VPd", t)])
        transposes(qaug, 8, 98, 5, stq, "a_stq", ["a_qaug_hi", "a_qaug_lo", "a_qaug_n", "a_qaug_r1", "a_qaug_r2"])
        S.dma("sp", d["QTd"][0:8, 0:98, t * 128:(t + 1) * 128].rearrange("h r t -> r h t"), stq[0:98, :, :], key="a_qtd", reads=["a_stq"], writes=[("QTd", t, 0)])
        transposes(kaug, 8, 98, 6, stk, "a_stk", ["a_kaug_n", "a_kaug_r", "a_kaug_one"])
        S.dma("sp", d["KTd"][0:8, 0:98, t * 128:(t + 1) * 128].rearrange("h r t -> r h t"), stk[0:98, :, :], key="a_ktd", reads=["a_stk"], writes=[("KTd", t, 0)])
        transposes(qaugf, 8, 66, 7, stqf, "a_stqf", ["a_qaugf_hi", "a_qaugf_lo", "a_qaugf_n"])
        S.dma("sp", d["QTd"][8:16, 0:66, t * 128:(t + 1) * 128].rearrange("h r t -> r h t"), stqf[0:66, :, :], key="a_qtdf", reads=["a_stqf"], writes=[("QTd", t, 1)])
        transposes(kaugf, 8, 66, 4, stkf, "a_stkf", ["a_kaugf_n", "a_kaugf_one"])
        S.dma("sp", d["KTd"][8:16, 0:66, t * 128:(t + 1) * 128].rearrange("h r t -> r h t"), stkf[0:66, :, :], key="a_ktdf", reads=["a_stkf"], writes=[("KTd", t, 1)])
    self.end_phase()


def _phase_odd_b(self, layer):
    S, nc, d = self.S, self.nc, self.d
    self.begin_phase()
    sb = self.sb
    PB = self.pb
    kb_all, ef_all = self.kb_all, self.ef_all
    QT = [sb(f"b_QT{i}", [128, S_TOK], BF16) for i in range(2)]
    KT = [sb(f"b_KT{i}", [128, S_TOK], BF16) for i in range(2)]
    VP = [sb(f"b_VP{i}", [128, NT, 64], BF16) for i in range(2)]
    W = [sb(f"b_W{i}", [128, NT, 64], BF16) for i in range(2)]
    pT = [sb(f"b_pT{i}", [128, 512], BF16) for i in range(3)]
    rl = [sb(f"b_rl{i}", [64, 512], F32) for i in range(2)]
    yo = [sb(f"b_yo{i}", [64, 512], BF16) for i in range(2)]
    mk = sb("b_mk", [128, 2, 128], F32)
    mkb = sb("b_mkb", [128, 2, 128], BF16)
    S.dma("sp", mk[:, 0, :], d["c_maskc"], key="b_mk0", writes=["b_mk0"])
    S.dma("sp", mk[:, 1, :], d["c_maskf"], key="b_mk1", writes=["b_mk1"])
    S.op("dve", lambda: nc.vector.tensor_copy(mkb[:], mk[:]), ["b_mk0", "b_mk1"], ["b_mkb"])
    unit = 0
    for hh in range(16):
        b = hh % 2
        fox = 1 if hh >= 8 else 0
        da = 66 if fox else 98
        S.dma("sp", QT[b][0:da, :], d["QTd"][hh, 0:da, :], key=f"b_QT{b}", reads=[("QTd", t, fox) for t in range(NT)], writes=[f"b_QT{b}"])
        S.dma("sp", KT[b][0:da, :], d["KTd"][hh, 0:da, :], key=f"b_KT{b}", reads=[("KTd", t, fox) for t in range(NT)], writes=[f"b_KT{b}"])
        S.dma("sp", VP[b][:], d["VPd"][:, hh, :].rearrange("(kt p) c -> p kt c", p=128), key=f"b_VP{b}",
              reads=[("VPd", t) for t in range(NT)], writes=[f"b_VP{b}"])
        S.op("pool", lambda b=b, hh=hh: nc.gpsimd.tensor_copy(W[b][:], ef_all[:, :, hh].unsqueeze(2).to_broadcast([128, NT, 64])),
             [("ef", t, fox) for t in range(NT)], [f"b_W{b}"])
        for j in range(S_TOK // 512):
            ob = 4 + 2 * (j % 2)
            nkt = 4 * j + 4
            for kt in range(nkt):
                qoff = 0 if kt < 4 * j else 128 * (kt - 4 * j)
                ncol = 512 - qoff
                sbk = unit % 3
                unit += 1
                S.op("pe", self.mm_group(PB[sbk][:, 0:ncol], [(KT[b][0:da, kt * 128:(kt + 1) * 128], QT[b][0:da, j * 512 + qoff:(j + 1) * 512])]),
                     [f"b_KT{b}", f"b_QT{b}"], [f"pb{sbk}"])
                p = pT[sbk]
                self.act(p[:, 0:ncol], PB[sbk][:, 0:ncol], AF.Exp, [f"pb{sbk}", ("kb", kt, fox)], [f"b_pT{sbk}"],
                         bias=kb_all[:, kt, hh:hh + 1], scale=1.0)
                if kt >= 4 * j:
                    self.tt("pool", p[:, 0:128], p[:, 0:128], mkb[:, fox, :], ALU.mult, [f"b_pT{sbk}", "b_mkb"], [f"b_pT{sbk}"])
                st, sp_ = (kt == 0), (kt == nkt - 1)

                def fpv(b=b, kt=kt, qoff=qoff, p=p, ncol=ncol, st=st, sp_=sp_, ob=ob):
                    nc.tensor.matmul(PB[ob][0:64, qoff:512], VP[b][:, kt, :], p[:, 0:ncol], start=st, stop=sp_)
                    return nc.tensor.matmul(PB[ob + 1][0:64, qoff:512], W[b][:, kt, :], p[:, 0:ncol], start=st, stop=sp_)
                S.op("pe", fpv, [f"b_pT{sbk}", f"b_VP{b}", f"b_W{b}"], [f"pb{ob}", f"pb{ob + 1}"])
            r = j % 2
            S.op("dve", lambda r=r, ob=ob: nc.vector.reciprocal(rl[r][:], PB[ob + 1][0:64, :]), [f"pb{ob + 1}"], [f"b_rl{r}"])
            self.tt("dve", yo[r][:], PB[ob][0:64, :], rl[r][:], ALU.mult, [f"pb{ob}", f"b_rl{r}"], [f"b_yo{r}"])
            ck, hf = hh // 2, hh % 2
            S.dma("sp", d["mixTd"][hf * 64:(hf + 1) * 64, ck, j * 512:(j + 1) * 512], yo[r][:], key=f"b_yo{r}",
                  reads=[f"b_yo{r}"], writes=[("mixTd", j, hh)])
    self.end_phase()


def _phase_odd_c(self, layer):
    S, nc, d = self.S, self.nc, self.d
    i = self.PI(layer)
    self.begin_phase()
    sb = self.sb
    PB = self.pb
    self.alloc_finish()
    self.load_ln_params("ln_mix_g", "ln_mix_b", layer, router_layer=layer)
    w_out = sb("c_wout", [128, KC, D], BF16)
    for k in range(KC):
        S.dma("pool", w_out[:, k, :], d["od_w_out"][i, k * 128:(k + 1) * 128, :], key="c_wout", writes=["c_wout"])
    mix = [sb(f"c_mix{b}", [128, KC, 512], BF16) for b in range(2)]
    z = sb("c_z", [128, D], F32)
    xsrc = d["x"] if self.single is not None else d["xres"]
    for tb in range(NT // 4):
        mb = mix[tb % 2]
        mn = f"c_mix{tb % 2}"
        S.dma("sp", mb[:], d["mixTd"][:, :, tb * 512:(tb + 1) * 512], key=mn, reads=[("mixTd", tb, hh) for hh in range(16)], writes=[mn])
        for tt in range(4):
            t = tb * 4 + tt
            tok = slice(tt * 128, (tt + 1) * 128)
            S.dma("sp", z[:], xsrc[t * 128:(t + 1) * 128, :], key="c_z", reads=[("res", t)], writes=["c_z"])
            for nb in range(2):
                bank = 5 + nb
                S.op("pe", self.mm_group(PB[bank][:], [(mb[:, k, tok], w_out[:, k, nb * 512:(nb + 1) * 512]) for k in range(KC)]),
                     reads=[mn, "c_wout"], writes=[f"pb{bank}"])
                self.stt("dve", z[:, nb * 512:(nb + 1) * 512], z[:, nb * 512:(nb + 1) * 512], DN_ALPHA, PB[bank][:],
                         ALU.mult, ALU.add, ["c_z", f"pb{bank}"], ["c_z"])
            self.finish_tile(z, "c_z", t, d["xres"], True, [7, 0, 1, 2])
    self.end_phase()


Prog.phase_odd = _phase_odd
Prog.phase_odd_a = _phase_odd_a
Prog.phase_odd_b = _phase_odd_b
Prog.phase_odd_c = _phase_odd_c
```

```python
import numpy as np
import concourse.bass as bass
import concourse.mybir as mybir
from concourse.bass_utils import run_bass_kernel_spmd
from contextlib import ExitStack

F32 = mybir.dt.float32
BF16 = mybir.dt.bfloat16
ALU = mybir.AluOpType
AF = mybir.ActivationFunctionType
AX = mybir.AxisListType


class _Op:
    __slots__ = ("eng", "fn", "deps", "signal", "sigval", "dma_key", "dma_val")

    def __init__(self, eng, fn):
        self.eng = eng
        self.fn = fn
        self.deps = []
        self.signal = False
        self.sigval = 0
        self.dma_key = None
        self.dma_val = 0


class Sched:
    ENGS = ("pe", "dve", "act", "pool", "sp")

    def __init__(self, nc, es):
        self.nc = nc
        self.es = es
        self.h = {"pe": nc.tensor, "dve": nc.vector, "act": nc.scalar,
                  "pool": nc.gpsimd, "sp": nc.sync}
        self.sem = {e: es.enter_context(nc.semaphore("s_" + e)) for e in self.ENGS}
        self.dsem = {}
        self.reset()

    def sbuf(self, name, shape, dt, es=None):
        return (es or self.es).enter_context(self.nc.sbuf_tensor(name, list(shape), dt))

    def psum(self, name, shape, dt=F32, es=None):
        return (es or self.es).enter_context(self.nc.psum_tensor(name, list(shape), dt))

    def reset(self):
        self.ops = {e: [] for e in self.ENGS}
        self.buf = {}
        self.dcount = {}

    def _track(self, op, reads, writes):
        deps = []
        for b in reads:
            st = self.buf.get(b)
            if st is None:
                st = self.buf[b] = [None, []]
            if st[0] is not None:
                deps.append(st[0])
        for b in writes:
            st = self.buf.get(b)
            if st is None:
                st = self.buf[b] = [None, []]
            if st[0] is not None:
                deps.append(st[0])
            deps.extend(st[1])
        for b in reads:
            self.buf[b][1].append(op)
        for b in writes:
            self.buf[b][0] = op
            self.buf[b][1] = []
        seen = set()
        for d in deps:
            if d is op or id(d) in seen:
                continue
            seen.add(id(d))
            if d.dma_key is None and d.eng == "pe" and op.eng == "pe" and op.dma_key is None:
                continue
            op.deps.append(d)
            if d.dma_key is None:
                d.signal = True

    def op(self, eng, fn, reads=(), writes=()):
        o = _Op(eng, fn)
        self._track(o, reads, writes)
        self.ops[eng].append(o)
        return o

    def dma(self, eng, out, in_, key, reads=(), writes=(), **kw):
        h = self.h[eng]
        o = _Op(eng, lambda: h.dma_start(out=out, in_=in_, **kw))
        o.dma_key = key
        if key not in self.dsem:
            self.dsem[key] = self.es.enter_context(self.nc.semaphore("d_" + str(key)))
        self.dcount[key] = self.dcount.get(key, 0) + 1
        o.dma_val = 16 * self.dcount[key]
        self._track(o, reads, writes)
        self.ops[eng].append(o)
        return o

    def flush(self, final=False):
        nc = self.nc
        for e in self.ENGS:
            c = 0
            for o in self.ops[e]:
                if o.dma_key is None and o.signal:
                    c += 1
                    o.sigval = c
        used_keys = {e: {} for e in self.ENGS}

        def emit(e, h):
            waited = {}
            for o in self.ops[e]:
                need = {}
                for d in o.deps:
                    if d.dma_key is not None:
                        k = ("d", d.dma_key)
                        v = d.dma_val
                    else:
                        k = ("e", d.eng)
                        v = d.sigval
                    if v > need.get(k, 0):
                        need[k] = v
                for k, v in need.items():
                    if waited.get(k, 0) >= v:
                        continue
                    waited[k] = v
                    s = self.dsem[k[1]] if k[0] == "d" else self.sem[k[1]]
                    h.wait_ge(s, v)
                ins = o.fn()
                if o.dma_key is not None:
                    ins.then_inc(self.dsem[o.dma_key], 16)
                    used_keys[e][o.dma_key] = o.dma_val
                elif o.signal:
                    ins.then_inc(self.sem[o.eng], 1)
            for k, v in used_keys[e].items():
                if waited.get(("d", k), 0) < v:
                    h.wait_ge(self.dsem[k], v)

        with nc.Block() as block:
            @block.tensor
            def _(h):
                emit("pe", h)

            @block.vector
            def _(h):
                emit("dve", h)

            @block.scalar
            def _(h):
                emit("act", h)

            @block.gpsimd
            def _(h):
                emit("pool", h)

            @block.sync
            def _(h):
                emit("sp", h)
        if not final:
            nc.all_engine_barrier()
            for e in self.ENGS:
                self.h[e].sem_clear(self.sem[e])
            ks = list(self.dcount.keys())
            for i, k in enumerate(ks):
                self.h["sp"].sem_clear(self.dsem[k])
            nc.all_engine_barrier()
        self.reset()


import os
S_TOK = int(os.environ.get('K_STOK', '4096'))
NT = S_TOK // 128
D = 1024
KC = 8
DEPTH = 4
NE = 32
DN_ALPHA = float((2 * DEPTH) ** 0.25)
EPS = 1e-5
GELU_C = 1.5957691216057308


class Prog:
    def __init__(self, stop_after=None, single=None):
        self.stop_after = stop_after
        self.single = single
        nc = self.nc = bass.Bass("TRN2", target_bir_lowering=False)
        self.es = ExitStack()
        self.S = Sched(nc, self.es)
        self.d = {}

    def din(self, name, shape, dt=F32):
        t = self.nc.dram_tensor(name, list(shape), dt, kind="ExternalInput")
        self.d[name] = t.ap()
        return self.d[name]

    def dscratch(self, name, shape, dt):
        t = self.nc.dram_tensor(name, list(shape), dt, kind="Internal")
        self.d[name] = t.ap()
        return self.d[name]

    def PI(self, layer):
        return 0 if self.single is not None else layer // 2

    def LI(self, layer):
        return 0 if self.single is not None else layer

    def declare(self):
        d0 = self.din
        one = self.single is not None

        def d(name, shape, dt=F32):
            if one and not name.startswith("c_") and name != "x":
                shape = [1] + list(shape[1:])
            return d0(name, shape, dt)
        d("x", [S_TOK, D])
        if self.single in (None, 0):
            d("ev_w_in", [2, D, 2560]); d("ev_conv_w", [2, 3, 512]); d("ev_sgu_ln_g", [2, 512]); d("ev_sgu_ln_b", [2, 512])
            d("ev_sgu_w", [2, 8, 128, 128]); d("ev_sgu_b", [2, 8, 128]); d("ev_w_out", [2, D, D])
        if self.single in (None, 1):
            d("od_w_in", [2, D, 1960]); d("od_q_norm_g", [2, 256]); d("od_kv_norm_g", [2, 128])
            d("od_w_uq", [2, 256, 768]); d("od_w_ukv", [2, 128, 1024]); d("od_f_bias", [2, 8]); d("od_w_out", [2, D, D])
        d("ln_mix_g", [4, D]); d("ln_mix_b", [4, D]); d("ln_ffn_g", [4, D]); d("ln_ffn_b", [4, D])
        d("moe_w_router", [4, D, NE]); d("moe_b_router", [4, NE])
        d("moe_w1p", [4, NE, 8, D, 256]); d("moe_b1p", [4, NE * 16, 128])
        d("moe_w2", [4, NE, D, D]); d("moe_b2", [4, NE, D])
        d("c_ident", [128, 128])
        if self.single in (None, 1):
            d("c_tri", [128, 128]); d("c_maskc", [128, 128]); d("c_maskf", [128, 128])
            d("c_cos", [S_TOK, 16]); d("c_sin", [S_TOK, 16])
        self.out = self.nc.dram_tensor("out", [S_TOK, D], F32, kind="ExternalOutput").ap()
        self.dscratch("xres", [S_TOK, D], F32)
        self.dscratch("xTd", [128, KC, S_TOK], BF16)
        self.dscratch("ffn", [S_TOK, D], F32)

    def mm_group(self, out, pairs):
        nc = self.nc
        n = len(pairs)

        def f():
            ins = None
            for i, (l, r) in enumerate(pairs):
                ins = nc.tensor.matmul(out, l, r, start=(i == 0), stop=(i == n - 1))
            return ins
        return f

    def V(self, eng):
        return {"dve": self.nc.vector, "pool": self.nc.gpsimd}[eng]

    def tt(self, eng, out, a, b, op, reads, writes):
        h = self.V(eng)
        self.S.op(eng, lambda: h.tensor_tensor(out, a, b, op), reads, writes)

    def ts(self, eng, out, a, s1, s2, op0, op1, reads, writes):
        h = self.V(eng)
        if op1 is None:
            self.S.op(eng, lambda: h.tensor_scalar(out, a, s1, None, op0), reads, writes)
        else:
            self.S.op(eng, lambda: h.tensor_scalar(out, a, s1, s2, op0, op1), reads, writes)

    def stt(self, eng, out, a, sc, b, op0, op1, reads, writes):
        h = self.V(eng)
        self.S.op(eng, lambda: h.scalar_tensor_tensor(out, a, sc, b, op0, op1), reads, writes)

    def act(self, out, in_, func, reads, writes, bias=None, scale=None):
        nc = self.nc
        kw = {}
        if bias is not None:
            kw["bias"] = bias
        if scale is not None:
            kw["scale"] = scale
        self.S.op("act", lambda: nc.scalar.activation(out, in_, func, **kw), reads, writes)

    def rsum(self, eng, out, in_, reads, writes):
        h = self.V(eng)
        self.S.op(eng, lambda: h.reduce_sum(out, in_, AX.X), reads, writes)

    def begin_phase(self):
        self.pes = ExitStack()
        S = self.S
        self.phase_id = getattr(self, "phase_id", 0) + 1
        self.pb = [S.psum(f"pb{i}_p{self.phase_id}", [128, 512], F32, es=self.pes) for i in range(8)]
        self._uid = 0

    def end_phase(self, final=False):
        self.S.flush(final=final)
        self.pes.close()

    def sb(self, name, shape, dt):
        return self.S.sbuf(f"{name}_p{self.phase_id}", shape, dt, es=self.pes)

    def bfv(self, i):
        return self.pb[i][:].bitcast(BF16)

    def load_consts(self):
        S, nc, d = self.S, self.nc, self.d
        self.ident_b = S.sbuf("ident_b", [128, 128], BF16)
        self.ident_f = S.sbuf("ident_f", [128, 128], F32)
        self.gates = S.sbuf("gates", [128, NT, NE], F32)
        self.gT = S.sbuf("gT", [NE, S_TOK], BF16)
        S.dma("pool", self.ident_b[:], d["c_ident"], key="cid_b", writes=["ident_b"])
        S.dma("sp", self.ident_f[:], d["c_ident"], key="cid_f", writes=["ident_f"])

    def emit_xT(self, xb, xbname, stage, stname, j, bank):
        S, nc = self.S, self.nc
        pv = self.bfv(bank)
        pn = f"pb{bank}"

        def f():
            ins = None
            for k in range(KC):
                ins = nc.tensor.transpose(pv[:, k * 128:(k + 1) * 128], xb[:, k * 128:(k + 1) * 128], self.ident_b[:])
            return ins
        S.op("pe", f, reads=[xbname, "ident_b"], writes=[pn])
        pv3 = pv.rearrange("p (k t) -> p k t", k=KC)
        S.op("dve", lambda: nc.vector.tensor_copy(stage[:, :, j * 128:(j + 1) * 128], pv3), reads=[pn], writes=[stname + str(j)])

    def alloc_finish(self):
        sb = self.sb
        self.f_sq = sb("f_sq", [128, D], F32)
        self.f_xn = sb("f_xn", [128, D], F32)
        self.f_xb = sb("f_xb", [128, D], BF16)
        self.f_st = sb("f_st", [128, 16], F32)
        self.f_g = sb("f_g", [128, D], F32)
        self.f_b = sb("f_b", [128, D], F32)
        self.f_stage = sb("f_stage", [128, KC, 512], BF16)
        self.r_x32 = sb("r_x32", [128, KC, 128], F32)
        self.r_w = sb("r_w", [128, KC, NE], F32)
        self.r_bb = sb("r_bb", [128, NE], F32)
        self.r_lg = sb("r_lg", [128, NE], F32)
        self.r_t8 = sb("r_t8", [128, 8], F32)
        self.r_ex = sb("r_ex", [128, NE], F32)
        self.r_sm = sb("r_sm", [128, 4], F32)
        self.r_gb = sb("r_gb", [128, NE], BF16)

    def load_ln_params(self, gname, bname, layer, router_layer=None):
        S, d = self.S, self.d
        layer = self.LI(layer)
        if router_layer is not None:
            router_layer = layer
        S.dma("sp", self.f_g[:], d[gname][layer:layer + 1, :].to_broadcast([128, D]), key="f_g", writes=["f_g"])
        S.dma("sp", self.f_b[:], d[bname][layer:layer + 1, :].to_broadcast([128, D]), key="f_b", writes=["f_b"])
        if router_layer is not None:
            S.dma("sp", self.r_w[:], d["moe_w_router"][router_layer].rearrange("(k p) e -> p k e", p=128), key="r_w", writes=["r_w"])
            S.dma("sp", self.r_bb[:], d["moe_b_router"][router_layer:router_layer + 1, :].to_broadcast([128, NE]), key="r_bb", writes=["r_bb"])

    def finish_tile(self, z, zname, t, dst, router, banks):
        S, nc = self.S, self.nc
        st = self.f_st
        j = t % 4
        self.rsum("dve", st[:, 0:1], z[:], [zname], ["f_st0"])
        self.ts("dve", st[:, 1:2], st[:, 0:1], -1.0 / D, None, ALU.mult, None, ["f_st0"], ["f_st1"])
        self.act(self.f_sq[:], z[:], AF.Square, [zname, "f_st1"], ["f_sq"], bias=st[:, 1:2], scale=1.0)
        self.rsum("dve", st[:, 2:3], self.f_sq[:], ["f_sq"], ["f_st2"])
        self.ts("dve", st[:, 3:4], st[:, 2:3], 1.0 / D, EPS, ALU.mult, ALU.add, ["f_st2"], ["f_st3"])
        self.act(st[:, 4:5], st[:, 3:4], AF.Sqrt, ["f_st3"], ["f_st4"])
        S.op("dve", lambda: nc.vector.reciprocal(st[:, 4:5], st[:, 4:5]), ["f_st4"], ["f_st4"])
        self.ts("dve", self.f_xn[:], z[:], st[:, 1:2], st[:, 4:5], ALU.add, ALU.mult, [zname, "f_st1", "f_st4"], ["f_xn"])
        self.tt("dve", self.f_xn[:], self.f_xn[:], self.f_g[:], ALU.mult, ["f_xn", "f_g"], ["f_xn"])
        self.tt("dve", z[:], self.f_xn[:], self.f_b[:], ALU.add, ["f_xn", "f_b"], [zname])
        S.dma("sp", dst[t * 128:(t + 1) * 128, :], z[:], key="f_out_" + zname, reads=[zname], writes=[("res", t)])
        self.S.op("act", lambda: nc.scalar.copy(self.f_xb[:], z[:]), [zname], ["f_xb"])
        self.emit_xT(self.f_xb, "f_xb", self.f_stage, "f_stage", j, banks[0])
        if j == 3:
            blk = t // 4
            S.dma("sp", self.d["xTd"][:, :, blk * 512:(blk + 1) * 512], self.f_stage[:], key="f_xTd",
                  reads=["f_stage0", "f_stage1", "f_stage2", "f_stage3"], writes=[("xTd", blk)])
        if not router:
            return
        b1, b2, b3 = banks[1], banks[2], banks[3]

        def ftr():
            ins = None
            for k in range(KC):
                pbk = self.pb[b1] if k < 4 else self.pb[b2]
                ins = nc.tensor.transpose(pbk[:, (k % 4) * 128:(k % 4 + 1) * 128], z[:, k * 128:(k + 1) * 128], self.ident_f[:])
            return ins
        S.op("pe", ftr, reads=[zname, "ident_f"], writes=[f"pb{b1}", f"pb{b2}"])
        x32 = self.r_x32
        S.op("act", lambda: nc.scalar.copy(x32[:, 0:4, :], self.pb[b1][:].rearrange("p (k t) -> p k t", k=4)), [f"pb{b1}"], ["r_x32a"])
        S.op("act", lambda: nc.scalar.copy(x32[:, 4:8, :], self.pb[b2][:].rearrange("p (k t) -> p k t", k=4)), [f"pb{b2}"], ["r_x32b"])
        lgp = self.pb[b3][:, 0:NE]
        S.op("pe", self.mm_group(lgp, [(x32[:, k, :], self.r_w[:, k, :]) for k in range(KC)]),
             reads=["r_x32a", "r_x32b", "r_w"], writes=[f"pb{b3}"])
        lg, t8, ex, sm = self.r_lg, self.r_t8, self.r_ex, self.r_sm
        self.tt("dve", lg[:], lgp, self.r_bb[:], ALU.add, [f"pb{b3}", "r_bb"], ["r_lg"])
        S.op("dve", lambda: nc.vector.max(out=t8[:], in_=lg[:]), ["r_lg"], ["r_t8"])
        self.ts("dve", sm[:, 0:1], t8[:, 0:1], -1.0, None, ALU.mult, None, ["r_t8"], ["r_sm0"])
        self.act(ex[:], lg[:], AF.Exp, ["r_lg", "r_sm0"], ["r_ex"], bias=sm[:, 0:1], scale=1.0)
        self.stt("dve", ex[:], lg[:], t8[:, 3:4], ex[:], ALU.is_ge, ALU.mult, ["r_lg", "r_t8", "r_ex"], ["r_ex"])
        self.rsum("dve", sm[:, 1:2], ex[:], ["r_ex"], ["r_sm1"])
        S.op("dve", lambda: nc.vector.reciprocal(sm[:, 2:3], sm[:, 1:2]), ["r_sm1"], ["r_sm2"])
        self.ts("dve", self.gates[:, t, :], ex[:], sm[:, 2:3], None, ALU.mult, None, ["r_ex", "r_sm2"], [("gates", t)])
        S.op("act", lambda: nc.scalar.copy(self.r_gb[:], self.gates[:, t, :]), [("gates", t)], ["r_gb"])
        gtp = self.bfv(b3)[0:NE, 512:640]
        S.op("pe", lambda: nc.tensor.transpose(gtp, self.r_gb[:], self.ident_b[:]), ["r_gb", "ident_b"], [f"pb{b3}"])
        S.op("act", lambda: nc.scalar.copy(self.gT[:, t * 128:(t + 1) * 128], gtp), [f"pb{b3}"], [("gT", t)])

    def phase_init(self):
        S, nc, d = self.S, self.nc, self.d
        self.begin_phase()
        self.load_consts()
        xin = [self.sb(f"i_x{i}", [128, D], F32) for i in range(2)]
        xb = [self.sb(f"i_xb{i}", [128, D], BF16) for i in range(2)]
        stage = [self.sb(f"i_st{i}", [128, KC, 512], BF16) for i in range(2)]
        for t in range(NT):
            b = t % 2
            S.dma("sp", xin[b][:], d["x"][t * 128:(t + 1) * 128, :], key=f"i_x{b}", writes=[f"i_x{b}"])
            S.op("act", lambda b=b: nc.scalar.copy(xb[b][:], xin[b][:]), [f"i_x{b}"], [f"i_xb{b}"])
            sg = (t // 4) % 2
            self.emit_xT(xb[b], f"i_xb{b}", stage[sg], f"i_st{sg}_", t % 4, t % 2)
            if t % 4 == 3:
                blk = t // 4
                S.dma("sp", d["xTd"][:, :, blk * 512:(blk + 1) * 512], stage[sg][:], key=f"i_xTd{sg}",
                      reads=[f"i_st{sg}_{j}" for j in range(4)], writes=[("xTd", blk)])
        self.end_phase()

    def load_T(self, dst, dstname, src, R, tmp, tmpname, bank):
        S, nc = self.S, self.nc
        S.dma("sp", tmp[0:R, :], src, key=tmpname, writes=[tmpname])
        pv = self.pb[bank][:, 0:R]
        S.op("pe", lambda: nc.tensor.transpose(pv, tmp[0:R, :], self.ident_f[0:R, 0:R]), [tmpname, "ident_f"], [f"pb{bank}"])
        S.op("dve", lambda: nc.vector.tensor_copy(dst, pv), [f"pb{bank}"], [dstname])

    def phase_even(self, layer):
        S, nc, d = self.S, self.nc, self.d
        i = self.PI(layer)
        self.begin_phase()
        sb = self.sb
        self.alloc_finish()
        self.load_ln_params("ln_mix_g", "ln_mix_b", layer, router_layer=layer)
        w_in = sb("e_win", [128, KC, 2560], BF16)
        w_out = sb("e_wout", [128, KC, D], BF16)
        for k in range(KC):
            S.dma("pool", w_in[:, k, :], d["ev_w_in"][i, k * 128:(k + 1) * 128, :], key="e_win", writes=["e_win"])
        for k in range(KC):
            S.dma("pool", w_out[:, k, :], d["ev_w_out"][i, k * 128:(k + 1) * 128, :], key="e_wout", writes=["e_wout"])
        tmpT = sb("e_tmpT", [128, 128], F32)
        cw = sb("e_cw", [128, 12], F32)
        self.load_T(cw[:, :], "e_cw", d["ev_conv_w"][i].rearrange("k (c p) -> (k c) p", p=128), 12, tmpT, "e_tmpT", 0)
        lg = sb("e_lg", [128, 512], F32)
        lb = sb("e_lb", [128, 512], F32)
        S.dma("sp", lg[:], d["ev_sgu_ln_g"][i:i + 1, :].to_broadcast([128, 512]), key="e_lg", writes=["e_lg"])
        S.dma("sp", lb[:], d["ev_sgu_ln_b"][i:i + 1, :].to_broadcast([128, 512]), key="e_lb", writes=["e_lb"])
        wraw = sb("e_wraw", [128, 8, 128], F32)
        wrb = sb("e_wrb", [128, 8, 128], BF16)
        wsT = sb("e_wsT", [128, 8, 128], BF16)
        S.dma("sp", wraw[:], d["ev_sgu_w"][i].rearrange("g i j -> i g j"), key="e_wraw", writes=["e_wraw"])
        S.op("pool", lambda: nc.gpsimd.memset(wraw[0:64, :, 64:128], 0.0), ["e_wraw"], ["e_wraw"])
        S.op("dve", lambda: nc.vector.tensor_copy(wrb[:], wraw[:]), ["e_wraw"], ["e_wrb"])
        pv = self.bfv(1)

        def ftr():
            ins = None
            for g in range(8):
                ins = nc.tensor.transpose(pv[:, g * 128:(g + 1) * 128], wrb[:, g, :], self.ident_b[:])
            return ins
        S.op("pe", ftr, ["e_wrb", "ident_b"], ["pb1"])
        S.op("dve", lambda: nc.vector.tensor_copy(wsT[:], pv.rearrange("p (g i) -> p g i", g=8)), ["pb1"], ["e_wsT"])
        sgub = sb("e_sgub", [128, 4, 4, 128], F32)
        for g in range(8):
            k, hh = g // 2, g % 2
            for tt in range(4):
                S.dma("sp", sgub[hh * 64:(hh + 1) * 64, k, tt, :], d["ev_sgu_b"][i, g:g + 1, :].to_broadcast([64, 128]),
                      key="e_sgub", writes=["e_sgub"])
        xblk = [sb(f"e_xblk{b}", [128, KC, 512], BF16) for b in range(2)]
        hbuf = sb("e_h", [128, 4, 514], F32)
        S.op("pool", lambda: nc.gpsimd.memset(hbuf[:, :, 0:2], 0.0), [], ["e_h0", "e_h1", "e_h2", "e_h3"])
        mix = sb("e_mix", [128, KC, 512], BF16)
        ug = sb("e_ug", [128, 4, 512], F32)
        t1 = sb("e_t1", [128, 512], F32)
        t2 = sb("e_t2", [128, 512], F32)
        t3 = sb("e_t3", [128, 512], F32)
        t4 = sb("e_t4", [128, 512], F32)
        vz = sb("e_vz", [128, 512], F32)
        vc = sb("e_vc", [128, 512], F32)
        vs = sb("e_vs", [128, 32], F32)
        vnb = sb("e_vnb", [128, 4, 512], BF16)
        z = sb("e_z", [128, D], F32)
        xsrc = d["x"] if (layer == 0 or self.single is not None) else d["xres"]
        PB = self.pb

        def gelu(src_ps, srcname, dst, dstname):
            self.act(dst, src_ps, AF.Gelu_apprx_tanh, [srcname], [dstname])

        for tb in range(NT // 4):
            xb = xblk[tb % 2]
            xbn = f"e_xblk{tb % 2}"
            S.dma("sp", xb[:], d["xTd"][:, :, tb * 512:(tb + 1) * 512], key=xbn, reads=[("xTd", tb)], writes=[xbn])

            def proj_fm(bank, col0):
                S.op("pe", self.mm_group(PB[bank][:], [(w_in[:, k, col0:col0 + 128], xb[:, k, :]) for k in range(KC)]),
                     reads=["e_win", xbn], writes=[f"pb{bank}"])
            for c in range(4):
                proj_fm(0, c * 128)
                proj_fm(1, 1024 + c * 128)
                proj_fm(2, 512 + c * 128)
                S.op("act", lambda: nc.scalar.copy(t2[:], PB[0][:]), ["pb0"], ["e_t2"])
                hn = f"e_h{c}"
                self.tt("dve", hbuf[:, c, 2:514], t2[:], PB[1][:], ALU.mult, ["e_t2", "pb1"], [hn])
                self.ts("dve", t3[:], hbuf[:, c, 2:514], cw[:, 8 + c:9 + c], None, ALU.mult, None, [hn, "e_cw"], ["e_t3"])
                self.stt("dve", t3[:], hbuf[:, c, 1:513], cw[:, 4 + c:5 + c], t3[:], ALU.mult, ALU.add, [hn, "e_cw", "e_t3"], ["e_t3"])
                self.stt("dve", t3[:], hbuf[:, c, 0:512], cw[:, c:c + 1], t3[:], ALU.mult, ALU.add, [hn, "e_cw", "e_t3"], ["e_t3"])
                self.tt("dve", mix[:, c, :], t3[:], PB[2][:], ALU.mult, ["e_t3", "pb2"], ["e_mix"])
                S.op("pool", lambda c=c: nc.gpsimd.tensor_copy(hbuf[:, c, 0:2], hbuf[:, c, 512:514]), [hn], [hn])
            for c in range(4):
                proj_fm(3, 1536 + c * 128)
                gelu(PB[3][:], "pb3", ug[:, c, :], f"e_ug{c}")
            for tt in range(4):
                tok = slice(tt * 128, (tt + 1) * 128)
                S.op("pe", self.mm_group(PB[4][:], [(xb[:, k, tok], w_in[:, k, 2048:2560]) for k in range(KC)]),
                     reads=["e_win", xbn], writes=["pb4"])
                gelu(PB[4][:], "pb4", vz[:], "e_vz")
                vz3 = vz[:].rearrange("p (g c) -> p g c", g=8)
                vc3 = vc[:].rearrange("p (g c) -> p g c", g=8)
                self.rsum("dve", vs[:, 0:8], vz3, ["e_vz"], ["e_vs0"])
                self.ts("dve", vs[:, 8:16], vs[:, 0:8], 1.0 / 64, None, ALU.mult, None, ["e_vs0"], ["e_vs1"])
                self.tt("dve", vc3, vz3, vs[:, 8:16].unsqueeze(2).to_broadcast([128, 8, 64]), ALU.subtract, ["e_vz", "e_vs1"], ["e_vc"])
                self.act(vz[:], vc[:], AF.Square, ["e_vc"], ["e_vz"])
                self.rsum("dve", vs[:, 16:24], vz3, ["e_vz"], ["e_vs2"])
                self.ts("dve", vs[:, 16:24], vs[:, 16:24], 1.0 / 64, EPS, ALU.mult, ALU.add, ["e_vs2"], ["e_vs2"])
                self.act(vs[:, 24:32], vs[:, 16:24], AF.Sqrt, ["e_vs2"], ["e_vs3"])
                S.op("dve", lambda: nc.vector.reciprocal(vs[:, 24:32], vs[:, 24:32]), ["e_vs3"], ["e_vs3"])
                self.tt("dve", vc3, vc3, vs[:, 24:32].unsqueeze(2).to_broadcast([128, 8, 64]), ALU.mult, ["e_vc", "e_vs3"], ["e_vc"])
                self.tt("dve", vc[:], vc[:], lg[:], ALU.mult, ["e_vc", "e_lg"], ["e_vc"])
                self.tt("dve", vnb[:, tt, :], vc[:], lb[:], ALU.add, ["e_vc", "e_lb"], [f"e_vnb{tt}"])
            for k in range(4):
                def fsg(k=k):
                    ins = None
                    for tt in range(4):
                        nc.tensor.matmul(PB[5][:, tt * 128:(tt + 1) * 128], vnb[:, tt, k * 128:(k + 1) * 128], wsT[:, 2 * k, :], start=True, stop=True)
                        ins = nc.tensor.matmul(PB[6][:, tt * 128:(tt + 1) * 128], vnb[:, tt, k * 128:(k + 1) * 128], wsT[:, 2 * k + 1, :], start=True, stop=True)
                    return ins
                S.op("pe", fsg, reads=[f"e_vnb{tt}" for tt in range(4)] + ["e_wsT"], writes=["pb5", "pb6"])
                sg = sgub[:, k, :, :].rearrange("p t i -> p (t i)")
                self.tt("dve", t4[0:64, :], PB[5][0:64, :], sg[0:64, :], ALU.add, ["pb5", "e_sgub"], ["e_t4a"])
                self.tt("dve", t4[64:128, :], PB[6][64:128, :], sg[64:128, :], ALU.add, ["pb6", "e_sgub"], ["e_t4b"])
                self.tt("dve", mix[:, 4 + k, :], t4[:], ug[:, k, :], ALU.mult, ["e_t4a", "e_t4b", f"e_ug{k}"], ["e_mix"])
            for tt in range(4):
                t = tb * 4 + tt
                tok = slice(tt * 128, (tt + 1) * 128)
                S.dma("sp", z[:], xsrc[t * 128:(t + 1) * 128, :], key="e_z", reads=[("res", t)], writes=["e_z"])
                for nb in range(2):
                    bank = 5 + nb
                    S.op("pe", self.mm_group(PB[bank][:], [(mix[:, k, tok], w_out[:, k, nb * 512:(nb + 1) * 512]) for k in range(KC)]),
                         reads=["e_mix", "e_wout"], writes=[f"pb{bank}"])
                    self.stt("dve", z[:, nb * 512:(nb + 1) * 512], z[:, nb * 512:(nb + 1) * 512], DN_ALPHA, PB[bank][:],
                             ALU.mult, ALU.add, ["e_z", f"pb{bank}"], ["e_z"])
                self.finish_tile(z, "e_z", t, d["xres"], True, [7, 0, 1, 2])
        self.end_phase()

    def phase_copy_out(self):
        S, d = self.S, self.d
        self.begin_phase()
        for q in range(S_TOK // 512):
            S.dma("sp", self.out[q * 512:(q + 1) * 512, :], d["xres"][q * 512:(q + 1) * 512, :], key="cp_out", writes=[("o", q)])
        self.end_phase(final=True)

    def build(self):
        self.declare()
        if self.single is not None:
            layer = self.single
            self.phase_init()
            if layer % 2 == 0:
                self.phase_even(layer)
            else:
                self.phase_odd(layer)
            self.phase_moe(layer, final=True)
            return self.nc
        stop = self.stop_after
        self.phase_init()
        for layer in range(DEPTH):
            if layer % 2 == 0:
                self.phase_even(layer)
            else:
                self.phase_odd(layer)
            if stop == f"mix{layer}":
                self.phase_copy_out()
                return self.nc
            self.phase_moe(layer, final=(layer == DEPTH - 1))
            if stop == f"ffn{layer}":
                if layer != DEPTH - 1:
                    self.phase_copy_out()
                return self.nc
        return self.nc


def _consts():
    c = {}
    c["c_ident"] = np.eye(128, dtype=np.float32)
    i = np.arange(128)
    c["c_tri"] = (i[:, None] <= i[None, :]).astype(np.float32)
    c["c_maskc"] = ((i[:, None] // 64) <= (i[None, :] // 64)).astype(np.float32)
    c["c_maskf"] = (i[:, None] <= i[None, :]).astype(np.float32)
    inv = (10000.0 ** (-np.arange(0, 32, 2, dtype=np.float32) / 32)).astype(np.float32)
    ang = (np.arange(S_TOK, dtype=np.float32)[:, None] * inv[None, :]).astype(np.float32)
    c["c_cos"] = np.cos(ang).astype(np.float32)
    c["c_sin"] = np.sin(ang).astype(np.float32)
    return c


def _prep_weights(inputs):
    w = {}
    for k, v in inputs.items():
        if k in ("x", "moe_w1", "moe_b1"):
            continue
        w[k] = np.ascontiguousarray(np.asarray(v, dtype=np.float32))
    w1 = np.asarray(inputs["moe_w1"], dtype=np.float32)
    L, E = w1.shape[0], w1.shape[1]
    w1v = w1.reshape(L, E, D, 8, 128, 2)
    w1p = np.ascontiguousarray(w1v.transpose(0, 1, 3, 2, 5, 4)).reshape(L, E, 8, D, 256)
    w["moe_w1p"] = w1p
    b1 = np.asarray(inputs["moe_b1"], dtype=np.float32).reshape(L, E, 8, 128, 2)
    b1p = np.ascontiguousarray(b1.transpose(0, 1, 2, 4, 3)).reshape(L, E * 16, 128)
    w["moe_b1p"] = b1p
    w.update(_consts())
    return w


_CACHE = {}


def _get_prog(stop_after=None, single=None):
    key = (stop_after, single)
    if key not in _CACHE:
        p = Prog(stop_after, single)
        p.build()
        _CACHE[key] = p
    return _CACHE[key]


_EV = ("ev_w_in", "ev_conv_w", "ev_sgu_ln_g", "ev_sgu_ln_b", "ev_sgu_w", "ev_sgu_b", "ev_w_out")
_OD = ("od_w_in", "od_q_norm_g", "od_kv_norm_g", "od_w_uq", "od_w_ukv", "od_f_bias", "od_w_out")
_LY = ("ln_mix_g", "ln_mix_b", "ln_ffn_g", "ln_ffn_b", "moe_w_router", "moe_b_router", "moe_w1p", "moe_b1p", "moe_w2", "moe_b2")
_CO = ("c_tri", "c_maskc", "c_maskf", "c_cos", "c_sin")


def run(inputs, stop_after=None, cores=None, trace=False):
    x = np.asarray(inputs["x"], dtype=np.float32)
    n = x.shape[0]
    cores = list(range(n)) if cores is None else cores
    w = _prep_weights(inputs)
    p = _get_prog(stop_after)
    in_maps = []
    for c in cores:
        m = dict(w)
        m["x"] = np.ascontiguousarray(x[c])
        in_maps.append(m)
    res = run_bass_kernel_spmd(p.nc, in_maps, core_ids=list(range(len(cores))), trace=trace)
    return np.stack([r["out"] for r in res.results], axis=0), res


def run_layers(inputs, cores=None, nlayers=DEPTH):
    x = np.asarray(inputs["x"], dtype=np.float32)
    n = x.shape[0]
    cores = list(range(n)) if cores is None else cores
    w = _prep_weights(inputs)
    cur = [np.ascontiguousarray(x[c]) for c in cores]
    for L in range(nlayers):
        par = L % 2
        p = _get_prog(None, single=par)
        base = {"c_ident": w["c_ident"]}
        fam = _EV if par == 0 else _OD
        for k in fam:
            base[k] = w[k][L // 2:L // 2 + 1]
        for k in _LY:
            base[k] = w[k][L:L + 1]
        if par == 1:
            for k in _CO:
                base[k] = w[k]
        in_maps = []
        for ci in range(len(cores)):
            m = dict(base)
            m["x"] = cur[ci]
            in_maps.append(m)
        res = run_bass_kernel_spmd(p.nc, in_maps, core_ids=list(range(len(cores))))
        cur = [np.ascontiguousarray(r["out"]) for r in res.results]
    return np.stack(cur, axis=0)


def kernel(**inputs):
    out = run_layers(inputs)
    return out.astype(np.float32)


def _phase_moe(self, layer, final):
    S, nc, d = self.S, self.nc, self.d
    layer_true = layer
    layer = self.LI(layer)
    self.begin_phase()
    sb = self.sb
    PB = self.pb
    NH = S_TOK // 2
    NTH = NH // 128
    NBH = NH // 512
    tmpT = sb("m_tmpT", [128, 128], F32)
    b1T = sb("m_b1T", [128, NE * 16], F32)
    for q in range(4):
        self.load_T(b1T[:, q * 128:(q + 1) * 128], "m_b1T", d["moe_b1p"][layer, q * 128:(q + 1) * 128, :], 128, tmpT, "m_tmpT", q % 2)
    b1T7 = sb("m_b1T7", [128, NE * 16], F32)
    self.ts("dve", b1T7[:], b1T[:], 7.0, None, ALU.add, None, ["m_b1T"], ["m_b1T7"])
    c14 = sb("m_c14", [128, 1], F32)
    S.op("dve", lambda: nc.vector.memset(c14[:], 14.0), [], ["m_c14"])
    b2 = sb("m_b2", [NE, D], BF16)
    S.dma("pool", b2[:], d["moe_b2"][layer], key="m_b2", writes=["m_b2"])
    xh = sb("m_xh", [128, KC, NH], BF16)
    acc = sb("m_acc", [128, NTH, D], F32)
    actb = sb("m_act", [128, 8, NH], BF16)
    NR = 3
    w1r = [sb(f"m_w1_{i}", [128, KC, 256], BF16) for i in range(NR)]
    w2b = sb("m_w2", [128, KC, D], BF16)
    gt = [sb(f"m_gt{i}", [128, 512], F32) for i in range(2)]
    st = [sb(f"m_st{i}", [128, 512], F32) for i in range(2)]
    lt = [sb(f"m_lt{i}", [128, 512], F32) for i in range(2)]

    def w1_dma(hf, e, fc):
        n = (hf * NE + e) * 8 + fc
        r = n % NR
        S.dma("pool", w1r[r][:], d["moe_w1p"][layer, e, fc].rearrange("(k p) c -> p k c", p=128),
              key=f"m_w1_{r}", writes=[f"m_w1_{r}"])

    for hf in range(2):
        S.dma("sp", xh[:], d["xTd"][:, :, hf * NH:(hf + 1) * NH], key="m_xh",
              reads=[("xTd", b) for b in range(hf * NBH, hf * NBH + NBH)], writes=["m_xh"])
        for tt in range(NTH):
            t = hf * NTH + tt
            for nb in range(2):
                bank = 4 + 2 * (tt % 2) + nb
                S.op("pe", self.mm_group(PB[bank][:], [(self.gT[:, t * 128:(t + 1) * 128], b2[:, nb * 512:(nb + 1) * 512])]),
                     reads=[("gT", t), "m_b2"], writes=[f"pb{bank}"])
                S.op("act", lambda tt=tt, nb=nb, bank=bank: nc.scalar.copy(acc[:, tt, nb * 512:(nb + 1) * 512], PB[bank][:]),
                     [f"pb{bank}"], [("m_acc", tt, nb)])
        w1_dma(hf, 0, 0)
        w1_dma(hf, 0, 1)
        unit = 0
        for e in range(NE):
            S.dma("pool", w2b[:], d["moe_w2"][layer, e].rearrange("(k p) n -> p k n", p=128), key="m_w2", writes=["m_w2"])
            for fc in range(8):
                nxt = e * 8 + fc + 2
                if nxt < NE * 8:
                    w1_dma(hf, nxt // 8, nxt % 8)
                r = ((hf * NE + e) * 8 + fc) % NR
                wp = w1r[r]
                wn = f"m_w1_{r}"
                cg = e * 16 + fc * 2
                for tb in range(NBH):
                    s = unit % 2
                    unit += 1
                    bg, bl = 2 * s, 2 * s + 1
                    tok = slice(tb * 512, (tb + 1) * 512)
                    S.op("pe", self.mm_group(PB[bg][:], [(wp[:, k, 0:128], xh[:, k, tok]) for k in range(KC)]),
                         reads=[wn, "m_xh"], writes=[f"pb{bg}"])
                    S.op("pe", self.mm_group(PB[bl][:], [(wp[:, k, 128:256], xh[:, k, tok]) for k in range(KC)]),
                         reads=[wn, "m_xh"], writes=[f"pb{bl}"])
                    self.ts("dve", gt[s][:], PB[bg][:], b1T[:, cg:cg + 1], 7.0, ALU.add, ALU.min, [f"pb{bg}", "m_b1T"], [f"m_gt{s}"])
                    self.act(st[s][:], gt[s][:], AF.Sigmoid, [f"m_gt{s}"], [f"m_st{s}"], scale=1.702)
                    self.act(lt[s][:], PB[bl][:], AF.Relu, [f"pb{bl}", "m_b1T7"], [f"m_lt{s}"], bias=b1T7[:, cg + 1:cg + 2], scale=1.0)
                    self.act(lt[s][:], lt[s][:], AF.Relu, [f"m_lt{s}", "m_c14"], [f"m_lt{s}"], bias=c14[:, 0:1], scale=-1.0)
                    self.stt("dve", gt[s][:], gt[s][:], -1.0, st[s][:], ALU.mult, ALU.mult, [f"m_gt{s}", f"m_st{s}"], [f"m_gt{s}"])
                    self.stt("dve", actb[:, fc, tok], lt[s][:], -8.0, gt[s][:], ALU.add, ALU.mult,
                             [f"m_lt{s}", f"m_gt{s}"], [("m_act", fc, tb)])
            for tt in range(NTH):
                t = hf * NTH + tt
                tk = slice(tt * 128, (tt + 1) * 128)
                for nb in range(2):
                    bank = 4 + 2 * (tt % 2) + nb
                    S.op("pe", self.mm_group(PB[bank][:], [(actb[:, k, tk], w2b[:, k, nb * 512:(nb + 1) * 512]) for k in range(KC)]),
                         reads=[("m_act", k, tt // 4) for k in range(KC)] + ["m_w2"], writes=[f"pb{bank}"])
                    a = acc[:, tt, nb * 512:(nb + 1) * 512]
                    self.stt("dve", a, PB[bank][:], self.gates[:, t, e:e + 1], a, ALU.mult, ALU.add,
                             [f"pb{bank}", ("gates", t), ("m_acc", tt, nb)], [("m_acc", tt, nb)])
        for tt in range(NTH):
            t = hf * NTH + tt
            S.dma("sp", d["ffn"][t * 128:(t + 1) * 128, :], acc[:, tt, :], key=f"m_ffn{tt}",
                  reads=[("m_acc", tt, 0), ("m_acc", tt, 1)], writes=[("ffn", t)])
    self.end_phase()
    self.phase_ffn_ln(layer_true, final)


def _phase_ffn_ln(self, layer, final):
    S, nc, d = self.S, self.nc, self.d
    self.begin_phase()
    self.alloc_finish()
    self.load_ln_params("ln_ffn_g", "ln_ffn_b", layer, router_layer=None)
    z = [self.sb(f"l_z{i}", [128, D], F32) for i in range(2)]
    f = [self.sb(f"l_f{i}", [128, D], F32) for i in range(2)]
    dst = self.out if final else d["xres"]
    for t in range(NT):
        b = t % 2
        S.dma("sp", z[b][:], d["xres"][t * 128:(t + 1) * 128, :], key=f"l_z{b}", reads=[("res", t)], writes=[f"l_z{b}"])
        S.dma("sp", f[b][:], d["ffn"][t * 128:(t + 1) * 128, :], key=f"l_f{b}", reads=[("ffn", t)], writes=[f"l_f{b}"])
        self.stt("dve", z[b][:], z[b][:], DN_ALPHA, f[b][:], ALU.mult, ALU.add, [f"l_z{b}", f"l_f{b}"], [f"l_z{b}"])
        self.finish_tile(z[b], f"l_z{b}", t, dst, False, [t % 2, 2, 3, 4])
    self.end_phase(final=final)


Prog.phase_moe = _phase_moe
Prog.phase_ffn_ln = _phase_ffn_ln


SC_C = float(1.0 / np.sqrt(96.0))
SC_F = 0.125


def _phase_odd(self, layer):
    self.phase_odd_a(layer)
    self.phase_odd_b(layer)
    self.phase_odd_c(layer)


def _phase_odd_a(self, layer):
    S, nc, d = self.S, self.nc, self.d
    i = self.PI(layer)
    self.begin_phase()
    sb = self.sb
    PB = self.pb
    if not hasattr(self, "kb_all"):
        self.kb_all = S.sbuf("kb_all", [128, NT, 16], F32)
        self.ef_all = S.sbuf("ef_all", [128, NT, 16], F32)
        self.dscratch("QTd", [16, 128, S_TOK], BF16)
        self.dscratch("KTd", [16, 128, S_TOK], BF16)
        self.dscratch("VPd", [S_TOK, 16, 64], BF16)
        self.dscratch("mixTd", [128, KC, S_TOK], BF16)
    kb_all, ef_all = self.kb_all, self.ef_all
    w_in = sb("a_win", [128, KC, 1960], BF16)
    for k in range(KC):
        S.dma("pool", w_in[:, k, :], d["od_w_in"][i, k * 128:(k + 1) * 128, :], key="a_win", writes=["a_win"])
    w_uq = sb("a_wuq", [128, 2, 768], BF16)
    S.dma("pool", w_uq[:], d["od_w_uq"][i].rearrange("(c p) n -> p c n", p=128), key="a_wuq", writes=["a_wuq"])
    w_ukv = sb("a_wukv", [128, 1024], BF16)
    S.dma("pool", w_ukv[:], d["od_w_ukv"][i], key="a_wukv", writes=["a_wukv"])
    gq = sb("a_gq", [128, 256], F32)
    gkv = sb("a_gkv", [128, 128], F32)
    fb = sb("a_fb", [128, 8], F32)
    S.dma("sp", gq[:], d["od_q_norm_g"][i:i + 1, :].to_broadcast([128, 256]), key="a_gq", writes=["a_gq"])
    S.dma("sp", gkv[:], d["od_kv_norm_g"][i:i + 1, :].to_broadcast([128, 128]), key="a_gkv", writes=["a_gkv"])
    S.dma("sp", fb[:], d["od_f_bias"][i:i + 1, :].to_broadcast([128, 8]), key="a_fb", writes=["a_fb"])
    cos = sb("a_cos", [128, NT, 16], F32)
    sin = sb("a_sin", [128, NT, 16], F32)
    S.dma("sp", cos[:], d["c_cos"].rearrange("(t p) f -> p t f", p=128), key="a_cos", writes=["a_cos"])
    S.dma("sp", sin[:], d["c_sin"].rearrange("(t p) f -> p t f", p=128), key="a_sin", writes=["a_sin"])
    tri = sb("a_tri", [128, 128], F32)
    ones = sb("a_ones", [128, 128], F32)
    S.dma("sp", tri[:], d["c_tri"], key="a_tri", writes=["a_tri"])
    S.op("pool", lambda: nc.gpsimd.memset(ones[:], 1.0), [], ["a_ones"])
    carry = sb("a_carry", [128, 8], F32)
    S.op("pool", lambda: nc.gpsimd.memset(carry[:], 0.0), [], ["a_carry"])
    xblk = [sb(f"a_xblk{b}", [128, KC, 512], BF16) for b in range(2)]
    sq = sb("a_sq", [128, 1024], F32)
    sm = sb("a_sm", [128, 64], F32)
    cqn = sb("a_cqn", [128, 256], F32)
    cqb = sb("a_cqb", [128, 384], BF16)
    cT = sb("a_cT", [128, 3, 128], BF16)
    qf = sb("a_qf", [128, 768], F32)
    kvf = sb("a_kvf", [128, 1024], F32)
    krf = sb("a_krf", [128, 32], F32)
    kro = sb("a_kro", [128, 32], F32)
    rt = sb("a_rt", [128, 8, 32], F32)
    krt = sb("a_krt", [128, 16], F32)
    kro2 = sb("a_kro2", [128, 32], F32)
    qaug = sb("a_qaug", [128, 8, 98], BF16)
    kaug = sb("a_kaug", [128, 8, 98], BF16)
    vp = sb("a_vp", [128, 16, 64], BF16)
    fqf = sb("a_fqf", [128, 512], F32)
    fkf = sb("a_fkf", [128, 512], F32)
    qaugf = sb("a_qaugf", [128, 8, 66], BF16)
    kaugf = sb("a_kaugf", [128, 8, 66], BF16)
    Ft = sb("a_Ft", [128, 8], F32)
    hib = sb("a_hib", [128, 16], BF16)
    hif = sb("a_hif", [128, 16], F32)
    av = sb("a_av", [128, 16], F32)
    stq = sb("a_stq", [128, 8, 128], BF16)
    stk = sb("a_stk", [128, 8, 128], BF16)
    stqf = sb("a_stqf", [128, 8, 128], BF16)
    stkf = sb("a_stkf", [128, 8, 128], BF16)
    S.op("pool", lambda: nc.gpsimd.memset(kaug[:, :, 96:98], 1.0), [], ["a_kaug_one"])
    S.op("pool", lambda: nc.gpsimd.memset(kaugf[:, :, 64:66], 1.0), [], ["a_kaugf_one"])

    def rstd(dst, src, n, name):
        self.ts("dve", dst, src, 1.0 / n, EPS, ALU.mult, ALU.add, [name + "_s"], [name])
        self.act(dst, dst, AF.Sqrt, [name], [name])
        S.op("dve", lambda: nc.vector.reciprocal(dst, dst), [name], [name])

    def hilo(a, n, dst_hi, dst_lo, aname, outname):
        S.op("dve", lambda: nc.vector.tensor_copy(hib[:, 0:n], a), [aname], ["a_hib"])
        S.op("dve", lambda: nc.vector.tensor_copy(hif[:, 0:n], hib[:, 0:n]), ["a_hib"], ["a_hif"])
        S.op("dve", lambda: nc.vector.tensor_copy(dst_hi, hib[:, 0:n]), ["a_hib"], [outname + "_hi"])
        self.tt("dve", dst_lo, a, hif[:, 0:n], ALU.subtract, [aname, "a_hif"], [outname + "_lo"])

    def transposes(src, nh, da, bank, stage, stname, srcnames):
        pv = self.bfv(bank)

        def f():
            ins = None
            for h in range(nh):
                ins = nc.tensor.transpose(pv[0:da, h * 128:(h + 1) * 128], src[:, h, :], self.ident_b[:])
            return ins
        S.op("pe", f, srcnames + ["ident_b"], [f"pb{bank}"])
        S.op("act", lambda: nc.scalar.copy(stage[0:da, :, :], pv[0:da, :].rearrange("p (h t) -> p h t", h=nh)), [f"pb{bank}"], [stname])

    for t in range(NT):
        tb, tt = t // 4, t % 4
        xb = xblk[tb % 2]
        xbn = f"a_xblk{tb % 2}"
        if tt == 0:
            S.dma("sp", xb[:], d["xTd"][:, :, tb * 512:(tb + 1) * 512], key=xbn, reads=[("xTd", tb)], writes=[xbn])
        tok = slice(tt * 128, (tt + 1) * 128)

        def fA(xb=xb, tok=tok):
            ins = None
            for k in range(KC):
                nc.tensor.matmul(PB[0][:, 0:416], xb[:, k, tok], w_in[:, k, 0:416], start=(k == 0), stop=(k == KC - 1))
            for k in range(KC):
                ins = nc.tensor.matmul(PB[0][:, 416:424], xb[:, k, tok], w_in[:, k, 1952:1960], start=(k == 0), stop=(k == KC - 1))
            return ins
        S.op("pe", fA, ["a_win", xbn], ["pb0"])
        for bi, c0 in ((1, 416), (2, 928), (3, 1440)):
            S.op("pe", self.mm_group(PB[bi][:], [(xb[:, k, tok], w_in[:, k, c0:c0 + 512]) for k in range(KC)]),
                 ["a_win", xbn], [f"pb{bi}"])
        self.act(sq[:, 0:384], PB[0][:, 0:384], AF.Square, ["pb0"], ["a_sq"])
        self.rsum("dve", sm[:, 0:1], sq[:, 0:256], ["a_sq"], ["a_sm0_s"])
        self.rsum("dve", sm[:, 1:2], sq[:, 256:384], ["a_sq"], ["a_sm1_s"])
        rstd(sm[:, 0:1], sm[:, 0:1], 256, "a_sm0")
        rstd(sm[:, 1:2], sm[:, 1:2], 128, "a_sm1")
        self.stt("dve", cqn[:, 0:256], PB[0][:, 0:256], sm[:, 0:1], gq[:], ALU.mult, ALU.mult, ["pb0", "a_sm0", "a_gq"], ["a_cqn"])
        S.op("act", lambda: nc.scalar.copy(cqb[:, 0:256], cqn[:, 0:256]), ["a_cqn"], ["a_cqb0"])
        self.stt("dve", cqb[:, 256:384], PB[0][:, 256:384], sm[:, 1:2], gkv[:], ALU.mult, ALU.mult, ["pb0", "a_sm1", "a_gkv"], ["a_cqb1"])
        S.op("act", lambda: nc.scalar.copy(krf[:], PB[0][:, 384:416]), ["pb0"], ["a_krf"])
        self.tt("dve", sm[:, 8:16], PB[0][:, 416:424], fb[:], ALU.add, ["pb0", "a_fb"], ["a_zb"])
        self.act(sm[:, 8:16], sm[:, 8:16], AF.Exp, ["a_zb"], ["a_zb"], scale=-1.0)
        self.act(sm[:, 8:16], sm[:, 8:16], AF.Ln, ["a_zb"], ["a_zb"], bias=1.0, scale=1.0)
        pv4 = self.bfv(4)

        def fT():
            ins = None
            for c in range(3):
                ins = nc.tensor.transpose(pv4[:, c * 128:(c + 1) * 128], cqb[:, c * 128:(c + 1) * 128], self.ident_b[:])
            return ins
        S.op("pe", fT, ["a_cqb0", "a_cqb1", "ident_b"], ["pb4"])
        S.op("act", lambda: nc.scalar.copy(cT[:], pv4[:, 0:384].rearrange("p (c t) -> p c t", c=3)), ["pb4"], ["a_cT"])
        S.op("pe", self.mm_group(PB[5][:], [(cT[:, c, :], w_uq[:, c, 0:512]) for c in range(2)]), ["a_cT", "a_wuq"], ["pb5"])
        S.op("pe", self.mm_group(PB[6][:, 0:256], [(cT[:, c, :], w_uq[:, c, 512:768]) for c in range(2)]), ["a_cT", "a_wuq"], ["pb6"])
        S.op("act", lambda: nc.scalar.copy(qf[:, 0:512], PB[5][:]), ["pb5"], ["a_qf0"])
        S.op("act", lambda: nc.scalar.copy(qf[:, 512:768], PB[6][:, 0:256]), ["pb6"], ["a_qf1"])
        S.op("pe", self.mm_group(PB[7][:], [(cT[:, 2, :], w_ukv[:, 0:512])]), ["a_cT", "a_wukv"], ["pb7"])
        S.op("pe", self.mm_group(PB[4][:], [(cT[:, 2, :], w_ukv[:, 512:1024])]), ["a_cT", "a_wukv"], ["pb4"])
        S.op("act", lambda: nc.scalar.copy(kvf[:, 0:512], PB[7][:]), ["pb7"], ["a_kvf0"])
        S.op("act", lambda: nc.scalar.copy(kvf[:, 512:1024], PB[4][:]), ["pb4"], ["a_kvf1"])
        q3 = qf[:].rearrange("p (h c) -> p h c", h=8)
        kv3 = kvf[:].rearrange("p (h c) -> p h c", h=8)
        QN = ["a_qf0", "a_qf1"]
        KN = ["a_kvf0", "a_kvf1"]
        sq3 = sq[:, 0:768].rearrange("p (h c) -> p h c", h=8)
        self.act(sq[:, 0:768], qf[:], AF.Square, QN, ["a_sq"])
        self.rsum("dve", av[:, 0:8], sq3, ["a_sq"], ["a_av_q"])
        self.ts("dve", av[:, 0:8], av[:, 0:8], -0.5 * SC_C, None, ALU.mult, None, ["a_av_q"], ["a_av_q"])
        hilo(av[:, 0:8], 8, qaug[:, :, 96], qaug[:, :, 97], "a_av_q", "a_qaug")
        self.ts("dve", qaug[:, :, 0:64], q3[:, :, 0:64], SC_C, None, ALU.mult, None, QN, ["a_qaug_n"])
        cosb = cos[:, t, :].unsqueeze(1).to_broadcast([128, 8, 16])
        sinb = sin[:, t, :].unsqueeze(1).to_broadcast([128, 8, 16])
        x1, x2 = q3[:, :, 64:80], q3[:, :, 80:96]
        self.tt("dve", rt[:, :, 0:16], x1, cosb, ALU.mult, QN + ["a_cos"], ["a_rt0"])
        self.tt("dve", rt[:, :, 16:32], x2, sinb, ALU.mult, QN + ["a_sin"], ["a_rt1"])
        self.tt("dve", rt[:, :, 0:16], rt[:, :, 0:16], rt[:, :, 16:32], ALU.subtract, ["a_rt0", "a_rt1"], ["a_rt0"])
        self.ts("dve", qaug[:, :, 64:80], rt[:, :, 0:16], SC_C, None, ALU.mult, None, ["a_rt0"], ["a_qaug_r1"])
        self.tt("dve", rt[:, :, 0:16], x2, cosb, ALU.mult, QN + ["a_cos"], ["a_rt0"])
        self.tt("dve", rt[:, :, 16:32], x1, sinb, ALU.mult, QN + ["a_sin"], ["a_rt1"])
        self.tt("dve", rt[:, :, 0:16], rt[:, :, 0:16], rt[:, :, 16:32], ALU.add, ["a_rt0", "a_rt1"], ["a_rt0"])
        self.ts("dve", qaug[:, :, 80:96], rt[:, :, 0:16], SC_C, None, ALU.mult, None, ["a_rt0"], ["a_qaug_r2"])
        c1, s1 = cos[:, t, :], sin[:, t, :]
        self.tt("pool", kro[:, 0:16], krf[:, 0:16], c1, ALU.mult, ["a_krf", "a_cos"], ["a_kro0"])
        self.tt("pool", krt[:], krf[:, 16:32], s1, ALU.mult, ["a_krf", "a_sin"], ["a_krt"])
        self.tt("pool", kro[:, 0:16], kro[:, 0:16], krt[:], ALU.subtract, ["a_kro0", "a_krt"], ["a_kro0"])
        self.tt("pool", kro[:, 16:32], krf[:, 16:32], c1, ALU.mult, ["a_krf", "a_cos"], ["a_kro1"])
        self.tt("pool", krt[:], krf[:, 0:16], s1, ALU.mult, ["a_krf", "a_sin", "a_krt"], ["a_krt"])
        self.tt("pool", kro[:, 16:32], kro[:, 16:32], krt[:], ALU.add, ["a_kro1", "a_krt"], ["a_kro1"])
        S.op("act", lambda: nc.scalar.copy(kaug[:, :, 0:64], kv3[:, :, 0:64]), KN, ["a_kaug_n"])
        S.op("dve", lambda: nc.vector.tensor_copy(kaug[:, :, 64:96], kro[:].unsqueeze(1).to_broadcast([128, 8, 32])), ["a_kro0", "a_kro1"], ["a_kaug_r"])
        sqk = sq[:].rearrange("p (h c) -> p h c", h=8)
        self.act(sq[:], kvf[:], AF.Square, KN + ["a_sq"], ["a_sq"])
        self.rsum("dve", av[:, 8:16], sqk[:, :, 0:64], ["a_sq"], ["a_av_k"])
        self.tt("pool", kro2[:], kro[:], kro[:], ALU.mult, ["a_kro0", "a_kro1"], ["a_kro2"])
        self.rsum("dve", sm[:, 2:3], kro2[:], ["a_kro2"], ["a_sm2"])
        self.ts("dve", av[:, 8:16], av[:, 8:16], sm[:, 2:3], 0.5 * SC_C, ALU.add, ALU.mult, ["a_av_k", "a_sm2"], ["a_av_k"])
        self.ts("dve", kb_all[:, t, 0:8], av[:, 8:16], -1.0, None, ALU.mult, None, ["a_av_k"], [("kb", t, 0)])
        self.act(ef_all[:, t, 0:8], av[:, 8:16], AF.Exp, ["a_av_k"], [("ef", t, 0)])
        self.tt("dve", vp[:, 0:8, :], kv3[:, :, 64:128], ef_all[:, t, 0:8].unsqueeze(2).to_broadcast([128, 8, 64]), ALU.mult,
                KN + [("ef", t, 0)], ["a_vp0"])
        S.op("pe", self.mm_group(PB[5][:, 0:8], [(tri[:], sm[:, 8:16])]), ["a_tri", "a_zb"], ["pb5"])
        S.op("pe", self.mm_group(PB[6][:, 0:8], [(ones[:], sm[:, 8:16])]), ["a_ones", "a_zb"], ["pb6"])
        self.tt("dve", Ft[:], PB[5][:, 0:8], carry[:], ALU.add, ["pb5", "a_carry"], ["a_Ft"])
        self.tt("dve", carry[:], carry[:], PB[6][:, 0:8], ALU.add, ["pb6", "a_carry"], ["a_carry"])
        S.op("act", lambda: nc.scalar.copy(fqf[:], PB[1][:]), ["pb1"], ["a_fqf"])
        S.op("act", lambda: nc.scalar.copy(fkf[:], PB[2][:]), ["pb2"], ["a_fkf"])
        fq3 = fqf[:].rearrange("p (h c) -> p h c", h=8)
        fk3 = fkf[:].rearrange("p (h c) -> p h c", h=8)
        sqf = sq[:, 0:512].rearrange("p (h c) -> p h c", h=8)
        self.act(sq[:, 0:512], fqf[:], AF.Square, ["a_fqf", "a_sq"], ["a_sq"])
        self.rsum("dve", av[:, 0:8], sqf, ["a_sq"], ["a_av_fq"])
        self.stt("dve", av[:, 0:8], av[:, 0:8], 0.5 * SC_F, Ft[:], ALU.mult, ALU.add, ["a_av_fq", "a_Ft"], ["a_av_fq"])
        self.ts("dve", av[:, 0:8], av[:, 0:8], -1.0, None, ALU.mult, None, ["a_av_fq"], ["a_av_fq"])
        hilo(av[:, 0:8], 8, qaugf[:, :, 64], qaugf[:, :, 65], "a_av_fq", "a_qaugf")
        self.ts("dve", qaugf[:, :, 0:64], fq3, SC_F, None, ALU.mult, None, ["a_fqf"], ["a_qaugf_n"])
        S.op("act", lambda: nc.scalar.copy(kaugf[:, :, 0:64], fk3), ["a_fkf"], ["a_kaugf_n"])
        self.act(sq[:, 0:512], fkf[:], AF.Square, ["a_fkf", "a_sq"], ["a_sq"])
        self.rsum("dve", av[:, 8:16], sqf, ["a_sq"], ["a_av_fk"])
        self.ts("dve", av[:, 8:16], av[:, 8:16], 0.5 * SC_F, None, ALU.mult, None, ["a_av_fk"], ["a_av_fk"])
        self.tt("dve", kb_all[:, t, 8:16], Ft[:], av[:, 8:16], ALU.subtract, ["a_Ft", "a_av_fk"], [("kb", t, 1)])
        self.act(ef_all[:, t, 8:16], av[:, 8:16], AF.Exp, ["a_av_fk"], [("ef", t, 1)])
        self.tt("dve", vp[:, 8:16, :], PB[3][:].rearrange("p (h c) -> p h c", h=8),
                ef_all[:, t, 8:16].unsqueeze(2).to_broadcast([128, 8, 64]), ALU.mult, ["pb3", ("ef", t, 1)], ["a_vp1"])
        S.dma("sp", d["VPd"][t * 128:(t + 1) * 128, :, :], vp[:], key="a_vpd", reads=["a_vp0", "a_vp1"], writes=[("VPd", t)])
        transposes(qaug, 8, 98, 5, stq, "a_stq", ["a_qaug_hi", "a_qaug_lo", "a_qaug_n", "a_qaug_r1", "a_qaug_r2"])
        S.dma("sp", d["QTd"][0:8, 0:98, t * 128:(t + 1) * 128].rearrange("h r t -> r h t"), stq[0:98, :, :], key="a_qtd", reads=["a_stq"], writes=[("QTd", t, 0)])
        transposes(kaug, 8, 98, 6, stk, "a_stk", ["a_kaug_n", "a_kaug_r", "a_kaug_one"])
        S.dma("sp", d["KTd"][0:8, 0:98, t * 128:(t + 1) * 128].rearrange("h r t -> r h t"), stk[0:98, :, :], key="a_ktd", reads=["a_stk"], writes=[("KTd", t, 0)])
        transposes(qaugf, 8, 66, 7, stqf, "a_stqf", ["a_qaugf_hi", "a_qaugf_lo", "a_qaugf_n"])
        S.dma("sp", d["QTd"][8:16, 0:66, t * 128:(t + 1) * 128].rearrange("h r t -> r h t"), stqf[0:66, :, :], key="a_qtdf", reads=["a_stqf"], writes=[("QTd", t, 1)])
        transposes(kaugf, 8, 66, 4, stkf, "a_stkf", ["a_kaugf_n", "a_kaugf_one"])
        S.dma("sp", d["KTd"][8:16, 0:66, t * 128:(t + 1) * 128].rearrange("h r t -> r h t"), stkf[0:66, :, :], key="a_ktdf", reads=["a_stkf"], writes=[("KTd", t, 1)])
    self.end_phase()


def _phase_odd_b(self, layer):
    S, nc, d = self.S, self.nc, self.d
    self.begin_phase()
    sb = self.sb
    PB = self.pb
    kb_all, ef_all = self.kb_all, self.ef_all
    QT = [sb(f"b_QT{i}", [128, S_TOK], BF16) for i in range(2)]
    KT = [sb(f"b_KT{i}", [128, S_TOK], BF16) for i in range(2)]
    VP = [sb(f"b_VP{i}", [128, NT, 64], BF16) for i in range(2)]
    W = [sb(f"b_W{i}", [128, NT, 64], BF16) for i in range(2)]
    pT = [sb(f"b_pT{i}", [128, 512], BF16) for i in range(3)]
    rl = [sb(f"b_rl{i}", [64, 512], F32) for i in range(2)]
    yo = [sb(f"b_yo{i}", [64, 512], BF16) for i in range(2)]
    mk = sb("b_mk", [128, 2, 128], F32)
    mkb = sb("b_mkb", [128, 2, 128], BF16)
    S.dma("sp", mk[:, 0, :], d["c_maskc"], key="b_mk0", writes=["b_mk0"])
    S.dma("sp", mk[:, 1, :], d["c_maskf"], key="b_mk1", writes=["b_mk1"])
    S.op("dve", lambda: nc.vector.tensor_copy(mkb[:], mk[:]), ["b_mk0", "b_mk1"], ["b_mkb"])
    unit = 0
    for hh in range(16):
        b = hh % 2
        fox = 1 if hh >= 8 else 0
        da = 66 if fox else 98
        S.dma("sp", QT[b][0:da, :], d["QTd"][hh, 0:da, :], key=f"b_QT{b}", reads=[("QTd", t, fox) for t in range(NT)], writes=[f"b_QT{b}"])
        S.dma("sp", KT[b][0:da, :], d["KTd"][hh, 0:da, :], key=f"b_KT{b}", reads=[("KTd", t, fox) for t in range(NT)], writes=[f"b_KT{b}"])
        S.dma("sp", VP[b][:], d["VPd"][:, hh, :].rearrange("(kt p) c -> p kt c", p=128), key=f"b_VP{b}",
              reads=[("VPd", t) for t in range(NT)], writes=[f"b_VP{b}"])
        S.op("pool", lambda b=b, hh=hh: nc.gpsimd.tensor_copy(W[b][:], ef_all[:, :, hh].unsqueeze(2).to_broadcast([128, NT, 64])),
             [("ef", t, fox) for t in range(NT)], [f"b_W{b}"])
        for j in range(S_TOK // 512):
            ob = 4 + 2 * (j % 2)
            nkt = 4 * j + 4
            for kt in range(nkt):
                qoff = 0 if kt < 4 * j else 128 * (kt - 4 * j)
                ncol = 512 - qoff
                sbk = unit % 3
                unit += 1
                S.op("pe", self.mm_group(PB[sbk][:, 0:ncol], [(KT[b][0:da, kt * 128:(kt + 1) * 128], QT[b][0:da, j * 512 + qoff:(j + 1) * 512])]),
                     [f"b_KT{b}", f"b_QT{b}"], [f"pb{sbk}"])
                p = pT[sbk]
                self.act(p[:, 0:ncol], PB[sbk][:, 0:ncol], AF.Exp, [f"pb{sbk}", ("kb", kt, fox)], [f"b_pT{sbk}"],
                         bias=kb_all[:, kt, hh:hh + 1], scale=1.0)
                if kt >= 4 * j:
                    self.tt("dve", p[:, 0:128], p[:, 0:128], mkb[:, fox, :], ALU.mult, [f"b_pT{sbk}", "b_mkb"], [f"b_pT{sbk}"])
                st, sp_ = (kt == 0), (kt == nkt - 1)

                def fpv(b=b, kt=kt, qoff=qoff, p=p, ncol=ncol, st=st, sp_=sp_, ob=ob):
                    nc.tensor.matmul(PB[ob][0:64, qoff:512], VP[b][:, kt, :], p[:, 0:ncol], start=st, stop=sp_)
                    return nc.tensor.matmul(PB[ob + 1][0:64, qoff:512], W[b][:, kt, :], p[:, 0:ncol], start=st, stop=sp_)
                S.op("pe", fpv, [f"b_pT{sbk}", f"b_VP{b}", f"b_W{b}"], [f"pb{ob}", f"pb{ob + 1}"])
            r = j % 2
            S.op("dve", lambda r=r, ob=ob: nc.vector.reciprocal(rl[r][:], PB[ob + 1][0:64, :]), [f"pb{ob + 1}"], [f"b_rl{r}"])
            self.tt("dve", yo[r][:], PB[ob][0:64, :], rl[r][:], ALU.mult, [f"pb{ob}", f"b_rl{r}"], [f"b_yo{r}"])
            ck, hf = hh // 2, hh % 2
            S.dma("sp", d["mixTd"][hf * 64:(hf + 1) * 64, ck, j * 512:(j + 1) * 512], yo[r][:], key=f"b_yo{r}",
                  reads=[f"b_yo{r}"], writes=[("mixTd", j, hh)])
    self.end_phase()


def _phase_odd_c(self, layer):
    S, nc, d = self.S, self.nc, self.d
    i = self.PI(layer)
    self.begin_phase()
    sb = self.sb
    PB = self.pb
    self.alloc_finish()
    self.load_ln_params("ln_mix_g", "ln_mix_b", layer, router_layer=layer)
    w_out = sb("c_wout", [128, KC, D], BF16)
    for k in range(KC):
        S.dma("pool", w_out[:, k, :], d["od_w_out"][i, k * 128:(k + 1) * 128, :], key="c_wout", writes=["c_wout"])
    mix = [sb(f"c_mix{b}", [128, KC, 512], BF16) for b in range(2)]
    z = sb("c_z", [128, D], F32)
    xsrc = d["x"] if self.single is not None else d["xres"]
    for tb in range(NT // 4):
        mb = mix[tb % 2]
        mn = f"c_mix{tb % 2}"
        S.dma("sp", mb[:], d["mixTd"][:, :, tb * 512:(tb + 1) * 512], key=mn, reads=[("mixTd", tb, hh) for hh in range(16)], writes=[mn])
        for tt in range(4):
            t = tb * 4 + tt
            tok = slice(tt * 128, (tt + 1) * 128)
            S.dma("sp", z[:], xsrc[t * 128:(t + 1) * 128, :], key="c_z", reads=[("res", t)], writes=["c_z"])
            for nb in range(2):
                bank = 5 + nb
                S.op("pe", self.mm_group(PB[bank][:], [(mb[:, k, tok], w_out[:, k, nb * 512:(nb + 1) * 512]) for k in range(KC)]),
                     reads=[mn, "c_wout"], writes=[f"pb{bank}"])
                self.stt("dve", z[:, nb * 512:(nb + 1) * 512], z[:, nb * 512:(nb + 1) * 512], DN_ALPHA, PB[bank][:],
                         ALU.mult, ALU.add, ["c_z", f"pb{bank}"], ["c_z"])
            self.finish_tile(z, "c_z", t, d["xres"], True, [7, 0, 1, 2])
    self.end_phase()


Prog.phase_odd = _phase_odd
Prog.phase_odd_a = _phase_odd_a
Prog.phase_odd_b = _phase_odd_b
Prog.phase_odd_c = _phase_odd_c
```

```python
import numpy as np
import concourse.bass as bass
import concourse.mybir as mybir
from concourse.bass_utils import run_bass_kernel_spmd
from contextlib import ExitStack

F32 = mybir.dt.float32
BF16 = mybir.dt.bfloat16
ALU = mybir.AluOpType
AF = mybir.ActivationFunctionType
AX = mybir.AxisListType


class _Op:
    __slots__ = ("eng", "fn", "deps", "signal", "sigval", "dma_key", "dma_val")

    def __init__(self, eng, fn):
        self.eng = eng
        self.fn = fn
        self.deps = []
        self.signal = False
        self.sigval = 0
        self.dma_key = None
        self.dma_val = 0


class Sched:
    ENGS = ("pe", "dve", "act", "pool", "sp")

    def __init__(self, nc, es):
        self.nc = nc
        self.es = es
        self.h = {"pe": nc.tensor, "dve": nc.vector, "act": nc.scalar,
                  "pool": nc.gpsimd, "sp": nc.sync}
        self.sem = {e: es.enter_context(nc.semaphore("s_" + e)) for e in self.ENGS}
        self.dsem = {}
        self.reset()

    def sbuf(self, name, shape, dt, es=None):
        return (es or self.es).enter_context(self.nc.sbuf_tensor(name, list(shape), dt))

    def psum(self, name, shape, dt=F32, es=None):
        return (es or self.es).enter_context(self.nc.psum_tensor(name, list(shape), dt))

    def reset(self):
        self.ops = {e: [] for e in self.ENGS}
        self.buf = {}
        self.dcount = {}

    def _track(self, op, reads, writes):
        deps = []
        for b in reads:
            st = self.buf.get(b)
            if st is None:
                st = self.buf[b] = [None, []]
            if st[0] is not None:
                deps.append(st[0])
        for b in writes:
            st = self.buf.get(b)
            if st is None:
                st = self.buf[b] = [None, []]
            if st[0] is not None:
                deps.append(st[0])
            deps.extend(st[1])
        for b in reads:
            self.buf[b][1].append(op)
        for b in writes:
            self.buf[b][0] = op
            self.buf[b][1] = []
        seen = set()
        for d in deps:
            if d is op or id(d) in seen:
                continue
            seen.add(id(d))
            if d.dma_key is None and d.eng == "pe" and op.eng == "pe" and op.dma_key is None:
                continue
            op.deps.append(d)
            if d.dma_key is None:
                d.signal = True

    def op(self, eng, fn, reads=(), writes=()):
        o = _Op(eng, fn)
        self._track(o, reads, writes)
        self.ops[eng].append(o)
        return o

    def dma(self, eng, out, in_, key, reads=(), writes=(), **kw):
        h = self.h[eng]
        o = _Op(eng, lambda: h.dma_start(out=out, in_=in_, **kw))
        o.dma_key = key
        if key not in self.dsem:
            self.dsem[key] = self.es.enter_context(self.nc.semaphore("d_" + str(key)))
        self.dcount[key] = self.dcount.get(key, 0) + 1
        o.dma_val = 16 * self.dcount[key]
        self._track(o, reads, writes)
        self.ops[eng].append(o)
        return o

    def flush(self, final=False):
        nc = self.nc
        for e in self.ENGS:
            c = 0
            for o in self.ops[e]:
                if o.dma_key is None and o.signal:
                    c += 1
                    o.sigval = c
        used_keys = {e: {} for e in self.ENGS}

        def emit(e, h):
            waited = {}
            for o in self.ops[e]:
                need = {}
                for d in o.deps:
                    if d.dma_key is not None:
                        k = ("d", d.dma_key)
                        v = d.dma_val
                    else:
                        k = ("e", d.eng)
                        v = d.sigval
                    if v > need.get(k, 0):
                        need[k] = v
                for k, v in need.items():
                    if waited.get(k, 0) >= v:
                        continue
                    waited[k] = v
                    s = self.dsem[k[1]] if k[0] == "d" else self.sem[k[1]]
                    h.wait_ge(s, v)
                ins = o.fn()
                if o.dma_key is not None:
                    ins.then_inc(self.dsem[o.dma_key], 16)
                    used_keys[e][o.dma_key] = o.dma_val
                elif o.signal:
                    ins.then_inc(self.sem[o.eng], 1)
            for k, v in used_keys[e].items():
                if waited.get(("d", k), 0) < v:
                    h.wait_ge(self.dsem[k], v)

        with nc.Block() as block:
            @block.tensor
            def _(h):
                emit("pe", h)

            @block.vector
            def _(h):
                emit("dve", h)

            @block.scalar
            def _(h):
                emit("act", h)

            @block.gpsimd
            def _(h):
                emit("pool", h)

            @block.sync
            def _(h):
                emit("sp", h)
        if not final:
            nc.all_engine_barrier()
            for e in self.ENGS:
                self.h[e].sem_clear(self.sem[e])
            ks = list(self.dcount.keys())
            for i, k in enumerate(ks):
                self.h["sp"].sem_clear(self.dsem[k])
            nc.all_engine_barrier()
        self.reset()


import os
S_TOK = int(os.environ.get('K_STOK', '4096'))
NT = S_TOK // 128
D = 1024
KC = 8
DEPTH = 4
NE = 32
DN_ALPHA = float((2 * DEPTH) ** 0.25)
EPS = 1e-5
GELU_C = 1.5957691216057308


class Prog:
    def __init__(self, stop_after=None, single=None):
        self.stop_after = stop_after
        self.single = single
        nc = self.nc = bass.Bass("TRN2", target_bir_lowering=False)
        self.es = ExitStack()
        self.S = Sched(nc, self.es)
        self.d = {}

    def din(self, name, shape, dt=F32):
        t = self.nc.dram_tensor(name, list(shape), dt, kind="ExternalInput")
        self.d[name] = t.ap()
        return self.d[name]

    def dscratch(self, name, shape, dt):
        t = self.nc.dram_tensor(name, list(shape), dt, kind="Internal")
        self.d[name] = t.ap()
        return self.d[name]

    def PI(self, layer):
        return 0 if self.single is not None else layer // 2

    def LI(self, layer):
        return 0 if self.single is not None else layer

    def declare(self):
        d0 = self.din
        one = self.single is not None

        def d(name, shape, dt=F32):
            if one and not name.startswith("c_") and name != "x":
                shape = [1] + list(shape[1:])
            return d0(name, shape, dt)
        d("x", [S_TOK, D])
        if self.single in (None, 0):
            d("ev_w_in", [2, D, 2560]); d("ev_conv_w", [2, 3, 512]); d("ev_sgu_ln_g", [2, 512]); d("ev_sgu_ln_b", [2, 512])
            d("ev_sgu_w", [2, 8, 128, 128]); d("ev_sgu_b", [2, 8, 128]); d("ev_w_out", [2, D, D])
        if self.single in (None, 1):
            d("od_w_in", [2, D, 1960]); d("od_q_norm_g", [2, 256]); d("od_kv_norm_g", [2, 128])
            d("od_w_uq", [2, 256, 768]); d("od_w_ukv", [2, 128, 1024]); d("od_f_bias", [2, 8]); d("od_w_out", [2, D, D])
        d("ln_mix_g", [4, D]); d("ln_mix_b", [4, D]); d("ln_ffn_g", [4, D]); d("ln_ffn_b", [4, D])
        d("moe_w_router", [4, D, NE]); d("moe_b_router", [4, NE])
        d("moe_w1p", [4, NE, 8, D, 256]); d("moe_b1p", [4, NE * 16, 128])
        d("moe_w2", [4, NE, D, D]); d("moe_b2", [4, NE, D])
        d("c_ident", [128, 128])
        if self.single in (None, 1):
            d("c_tri", [128, 128]); d("c_maskc", [128, 128]); d("c_maskf", [128, 128])
            d("c_cos", [S_TOK, 16]); d("c_sin", [S_TOK, 16])
        self.out = self.nc.dram_tensor("out", [S_TOK, D], F32, kind="ExternalOutput").ap()
        self.dscratch("xres", [S_TOK, D], F32)
        self.dscratch("xTd", [128, KC, S_TOK], BF16)
        self.dscratch("ffn", [S_TOK, D], F32)

    def mm_group(self, out, pairs):
        nc = self.nc
        n = len(pairs)

        def f():
            ins = None
            for i, (l, r) in enumerate(pairs):
                ins = nc.tensor.matmul(out, l, r, start=(i == 0), stop=(i == n - 1))
            return ins
        return f

    def V(self, eng):
        return {"dve": self.nc.vector, "pool": self.nc.gpsimd}[eng]

    def tt(self, eng, out, a, b, op, reads, writes):
        h = self.V(eng)
        self.S.op(eng, lambda: h.tensor_tensor(out, a, b, op), reads, writes)

    def ts(self, eng, out, a, s1, s2, op0, op1, reads, writes):
        h = self.V(eng)
        if op1 is None:
            self.S.op(eng, lambda: h.tensor_scalar(out, a, s1, None, op0), reads, writes)
        else:
            self.S.op(eng, lambda: h.tensor_scalar(out, a, s1, s2, op0, op1), reads, writes)

    def stt(self, eng, out, a, sc, b, op0, op1, reads, writes):
        h = self.V(eng)
        self.S.op(eng, lambda: h.scalar_tensor_tensor(out, a, sc, b, op0, op1), reads, writes)

    def act(self, out, in_, func, reads, writes, bias=None, scale=None):
        nc = self.nc
        kw = {}
        if bias is not None:
            kw["bias"] = bias
        if scale is not None:
            kw["scale"] = scale
        self.S.op("act", lambda: nc.scalar.activation(out, in_, func, **kw), reads, writes)

    def rsum(self, eng, out, in_, reads, writes):
        h = self.V(eng)
        self.S.op(eng, lambda: h.reduce_sum(out, in_, AX.X), reads, writes)

    def begin_phase(self):
        self.pes = ExitStack()
        S = self.S
        self.phase_id = getattr(self, "phase_id", 0) + 1
        self.pb = [S.psum(f"pb{i}_p{self.phase_id}", [128, 512], F32, es=self.pes) for i in range(8)]
        self._uid = 0

    def end_phase(self, final=False):
        self.S.flush(final=final)
        self.pes.close()

    def sb(self, name, shape, dt):
        return self.S.sbuf(f"{name}_p{self.phase_id}", shape, dt, es=self.pes)

    def bfv(self, i):
        return self.pb[i][:].bitcast(BF16)

    def load_consts(self):
        S, nc, d = self.S, self.nc, self.d
        self.ident_b = S.sbuf("ident_b", [128, 128], BF16)
        self.ident_f = S.sbuf("ident_f", [128, 128], F32)
        self.gates = S.sbuf("gates", [128, NT, NE], F32)
        self.gT = S.sbuf("gT", [NE, S_TOK], BF16)
        S.dma("pool", self.ident_b[:], d["c_ident"], key="cid_b", writes=["ident_b"])
        S.dma("sp", self.ident_f[:], d["c_ident"], key="cid_f", writes=["ident_f"])

    def emit_xT(self, xb, xbname, stage, stname, j, bank):
        S, nc = self.S, self.nc
        pv = self.bfv(bank)
        pn = f"pb{bank}"

        def f():
            ins = None
            for k in range(KC):
                ins = nc.tensor.transpose(pv[:, k * 128:(k + 1) * 128], xb[:, k * 128:(k + 1) * 128], self.ident_b[:])
            return ins
        S.op("pe", f, reads=[xbname, "ident_b"], writes=[pn])
        pv3 = pv.rearrange("p (k t) -> p k t", k=KC)
        S.op("dve", lambda: nc.vector.tensor_copy(stage[:, :, j * 128:(j + 1) * 128], pv3), reads=[pn], writes=[stname + str(j)])

    def alloc_finish(self):
        sb = self.sb
        self.f_sq = sb("f_sq", [128, D], F32)
        self.f_xn = sb("f_xn", [128, D], F32)
        self.f_xb = sb("f_xb", [128, D], BF16)
        self.f_st = sb("f_st", [128, 16], F32)
        self.f_g = sb("f_g", [128, D], F32)
        self.f_b = sb("f_b", [128, D], F32)
        self.f_stage = sb("f_stage", [128, KC, 512], BF16)
        self.r_x32 = sb("r_x32", [128, KC, 128], F32)
        self.r_w = sb("r_w", [128, KC, NE], F32)
        self.r_bb = sb("r_bb", [128, NE], F32)
        self.r_lg = sb("r_lg", [128, NE], F32)
        self.r_t8 = sb("r_t8", [128, 8], F32)
        self.r_ex = sb("r_ex", [128, NE], F32)
        self.r_sm = sb("r_sm", [128, 4], F32)
        self.r_gb = sb("r_gb", [128, NE], BF16)

    def load_ln_params(self, gname, bname, layer, router_layer=None):
        S, d = self.S, self.d
        layer = self.LI(layer)
        if router_layer is not None:
            router_layer = layer
        S.dma("sp", self.f_g[:], d[gname][layer:layer + 1, :].to_broadcast([128, D]), key="f_g", writes=["f_g"])
        S.dma("sp", self.f_b[:], d[bname][layer:layer + 1, :].to_broadcast([128, D]), key="f_b", writes=["f_b"])
        if router_layer is not None:
            S.dma("sp", self.r_w[:], d["moe_w_router"][router_layer].rearrange("(k p) e -> p k e", p=128), key="r_w", writes=["r_w"])
            S.dma("sp", self.r_bb[:], d["moe_b_router"][router_layer:router_layer + 1, :].to_broadcast([128, NE]), key="r_bb", writes=["r_bb"])

    def finish_tile(self, z, zname, t, dst, router, banks):
        S, nc = self.S, self.nc
        st = self.f_st
        j = t % 4
        self.rsum("dve", st[:, 0:1], z[:], [zname], ["f_st0"])
        self.ts("dve", st[:, 1:2], st[:, 0:1], -1.0 / D, None, ALU.mult, None, ["f_st0"], ["f_st1"])
        self.act(self.f_sq[:], z[:], AF.Square, [zname, "f_st1"], ["f_sq"], bias=st[:, 1:2], scale=1.0)
        self.rsum("dve", st[:, 2:3], self.f_sq[:], ["f_sq"], ["f_st2"])
        self.ts("dve", st[:, 3:4], st[:, 2:3], 1.0 / D, EPS, ALU.mult, ALU.add, ["f_st2"], ["f_st3"])
        self.act(st[:, 4:5], st[:, 3:4], AF.Sqrt, ["f_st3"], ["f_st4"])
        S.op("dve", lambda: nc.vector.reciprocal(st[:, 4:5], st[:, 4:5]), ["f_st4"], ["f_st4"])
        self.ts("dve", self.f_xn[:], z[:], st[:, 1:2], st[:, 4:5], ALU.add, ALU.mult, [zname, "f_st1", "f_st4"], ["f_xn"])
        self.tt("dve", self.f_xn[:], self.f_xn[:], self.f_g[:], ALU.mult, ["f_xn", "f_g"], ["f_xn"])
        self.tt("dve", z[:], self.f_xn[:], self.f_b[:], ALU.add, ["f_xn", "f_b"], [zname])
        S.dma("sp", dst[t * 128:(t + 1) * 128, :], z[:], key="f_out_" + zname, reads=[zname], writes=[("res", t)])
        self.S.op("act", lambda: nc.scalar.copy(self.f_xb[:], z[:]), [zname], ["f_xb"])
        self.emit_xT(self.f_xb, "f_xb", self.f_stage, "f_stage", j, banks[0])
        if j == 3:
            blk = t // 4
            S.dma("sp", self.d["xTd"][:, :, blk * 512:(blk + 1) * 512], self.f_stage[:], key="f_xTd",
                  reads=["f_stage0", "f_stage1", "f_stage2", "f_stage3"], writes=[("xTd", blk)])
        if not router:
            return
        b1, b2, b3 = banks[1], banks[2], banks[3]

        def ftr():
            ins = None
            for k in range(KC):
                pbk = self.pb[b1] if k < 4 else self.pb[b2]
                ins = nc.tensor.transpose(pbk[:, (k % 4) * 128:(k % 4 + 1) * 128], z[:, k * 128:(k + 1) * 128], self.ident_f[:])
            return ins
        S.op("pe", ftr, reads=[zname, "ident_f"], writes=[f"pb{b1}", f"pb{b2}"])
        x32 = self.r_x32
        S.op("act", lambda: nc.scalar.copy(x32[:, 0:4, :], self.pb[b1][:].rearrange("p (k t) -> p k t", k=4)), [f"pb{b1}"], ["r_x32a"])
        S.op("act", lambda: nc.scalar.copy(x32[:, 4:8, :], self.pb[b2][:].rearrange("p (k t) -> p k t", k=4)), [f"pb{b2}"], ["r_x32b"])
        lgp = self.pb[b3][:, 0:NE]
        S.op("pe", self.mm_group(lgp, [(x32[:, k, :], self.r_w[:, k, :]) for k in range(KC)]),
             reads=["r_x32a", "r_x32b", "r_w"], writes=[f"pb{b3}"])
        lg, t8, ex, sm = self.r_lg, self.r_t8, self.r_ex, self.r_sm
        self.tt("dve", lg[:], lgp, self.r_bb[:], ALU.add, [f"pb{b3}", "r_bb"], ["r_lg"])
        S.op("dve", lambda: nc.vector.max(out=t8[:], in_=lg[:]), ["r_lg"], ["r_t8"])
        self.ts("dve", sm[:, 0:1], t8[:, 0:1], -1.0, None, ALU.mult, None, ["r_t8"], ["r_sm0"])
        self.act(ex[:], lg[:], AF.Exp, ["r_lg", "r_sm0"], ["r_ex"], bias=sm[:, 0:1], scale=1.0)
        self.stt("dve", ex[:], lg[:], t8[:, 3:4], ex[:], ALU.is_ge, ALU.mult, ["r_lg", "r_t8", "r_ex"], ["r_ex"])
        self.rsum("dve", sm[:, 1:2], ex[:], ["r_ex"], ["r_sm1"])
        S.op("dve", lambda: nc.vector.reciprocal(sm[:, 2:3], sm[:, 1:2]), ["r_sm1"], ["r_sm2"])
        self.ts("dve", self.gates[:, t, :], ex[:], sm[:, 2:3], None, ALU.mult, None, ["r_ex", "r_sm2"], [("gates", t)])
        S.op("act", lambda: nc.scalar.copy(self.r_gb[:], self.gates[:, t, :]), [("gates", t)], ["r_gb"])
        gtp = self.bfv(b3)[0:NE, 512:640]
        S.op("pe", lambda: nc.tensor.transpose(gtp, self.r_gb[:], self.ident_b[:]), ["r_gb", "ident_b"], [f"pb{b3}"])
        S.op("act", lambda: nc.scalar.copy(self.gT[:, t * 128:(t + 1) * 128], gtp), [f"pb{b3}"], [("gT", t)])

    def phase_init(self):
        S, nc, d = self.S, self.nc, self.d
        self.begin_phase()
        self.load_consts()
        xin = [self.sb(f"i_x{i}", [128, D], F32) for i in range(2)]
        xb = [self.sb(f"i_xb{i}", [128, D], BF16) for i in range(2)]
        stage = [self.sb(f"i_st{i}", [128, KC, 512], BF16) for i in range(2)]
        for t in range(NT):
            b = t % 2
            S.dma("sp", xin[b][:], d["x"][t * 128:(t + 1) * 128, :], key=f"i_x{b}", writes=[f"i_x{b}"])
            S.op("act", lambda b=b: nc.scalar.copy(xb[b][:], xin[b][:]), [f"i_x{b}"], [f"i_xb{b}"])
            sg = (t // 4) % 2
            self.emit_xT(xb[b], f"i_xb{b}", stage[sg], f"i_st{sg}_", t % 4, t % 2)
            if t % 4 == 3:
                blk = t // 4
                S.dma("sp", d["xTd"][:, :, blk * 512:(blk + 1) * 512], stage[sg][:], key=f"i_xTd{sg}",
                      reads=[f"i_st{sg}_{j}" for j in range(4)], writes=[("xTd", blk)])
        self.end_phase()

    def load_T(self, dst, dstname, src, R, tmp, tmpname, bank):
        S, nc = self.S, self.nc
        S.dma("sp", tmp[0:R, :], src, key=tmpname, writes=[tmpname])
        pv = self.pb[bank][:, 0:R]
        S.op("pe", lambda: nc.tensor.transpose(pv, tmp[0:R, :], self.ident_f[0:R, 0:R]), [tmpname, "ident_f"], [f"pb{bank}"])
        S.op("dve", lambda: nc.vector.tensor_copy(dst, pv), [f"pb{bank}"], [dstname])

    def phase_even(self, layer):
        S, nc, d = self.S, self.nc, self.d
        i = self.PI(layer)
        self.begin_phase()
        sb = self.sb
        self.alloc_finish()
        self.load_ln_params("ln_mix_g", "ln_mix_b", layer, router_layer=layer)
        w_in = sb("e_win", [128, KC, 2560], BF16)
        w_out = sb("e_wout", [128, KC, D], BF16)
        for k in range(KC):
            S.dma("pool", w_in[:, k, :], d["ev_w_in"][i, k * 128:(k + 1) * 128, :], key="e_win", writes=["e_win"])
        for k in range(KC):
            S.dma("pool", w_out[:, k, :], d["ev_w_out"][i, k * 128:(k + 1) * 128, :], key="e_wout", writes=["e_wout"])
        tmpT = sb("e_tmpT", [128, 128], F32)
        cw = sb("e_cw", [128, 12], F32)
        self.load_T(cw[:, :], "e_cw", d["ev_conv_w"][i].rearrange("k (c p) -> (k c) p", p=128), 12, tmpT, "e_tmpT", 0)
        lg = sb("e_lg", [128, 512], F32)
        lb = sb("e_lb", [128, 512], F32)
        S.dma("sp", lg[:], d["ev_sgu_ln_g"][i:i + 1, :].to_broadcast([128, 512]), key="e_lg", writes=["e_lg"])
        S.dma("sp", lb[:], d["ev_sgu_ln_b"][i:i + 1, :].to_broadcast([128, 512]), key="e_lb", writes=["e_lb"])
        wraw = sb("e_wraw", [128, 8, 128], F32)
        wrb = sb("e_wrb", [128, 8, 128], BF16)
        wsT = sb("e_wsT", [128, 8, 128], BF16)
        S.dma("sp", wraw[:], d["ev_sgu_w"][i].rearrange("g i j -> i g j"), key="e_wraw", writes=["e_wraw"])
        S.op("pool", lambda: nc.gpsimd.memset(wraw[0:64, :, 64:128], 0.0), ["e_wraw"], ["e_wraw"])
        S.op("dve", lambda: nc.vector.tensor_copy(wrb[:], wraw[:]), ["e_wraw"], ["e_wrb"])
        pv = self.bfv(1)

        def ftr():
            ins = None
            for g in range(8):
                ins = nc.tensor.transpose(pv[:, g * 128:(g + 1) * 128], wrb[:, g, :], self.ident_b[:])
            return ins
        S.op("pe", ftr, ["e_wrb", "ident_b"], ["pb1"])
        S.op("dve", lambda: nc.vector.tensor_copy(wsT[:], pv.rearrange("p (g i) -> p g i", g=8)), ["pb1"], ["e_wsT"])
        sgub = sb("e_sgub", [128, 4, 4, 128], F32)
        for g in range(8):
            k, hh = g // 2, g % 2
            for tt in range(4):
                S.dma("sp", sgub[hh * 64:(hh + 1) * 64, k, tt, :], d["ev_sgu_b"][i, g:g + 1, :].to_broadcast([64, 128]),
                      key="e_sgub", writes=["e_sgub"])
        xblk = [sb(f"e_xblk{b}", [128, KC, 512], BF16) for b in range(2)]
        hbuf = sb("e_h", [128, 4, 514], F32)
        S.op("pool", lambda: nc.gpsimd.memset(hbuf[:, :, 0:2], 0.0), [], ["e_h0", "e_h1", "e_h2", "e_h3"])
        mix = sb("e_mix", [128, KC, 512], BF16)
        ug = sb("e_ug", [128, 4, 512], F32)
        t1 = sb("e_t1", [128, 512], F32)
        t2 = sb("e_t2", [128, 512], F32)
        t3 = sb("e_t3", [128, 512], F32)
        t4 = sb("e_t4", [128, 512], F32)
        vz = sb("e_vz", [128, 512], F32)
        vc = sb("e_vc", [128, 512], F32)
        vs = sb("e_vs", [128, 32], F32)
        vnb = sb("e_vnb", [128, 4, 512], BF16)
        z = sb("e_z", [128, D], F32)
        xsrc = d["x"] if (layer == 0 or self.single is not None) else d["xres"]
        PB = self.pb

        def gelu(src_ps, srcname, dst, dstname):
            self.act(dst, src_ps, AF.Gelu_apprx_tanh, [srcname], [dstname])

        for tb in range(NT // 4):
            xb = xblk[tb % 2]
            xbn = f"e_xblk{tb % 2}"
            S.dma("sp", xb[:], d["xTd"][:, :, tb * 512:(tb + 1) * 512], key=xbn, reads=[("xTd", tb)], writes=[xbn])

            def proj_fm(bank, col0):
                S.op("pe", self.mm_group(PB[bank][:], [(w_in[:, k, col0:col0 + 128], xb[:, k, :]) for k in range(KC)]),
                     reads=["e_win", xbn], writes=[f"pb{bank}"])
            for c in range(4):
                proj_fm(0, c * 128)
                proj_fm(1, 1024 + c * 128)
                proj_fm(2, 512 + c * 128)
                S.op("act", lambda: nc.scalar.copy(t2[:], PB[0][:]), ["pb0"], ["e_t2"])
                hn = f"e_h{c}"
                self.tt("dve", hbuf[:, c, 2:514], t2[:], PB[1][:], ALU.mult, ["e_t2", "pb1"], [hn])
                self.ts("dve", t3[:], hbuf[:, c, 2:514], cw[:, 8 + c:9 + c], None, ALU.mult, None, [hn, "e_cw"], ["e_t3"])
                self.stt("dve", t3[:], hbuf[:, c, 1:513], cw[:, 4 + c:5 + c], t3[:], ALU.mult, ALU.add, [hn, "e_cw", "e_t3"], ["e_t3"])
                self.stt("dve", t3[:], hbuf[:, c, 0:512], cw[:, c:c + 1], t3[:], ALU.mult, ALU.add, [hn, "e_cw", "e_t3"], ["e_t3"])
                self.tt("dve", mix[:, c, :], t3[:], PB[2][:], ALU.mult, ["e_t3", "pb2"], ["e_mix"])
                S.op("pool", lambda c=c: nc.gpsimd.tensor_copy(hbuf[:, c, 0:2], hbuf[:, c, 512:514]), [hn], [hn])
            for c in range(4):
                proj_fm(3, 1536 + c * 128)
                gelu(PB[3][:], "pb3", ug[:, c, :], f"e_ug{c}")
            for tt in range(4):
                tok = slice(tt * 128, (tt + 1) * 128)
                S.op("pe", self.mm_group(PB[4][:], [(xb[:, k, tok], w_in[:, k, 2048:2560]) for k in range(KC)]),
                     reads=["e_win", xbn], writes=["pb4"])
                gelu(PB[4][:], "pb4", vz[:], "e_vz")
                vz3 = vz[:].rearrange("p (g c) -> p g c", g=8)
                vc3 = vc[:].rearrange("p (g c) -> p g c", g=8)
                self.rsum("dve", vs[:, 0:8], vz3, ["e_vz"], ["e_vs0"])
                self.ts("dve", vs[:, 8:16], vs[:, 0:8], 1.0 / 64, None, ALU.mult, None, ["e_vs0"], ["e_vs1"])
                self.tt("dve", vc3, vz3, vs[:, 8:16].unsqueeze(2).to_broadcast([128, 8, 64]), ALU.subtract, ["e_vz", "e_vs1"], ["e_vc"])
                self.act(vz[:], vc[:], AF.Square, ["e_vc"], ["e_vz"])
                self.rsum("dve", vs[:, 16:24], vz3, ["e_vz"], ["e_vs2"])
                self.ts("dve", vs[:, 16:24], vs[:, 16:24], 1.0 / 64, EPS, ALU.mult, ALU.add, ["e_vs2"], ["e_vs2"])
                self.act(vs[:, 24:32], vs[:, 16:24], AF.Sqrt, ["e_vs2"], ["e_vs3"])
                S.op("dve", lambda: nc.vector.reciprocal(vs[:, 24:32], vs[:, 24:32]), ["e_vs3"], ["e_vs3"])
                self.tt("dve", vc3, vc3, vs[:, 24:32].unsqueeze(2).to_broadcast([128, 8, 64]), ALU.mult, ["e_vc", "e_vs3"], ["e_vc"])
                self.tt("dve", vc[:], vc[:], lg[:], ALU.mult, ["e_vc", "e_lg"], ["e_vc"])
                self.tt("dve", vnb[:, tt, :], vc[:], lb[:], ALU.add, ["e_vc", "e_lb"], [f"e_vnb{tt}"])
            for k in range(4):
                def fsg(k=k):
                    ins = None
                    for tt in range(4):
                        nc.tensor.matmul(PB[5][:, tt * 128:(tt + 1) * 128], vnb[:, tt, k * 128:(k + 1) * 128], wsT[:, 2 * k, :], start=True, stop=True)
                        ins = nc.tensor.matmul(PB[6][:, tt * 128:(tt + 1) * 128], vnb[:, tt, k * 128:(k + 1) * 128], wsT[:, 2 * k + 1, :], start=True, stop=True)
                    return ins
                S.op("pe", fsg, reads=[f"e_vnb{tt}" for tt in range(4)] + ["e_wsT"], writes=["pb5", "pb6"])
                sg = sgub[:, k, :, :].rearrange("p t i -> p (t i)")
                self.tt("dve", t4[0:64, :], PB[5][0:64, :], sg[0:64, :], ALU.add, ["pb5", "e_sgub"], ["e_t4a"])
                self.tt("dve", t4[64:128, :], PB[6][64:128, :], sg[64:128, :], ALU.add, ["pb6", "e_sgub"], ["e_t4b"])
                self.tt("dve", mix[:, 4 + k, :], t4[:], ug[:, k, :], ALU.mult, ["e_t4a", "e_t4b", f"e_ug{k}"], ["e_mix"])
            for tt in range(4):
                t = tb * 4 + tt
                tok = slice(tt * 128, (tt + 1) * 128)
                S.dma("sp", z[:], xsrc[t * 128:(t + 1) * 128, :], key="e_z", reads=[("res", t)], writes=["e_z"])
                for nb in range(2):
                    bank = 5 + nb
                    S.op("pe", self.mm_group(PB[bank][:], [(mix[:, k, tok], w_out[:, k, nb * 512:(nb + 1) * 512]) for k in range(KC)]),
                         reads=["e_mix", "e_wout"], writes=[f"pb{bank}"])
                    self.stt("dve", z[:, nb * 512:(nb + 1) * 512], z[:, nb * 512:(nb + 1) * 512], DN_ALPHA, PB[bank][:],
                             ALU.mult, ALU.add, ["e_z", f"pb{bank}"], ["e_z"])
                self.finish_tile(z, "e_z", t, d["xres"], True, [7, 0, 1, 2])
        self.end_phase()

    def phase_copy_out(self):
        S, d = self.S, self.d
        self.begin_phase()
        for q in range(S_TOK // 512):
            S.dma("sp", self.out[q * 512:(q + 1) * 512, :], d["xres"][q * 512:(q + 1) * 512, :], key="cp_out", writes=[("o", q)])
        self.end_phase(final=True)

    def build(self):
        self.declare()
        if self.single is not None:
            layer = self.single
            self.phase_init()
            if layer % 2 == 0:
                self.phase_even(layer)
            else:
                self.phase_odd(layer)
            self.phase_moe(layer, final=True)
            return self.nc
        stop = self.stop_after
        self.phase_init()
        for layer in range(DEPTH):
            if layer % 2 == 0:
                self.phase_even(layer)
            else:
                self.phase_odd(layer)
            if stop == f"mix{layer}":
                self.phase_copy_out()
                return self.nc
            self.phase_moe(layer, final=(layer == DEPTH - 1))
            if stop == f"ffn{layer}":
                if layer != DEPTH - 1:
                    self.phase_copy_out()
                return self.nc
        return self.nc


def _consts():
    c = {}
    c["c_ident"] = np.eye(128, dtype=np.float32)
    i = np.arange(128)
    c["c_tri"] = (i[:, None] <= i[None, :]).astype(np.float32)
    c["c_maskc"] = ((i[:, None] // 64) <= (i[None, :] // 64)).astype(np.float32)
    c["c_maskf"] = (i[:, None] <= i[None, :]).astype(np.float32)
    inv = (10000.0 ** (-np.arange(0, 32, 2, dtype=np.float32) / 32)).astype(np.float32)
    ang = (np.arange(S_TOK, dtype=np.float32)[:, None] * inv[None, :]).astype(np.float32)
    c["c_cos"] = np.cos(ang).astype(np.float32)
    c["c_sin"] = np.sin(ang).astype(np.float32)
    return c


def _prep_weights(inputs):
    w = {}
    for k, v in inputs.items():
        if k in ("x", "moe_w1", "moe_b1"):
            continue
        w[k] = np.ascontiguousarray(np.asarray(v, dtype=np.float32))
    w1 = np.asarray(inputs["moe_w1"], dtype=np.float32)
    L, E = w1.shape[0], w1.shape[1]
    w1v = w1.reshape(L, E, D, 8, 128, 2)
    w1p = np.ascontiguousarray(w1v.transpose(0, 1, 3, 2, 5, 4)).reshape(L, E, 8, D, 256)
    w["moe_w1p"] = w1p
    b1 = np.asarray(inputs["moe_b1"], dtype=np.float32).reshape(L, E, 8, 128, 2)
    b1p = np.ascontiguousarray(b1.transpose(0, 1, 2, 4, 3)).reshape(L, E * 16, 128)
    w["moe_b1p"] = b1p
    w.update(_consts())
    return w


_CACHE = {}


def _get_prog(stop_after=None, single=None):
    key = (stop_after, single)
    if key not in _CACHE:
        p = Prog(stop_after, single)
        p.build()
        _CACHE[key] = p
    return _CACHE[key]


_EV = ("ev_w_in", "ev_conv_w", "ev_sgu_ln_g", "ev_sgu_ln_b", "ev_sgu_w", "ev_sgu_b", "ev_w_out")
_OD = ("od_w_in", "od_q_norm_g", "od_kv_norm_g", "od_w_uq", "od_w_ukv", "od_f_bias", "od_w_out")
_LY = ("ln_mix_g", "ln_mix_b", "ln_ffn_g", "ln_ffn_b", "moe_w_router", "moe_b_router", "moe_w1p", "moe_b1p", "moe_w2", "moe_b2")
_CO = ("c_tri", "c_maskc", "c_maskf", "c_cos", "c_sin")


def run(inputs, stop_after=None, cores=None, trace=False):
    x = np.asarray(inputs["x"], dtype=np.float32)
    n = x.shape[0]
    cores = list(range(n)) if cores is None else cores
    w = _prep_weights(inputs)
    p = _get_prog(stop_after)
    in_maps = []
    for c in cores:
        m = dict(w)
        m["x"] = np.ascontiguousarray(x[c])
        in_maps.append(m)
    res = run_bass_kernel_spmd(p.nc, in_maps, core_ids=list(range(len(cores))), trace=trace)
    return np.stack([r["out"] for r in res.results], axis=0), res


def run_layers(inputs, cores=None, nlayers=DEPTH):
    x = np.asarray(inputs["x"], dtype=np.float32)
    n = x.shape[0]
    cores = list(range(n)) if cores is None else cores
    w = _prep_weights(inputs)
    cur = [np.ascontiguousarray(x[c]) for c in cores]
    for L in range(nlayers):
        par = L % 2
        p = _get_prog(None, single=par)
        base = {"c_ident": w["c_ident"]}
        fam = _EV if par == 0 else _OD
        for k in fam:
            base[k] = w[k][L // 2:L // 2 + 1]
        for k in _LY:
            base[k] = w[k][L:L + 1]
        if par == 1:
            for k in _CO:
                base[k] = w[k]
        in_maps = []
        for ci in range(len(cores)):
            m = dict(base)
            m["x"] = cur[ci]
            in_maps.append(m)
        res = run_bass_kernel_spmd(p.nc, in_maps, core_ids=list(range(len(cores))))
        cur = [np.ascontiguousarray(r["out"]) for r in res.results]
    return np.stack(cur, axis=0)


def kernel(**inputs):
    out = run_layers(inputs)
    return out.astype(np.float32)


def _phase_moe(self, layer, final):
    S, nc, d = self.S, self.nc, self.d
    layer_true = layer
    layer = self.LI(layer)
    self.begin_phase()
    sb = self.sb
    PB = self.pb
    NH = S_TOK // 2
    NTH = NH // 128
    NBH = NH // 512
    tmpT = sb("m_tmpT", [128, 128], F32)
    b1T = sb("m_b1T", [128, NE * 16], F32)
    for q in range(4):
        self.load_T(b1T[:, q * 128:(q + 1) * 128], "m_b1T", d["moe_b1p"][layer, q * 128:(q + 1) * 128, :], 128, tmpT, "m_tmpT", q % 2)
    b1T7 = sb("m_b1T7", [128, NE * 16], F32)
    self.ts("dve", b1T7[:], b1T[:], 7.0, None, ALU.add, None, ["m_b1T"], ["m_b1T7"])
    c14 = sb("m_c14", [128, 1], F32)
    S.op("dve", lambda: nc.vector.memset(c14[:], 14.0), [], ["m_c14"])
    b2 = sb("m_b2", [NE, D], BF16)
    S.dma("pool", b2[:], d["moe_b2"][layer], key="m_b2", writes=["m_b2"])
    xh = sb("m_xh", [128, KC, NH], BF16)
    acc = sb("m_acc", [128, NTH, D], F32)
    actb = sb("m_act", [128, 8, NH], BF16)
    NR = 3
    w1r = [sb(f"m_w1_{i}", [128, KC, 256], BF16) for i in range(NR)]
    w2b = sb("m_w2", [128, KC, D], BF16)
    gt = [sb(f"m_gt{i}", [128, 512], F32) for i in range(2)]
    st = [sb(f"m_st{i}", [128, 512], F32) for i in range(2)]
    lt = [sb(f"m_lt{i}", [128, 512], F32) for i in range(2)]

    def w1_dma(hf, e, fc):
        n = (hf * NE + e) * 8 + fc
        r = n % NR
        S.dma("pool", w1r[r][:], d["moe_w1p"][layer, e, fc].rearrange("(k p) c -> p k c", p=128),
              key=f"m_w1_{r}", writes=[f"m_w1_{r}"])

    for hf in range(2):
        S.dma("sp", xh[:], d["xTd"][:, :, hf * NH:(hf + 1) * NH], key="m_xh",
              reads=[("xTd", b) for b in range(hf * NBH, hf * NBH + NBH)], writes=["m_xh"])
        for tt in range(NTH):
            t = hf * NTH + tt
            for nb in range(2):
                bank = 4 + 2 * (tt % 2) + nb
                S.op("pe", self.mm_group(PB[bank][:], [(self.gT[:, t * 128:(t + 1) * 128], b2[:, nb * 512:(nb + 1) * 512])]),
                     reads=[("gT", t), "m_b2"], writes=[f"pb{bank}"])
                S.op("act", lambda tt=tt, nb=nb, bank=bank: nc.scalar.copy(acc[:, tt, nb * 512:(nb + 1) * 512], PB[bank][:]),
                     [f"pb{bank}"], [("m_acc", tt, nb)])
        w1_dma(hf, 0, 0)
        w1_dma(hf, 0, 1)
        unit = 0
        for e in range(NE):
            S.dma("pool", w2b[:], d["moe_w2"][layer, e].rearrange("(k p) n -> p k n", p=128), key="m_w2", writes=["m_w2"])
            for fc in range(8):
                nxt = e * 8 + fc + 2
                if nxt < NE * 8:
                    w1_dma(hf, nxt // 8, nxt % 8)
                r = ((hf * NE + e) * 8 + fc) % NR
                wp = w1r[r]
                wn = f"m_w1_{r}"
                cg = e * 16 + fc * 2
                for tb in range(NBH):
                    s = unit % 2
                    unit += 1
                    bg, bl = 2 * s, 2 * s + 1
                    tok = slice(tb * 512, (tb + 1) * 512)
                    S.op("pe", self.mm_group(PB[bg][:], [(wp[:, k, 0:128], xh[:, k, tok]) for k in range(KC)]),
                         reads=[wn, "m_xh"], writes=[f"pb{bg}"])
                    S.op("pe", self.mm_group(PB[bl][:], [(wp[:, k, 128:256], xh[:, k, tok]) for k in range(KC)]),
                         reads=[wn, "m_xh"], writes=[f"pb{bl}"])
                    self.ts("dve", gt[s][:], PB[bg][:], b1T[:, cg:cg + 1], 7.0, ALU.add, ALU.min, [f"pb{bg}", "m_b1T"], [f"m_gt{s}"])
                    self.act(st[s][:], gt[s][:], AF.Sigmoid, [f"m_gt{s}"], [f"m_st{s}"], scale=1.702)
                    self.act(lt[s][:], PB[bl][:], AF.Relu, [f"pb{bl}", "m_b1T7"], [f"m_lt{s}"], bias=b1T7[:, cg + 1:cg + 2], scale=1.0)
                    self.act(lt[s][:], lt[s][:], AF.Relu, [f"m_lt{s}", "m_c14"], [f"m_lt{s}"], bias=c14[:, 0:1], scale=-1.0)
                    self.stt("dve", gt[s][:], gt[s][:], -1.0, st[s][:], ALU.mult, ALU.mult, [f"m_gt{s}", f"m_st{s}"], [f"m_gt{s}"])
                    self.stt("dve", actb[:, fc, tok], lt[s][:], -8.0, gt[s][:], ALU.add, ALU.mult,
                             [f"m_lt{s}", f"m_gt{s}"], [("m_act", fc, tb)])
            for tt in range(NTH):
                t = hf * NTH + tt
                tk = slice(tt * 128, (tt + 1) * 128)
                for nb in range(2):
                    bank = 4 + 2 * (tt % 2) + nb
                    S.op("pe", self.mm_group(PB[bank][:], [(actb[:, k, tk], w2b[:, k, nb * 512:(nb + 1) * 512]) for k in range(KC)]),
                         reads=[("m_act", k, tt // 4) for k in range(KC)] + ["m_w2"], writes=[f"pb{bank}"])
                    a = acc[:, tt, nb * 512:(nb + 1) * 512]
                    self.stt("dve", a, PB[bank][:], self.gates[:, t, e:e + 1], a, ALU.mult, ALU.add,
                             [f"pb{bank}", ("gates", t), ("m_acc", tt, nb)], [("m_acc", tt, nb)])
        for tt in range(NTH):
            t = hf * NTH + tt
            S.dma("sp", d["ffn"][t * 128:(t + 1) * 128, :], acc[:, tt, :], key=f"m_ffn{tt}",
                  reads=[("m_acc", tt, 0), ("m_acc", tt, 1)], writes=[("ffn", t)])
    self.end_phase()
    self.phase_ffn_ln(layer_true, final)


def _phase_ffn_ln(self, layer, final):
    S, nc, d = self.S, self.nc, self.d
    self.begin_phase()
    self.alloc_finish()
    self.load_ln_params("ln_ffn_g", "ln_ffn_b", layer, router_layer=None)
    z = [self.sb(f"l_z{i}", [128, D], F32) for i in range(2)]
    f = [self.sb(f"l_f{i}", [128, D], F32) for i in range(2)]
    dst = self.out if final else d["xres"]
    for t in range(NT):
        b = t % 2
        S.dma("sp", z[b][:], d["xres"][t * 128:(t + 1) * 128, :], key=f"l_z{b}", reads=[("res", t)], writes=[f"l_z{b}"])
        S.dma("sp", f[b][:], d["ffn"][t * 128:(t + 1) * 128, :], key=f"l_f{b}", reads=[("ffn", t)], writes=[f"l_f{b}"])
        self.stt("dve", z[b][:], z[b][:], DN_ALPHA, f[b][:], ALU.mult, ALU.add, [f"l_z{b}", f"l_f{b}"], [f"l_z{b}"])
        self.finish_tile(z[b], f"l_z{b}", t, dst, False, [t % 2, 2, 3, 4])
    self.end_phase(final=final)


Prog.phase_moe = _phase_moe
Prog.phase_ffn_ln = _phase_ffn_ln


SC_C = float(1.0 / np.sqrt(96.0))
SC_F = 0.125


def _phase_odd(self, layer):
    self.phase_odd_a(layer)
    self.phase_odd_b(layer)
    self.phase_odd_c(layer)


def _phase_odd_a(self, layer):
    S, nc, d = self.S, self.nc, self.d
    i = self.PI(layer)
    self.begin_phase()
    sb = self.sb
    PB = self.pb
    if not hasattr(self, "kb_all"):
        self.kb_all = S.sbuf("kb_all", [128, NT, 16], F32)
        self.ef_all = S.sbuf("ef_all", [128, NT, 16], F32)
        self.dscratch("QTd", [16, 128, S_TOK], BF16)
        self.dscratch("KTd", [16, 128, S_TOK], BF16)
        self.dscratch("VPd", [S_TOK, 16, 64], BF16)
        self.dscratch("mixTd", [128, KC, S_TOK], BF16)
    kb_all, ef_all = self.kb_all, self.ef_all
    w_in = sb("a_win", [128, KC, 1960], BF16)
    for k in range(KC):
        S.dma("pool", w_in[:, k, :], d["od_w_in"][i, k * 128:(k + 1) * 128, :], key="a_win", writes=["a_win"])
    w_uq = sb("a_wuq", [128, 2, 768], BF16)
    S.dma("pool", w_uq[:], d["od_w_uq"][i].rearrange("(c p) n -> p c n", p=128), key="a_wuq", writes=["a_wuq"])
    w_ukv = sb("a_wukv", [128, 1024], BF16)
    S.dma("pool", w_ukv[:], d["od_w_ukv"][i], key="a_wukv", writes=["a_wukv"])
    gq = sb("a_gq", [128, 256], F32)
    gkv = sb("a_gkv", [128, 128], F32)
    fb = sb("a_fb", [128, 8], F32)
    S.dma("sp", gq[:], d["od_q_norm_g"][i:i + 1, :].to_broadcast([128, 256]), key="a_gq", writes=["a_gq"])
    S.dma("sp", gkv[:], d["od_kv_norm_g"][i:i + 1, :].to_broadcast([128, 128]), key="a_gkv", writes=["a_gkv"])
    S.dma("sp", fb[:], d["od_f_bias"][i:i + 1, :].to_broadcast([128, 8]), key="a_fb", writes=["a_fb"])
    cos = sb("a_cos", [128, NT, 16], F32)
    sin = sb("a_sin", [128, NT, 16], F32)
    S.dma("sp", cos[:], d["c_cos"].rearrange("(t p) f -> p t f", p=128), key="a_cos", writes=["a_cos"])
    S.dma("sp", sin[:], d["c_sin"].rearrange("(t p) f -> p t f", p=128), key="a_sin", writes=["a_sin"])
    tri = sb("a_tri", [128, 128], F32)
    ones = sb("a_ones", [128, 128], F32)
    S.dma("sp", tri[:], d["c_tri"], key="a_tri", writes=["a_tri"])
    S.op("pool", lambda: nc.gpsimd.memset(ones[:], 1.0), [], ["a_ones"])
    carry = sb("a_carry", [128, 8], F32)
    S.op("pool", lambda: nc.gpsimd.memset(carry[:], 0.0), [], ["a_carry"])
    xblk = [sb(f"a_xblk{b}", [128, KC, 512], BF16) for b in range(2)]
    sq = sb("a_sq", [128, 1024], F32)
    sm = sb("a_sm", [128, 64], F32)
    cqn = sb("a_cqn", [128, 256], F32)
    cqb = sb("a_cqb", [128, 384], BF16)
    cT = sb("a_cT", [128, 3, 128], BF16)
    qf = sb("a_qf", [128, 768], F32)
    kvf = sb("a_kvf", [128, 1024], F32)
    krf = sb("a_krf", [128, 32], F32)
    kro = sb("a_kro", [128, 32], F32)
    rt = sb("a_rt", [128, 8, 32], F32)
    krt = sb("a_krt", [128, 16], F32)
    kro2 = sb("a_kro2", [128, 32], F32)
    qaug = sb("a_qaug", [128, 8, 98], BF16)
    kaug = sb("a_kaug", [128, 8, 98], BF16)
    vp = sb("a_vp", [128, 16, 64], BF16)
    fqf = sb("a_fqf", [128, 512], F32)
    fkf = sb("a_fkf", [128, 512], F32)
    qaugf = sb("a_qaugf", [128, 8, 66], BF16)
    kaugf = sb("a_kaugf", [128, 8, 66], BF16)
    Ft = sb("a_Ft", [128, 8], F32)
    hib = sb("a_hib", [128, 16], BF16)
    hif = sb("a_hif", [128, 16], F32)
    av = sb("a_av", [128, 16], F32)
    stq = sb("a_stq", [128, 8, 128], BF16)
    stk = sb("a_stk", [128, 8, 128], BF16)
    stqf = sb("a_stqf", [128, 8, 128], BF16)
    stkf = sb("a_stkf", [128, 8, 128], BF16)
    S.op("pool", lambda: nc.gpsimd.memset(kaug[:, :, 96:98], 1.0), [], ["a_kaug_one"])
    S.op("pool", lambda: nc.gpsimd.memset(kaugf[:, :, 64:66], 1.0), [], ["a_kaugf_one"])

    def rstd(dst, src, n, name):
        self.ts("dve", dst, src, 1.0 / n, EPS, ALU.mult, ALU.add, [name + "_s"], [name])
        self.act(dst, dst, AF.Sqrt, [name], [name])
        S.op("dve", lambda: nc.vector.reciprocal(dst, dst), [name], [name])

    def hilo(a, n, dst_hi, dst_lo, aname, outname):
        S.op("dve", lambda: nc.vector.tensor_copy(hib[:, 0:n], a), [aname], ["a_hib"])
        S.op("dve", lambda: nc.vector.tensor_copy(hif[:, 0:n], hib[:, 0:n]), ["a_hib"], ["a_hif"])
        S.op("dve", lambda: nc.vector.tensor_copy(dst_hi, hib[:, 0:n]), ["a_hib"], [outname + "_hi"])
        self.tt("dve", dst_lo, a, hif[:, 0:n], ALU.subtract, [aname, "a_hif"], [outname + "_lo"])

    def transposes(src, nh, da, bank, stage, stname, srcnames):
        pv = self.bfv(bank)

        def f():
            ins = None
            for h in range(nh):
                ins = nc.tensor.transpose(pv[0:da, h * 128:(h + 1) * 128], src[:, h, :], self.ident_b[:])
            return ins
        S.op("pe", f, srcnames + ["ident_b"], [f"pb{bank}"])
        S.op("act", lambda: nc.scalar.copy(stage[0:da, :, :], pv[0:da, :].rearrange("p (h t) -> p h t", h=nh)), [f"pb{bank}"], [stname])

    for t in range(NT):
        tb, tt = t // 4, t % 4
        xb = xblk[tb % 2]
        xbn = f"a_xblk{tb % 2}"
        if tt == 0:
            S.dma("sp", xb[:], d["xTd"][:, :, tb * 512:(tb + 1) * 512], key=xbn, reads=[("xTd", tb)], writes=[xbn])
        tok = slice(tt * 128, (tt + 1) * 128)

        def fA(xb=xb, tok=tok):
            ins = None
            for k in range(KC):
                nc.tensor.matmul(PB[0][:, 0:416], xb[:, k, tok], w_in[:, k, 0:416], start=(k == 0), stop=(k == KC - 1))
            for k in range(KC):
                ins = nc.tensor.matmul(PB[0][:, 416:424], xb[:, k, tok], w_in[:, k, 1952:1960], start=(k == 0), stop=(k == KC - 1))
            return ins
        S.op("pe", fA, ["a_win", xbn], ["pb0"])
        for bi, c0 in ((1, 416), (2, 928), (3, 1440)):
            S.op("pe", self.mm_group(PB[bi][:], [(xb[:, k, tok], w_in[:, k, c0:c0 + 512]) for k in range(KC)]),
                 ["a_win", xbn], [f"pb{bi}"])
        self.act(sq[:, 0:384], PB[0][:, 0:384], AF.Square, ["pb0"], ["a_sq"])
        self.rsum("dve", sm[:, 0:1], sq[:, 0:256], ["a_sq"], ["a_sm0_s"])
        self.rsum("dve", sm[:, 1:2], sq[:, 256:384], ["a_sq"], ["a_sm1_s"])
        rstd(sm[:, 0:1], sm[:, 0:1], 256, "a_sm0")
        rstd(sm[:, 1:2], sm[:, 1:2], 128, "a_sm1")
        self.stt("dve", cqn[:, 0:256], PB[0][:, 0:256], sm[:, 0:1], gq[:], ALU.mult, ALU.mult, ["pb0", "a_sm0", "a_gq"], ["a_cqn"])
        S.op("act", lambda: nc.scalar.copy(cqb[:, 0:256], cqn[:, 0:256]), ["a_cqn"], ["a_cqb0"])
        self.stt("dve", cqb[:, 256:384], PB[0][:, 256:384], sm[:, 1:2], gkv[:], ALU.mult, ALU.mult, ["pb0", "a_sm1", "a_gkv"], ["a_cqb1"])
        S.op("act", lambda: nc.scalar.copy(krf[:], PB[0][:, 384:416]), ["pb0"], ["a_krf"])
        self.tt("dve", sm[:, 8:16], PB[0][:, 416:424], fb[:], ALU.add, ["pb0", "a_fb"], ["a_zb"])
        self.act(sm[:, 8:16], sm[:, 8:16], AF.Exp, ["a_zb"], ["a_zb"], scale=-1.0)
        self.act(sm[:, 8:16], sm[:, 8:16], AF.Ln, ["a_zb"], ["a_zb"], bias=1.0, scale=1.0)
        pv4 = self.bfv(4)

        def fT():
            ins = None
            for c in range(3):
                ins = nc.tensor.transpose(pv4[:, c * 128:(c + 1) * 128], cqb[:, c * 128:(c + 1) * 128], self.ident_b[:])
            return ins
        S.op("pe", fT, ["a_cqb0", "a_cqb1", "ident_b"], ["pb4"])
        S.op("act", lambda: nc.scalar.copy(cT[:], pv4[:, 0:384].rearrange("p (c t) -> p c t", c=3)), ["pb4"], ["a_cT"])
        S.op("pe", self.mm_group(PB[5][:], [(cT[:, c, :], w_uq[:, c, 0:512]) for c in range(2)]), ["a_cT", "a_wuq"], ["pb5"])
        S.op("pe", self.mm_group(PB[6][:, 0:256], [(cT[:, c, :], w_uq[:, c, 512:768]) for c in range(2)]), ["a_cT", "a_wuq"], ["pb6"])
        S.op("act", lambda: nc.scalar.copy(qf[:, 0:512], PB[5][:]), ["pb5"], ["a_qf0"])
        S.op("act", lambda: nc.scalar.copy(qf[:, 512:768], PB[6][:, 0:256]), ["pb6"], ["a_qf1"])
        S.op("pe", self.mm_group(PB[7][:], [(cT[:, 2, :], w_ukv[:, 0:512])]), ["a_cT", "a_wukv"], ["pb7"])
        S.op("pe", self.mm_group(PB[4][:], [(cT[:, 2, :], w_ukv[:, 512:1024])]), ["a_cT", "a_wukv"], ["pb4"])
        S.op("act", lambda: nc.scalar.copy(kvf[:, 0:512], PB[7][:]), ["pb7"], ["a_kvf0"])
        S.op("act", lambda: nc.scalar.copy(kvf[:, 512:1024], PB[4][:]), ["pb4"], ["a_kvf1"])
        q3 = qf[:].rearrange("p (h c) -> p h c", h=8)
        kv3 = kvf[:].rearrange("p (h c) -> p h c", h=8)
        QN = ["a_qf0", "a_qf1"]
        KN = ["a_kvf0", "a_kvf1"]
        sq3 = sq[:, 0:768].rearrange("p (h c) -> p h c", h=8)
        self.act(sq[:, 0:768], qf[:], AF.Square, QN, ["a_sq"])
        self.rsum("dve", av[:, 0:8], sq3, ["a_sq"], ["a_av_q"])
        self.ts("dve", av[:, 0:8], av[:, 0:8], -0.5 * SC_C, None, ALU.mult, None, ["a_av_q"], ["a_av_q"])
        hilo(av[:, 0:8], 8, qaug[:, :, 96], qaug[:, :, 97], "a_av_q", "a_qaug")
        self.ts("dve", qaug[:, :, 0:64], q3[:, :, 0:64], SC_C, None, ALU.mult, None, QN, ["a_qaug_n"])
        cosb = cos[:, t, :].unsqueeze(1).to_broadcast([128, 8, 16])
        sinb = sin[:, t, :].unsqueeze(1).to_broadcast([128, 8, 16])
        x1, x2 = q3[:, :, 64:80], q3[:, :, 80:96]
        self.tt("dve", rt[:, :, 0:16], x1, cosb, ALU.mult, QN + ["a_cos"], ["a_rt0"])
        self.tt("dve", rt[:, :, 16:32], x2, sinb, ALU.mult, QN + ["a_sin"], ["a_rt1"])
        self.tt("dve", rt[:, :, 0:16], rt[:, :, 0:16], rt[:, :, 16:32], ALU.subtract, ["a_rt0", "a_rt1"], ["a_rt0"])
        self.ts("dve", qaug[:, :, 64:80], rt[:, :, 0:16], SC_C, None, ALU.mult, None, ["a_rt0"], ["a_qaug_r1"])
        self.tt("dve", rt[:, :, 0:16], x2, cosb, ALU.mult, QN + ["a_cos"], ["a_rt0"])
        self.tt("dve", rt[:, :, 16:32], x1, sinb, ALU.mult, QN + ["a_sin"], ["a_rt1"])
        self.tt("dve", rt[:, :, 0:16], rt[:, :, 0:16], rt[:, :, 16:32], ALU.add, ["a_rt0", "a_rt1"], ["a_rt0"])
        self.ts("dve", qaug[:, :, 80:96], rt[:, :, 0:16], SC_C, None, ALU.mult, None, ["a_rt0"], ["a_qaug_r2"])
        c1, s1 = cos[:, t, :], sin[:, t, :]
        self.tt("pool", kro[:, 0:16], krf[:, 0:16], c1, ALU.mult, ["a_krf", "a_cos"], ["a_kro0"])
        self.tt("pool", krt[:], krf[:, 16:32], s1, ALU.mult, ["a_krf", "a_sin"], ["a_krt"])
        self.tt("pool", kro[:, 0:16], kro[:, 0:16], krt[:], ALU.subtract, ["a_kro0", "a_krt"], ["a_kro0"])
        self.tt("pool", kro[:, 16:32], krf[:, 16:32], c1, ALU.mult, ["a_krf", "a_cos"], ["a_kro1"])
        self.tt("pool", krt[:], krf[:, 0:16], s1, ALU.mult, ["a_krf", "a_sin", "a_krt"], ["a_krt"])
        self.tt("pool", kro[:, 16:32], kro[:, 16:32], krt[:], ALU.add, ["a_kro1", "a_krt"], ["a_kro1"])
        S.op("act", lambda: nc.scalar.copy(kaug[:, :, 0:64], kv3[:, :, 0:64]), KN, ["a_kaug_n"])
        S.op("dve", lambda: nc.vector.tensor_copy(kaug[:, :, 64:96], kro[:].unsqueeze(1).to_broadcast([128, 8, 32])), ["a_kro0", "a_kro1"], ["a_kaug_r"])
        sqk = sq[:].rearrange("p (h c) -> p h c", h=8)
        self.act(sq[:], kvf[:], AF.Square, KN + ["a_sq"], ["a_sq"])
        self.rsum("dve", av[:, 8:16], sqk[:, :, 0:64], ["a_sq"], ["a_av_k"])
        self.tt("pool", kro2[:], kro[:], kro[:], ALU.mult, ["a_kro0", "a_kro1"], ["a_kro2"])
        self.rsum("dve", sm[:, 2:3], kro2[:], ["a_kro2"], ["a_sm2"])
        self.ts("dve", av[:, 8:16], av[:, 8:16], sm[:, 2:3], 0.5 * SC_C, ALU.add, ALU.mult, ["a_av_k", "a_sm2"], ["a_av_k"])
        self.ts("dve", kb_all[:, t, 0:8], av[:, 8:16], -1.0, None, ALU.mult, None, ["a_av_k"], [("kb", t, 0)])
        self.act(ef_all[:, t, 0:8], av[:, 8:16], AF.Exp, ["a_av_k"], [("ef", t, 0)])
        self.tt("dve", vp[:, 0:8, :], kv3[:, :, 64:128], ef_all[:, t, 0:8].unsqueeze(2).to_broadcast([128, 8, 64]), ALU.mult,
                KN + [("ef", t, 0)], ["a_vp0"])
        S.op("pe", self.mm_group(PB[5][:, 0:8], [(tri[:], sm[:, 8:16])]), ["a_tri", "a_zb"], ["pb5"])
        S.op("pe", self.mm_group(PB[6][:, 0:8], [(ones[:], sm[:, 8:16])]), ["a_ones", "a_zb"], ["pb6"])
        self.tt("dve", Ft[:], PB[5][:, 0:8], carry[:], ALU.add, ["pb5", "a_carry"], ["a_Ft"])
        self.tt("dve", carry[:], carry[:], PB[6][:, 0:8], ALU.add, ["pb6", "a_carry"], ["a_carry"])
        S.op("act", lambda: nc.scalar.copy(fqf[:], PB[1][:]), ["pb1"], ["a_fqf"])
        S.op("act", lambda: nc.scalar.copy(fkf[:], PB[2][:]), ["pb2"], ["a_fkf"])
        fq3 = fqf[:].rearrange("p (h c) -> p h c", h=8)
        fk3 = fkf[:].rearrange("p (h c) -> p h c", h=8)
        sqf = sq[:, 0:512].rearrange("p (h c) -> p h c", h=8)
        self.act(sq[:, 0:512], fqf[:], AF.Square, ["a_fqf", "a_sq"], ["a_sq"])
        self.rsum("dve", av[:, 0:8], sqf, ["a_sq"], ["a_av_fq"])
        self.stt("dve", av[:, 0:8], av[:, 0:8], 0.5 * SC_F, Ft[:], ALU.mult, ALU.add, ["a_av_fq", "a_Ft"], ["a_av_fq"])
        self.ts("dve", av[:, 0:8], av[:, 0:8], -1.0, None, ALU.mult, None, ["a_av_fq"], ["a_av_fq"])
        hilo(av[:, 0:8], 8, qaugf[:, :, 64], qaugf[:, :, 65], "a_av_fq", "a_qaugf")
        self.ts("dve", qaugf[:, :, 0:64], fq3, SC_F, None, ALU.mult, None, ["a_fqf"], ["a_qaugf_n"])
        S.op("act", lambda: nc.scalar.copy(kaugf[:, :, 0:64], fk3), ["a_fkf"], ["a_kaugf_n"])
        self.act(sq[:, 0:512], fkf[:], AF.Square, ["a_fkf", "a_sq"], ["a_sq"])
        self.rsum("dve", av[:, 8:16], sqf, ["a_sq"], ["a_av_fk"])
        self.ts("dve", av[:, 8:16], av[:, 8:16], 0.5 * SC_F, None, ALU.mult, None, ["a_av_fk"], ["a_av_fk"])
        self.tt("dve", kb_all[:, t, 8:16], Ft[:], av[:, 8:16], ALU.subtract, ["a_Ft", "a_av_fk"], [("kb", t, 1)])
        self.act(ef_all[:, t, 8:16], av[:, 8:16], AF.Exp, ["a_av_fk"], [("ef", t, 1)])
        self.tt("dve", vp[:, 8:16, :], PB[3][:].rearrange("p (h c) -> p h c", h=8),
                ef_all[:, t, 8:16].unsqueeze(2).to_broadcast([128, 8, 64]), ALU.mult, ["pb3", ("ef", t, 1)], ["a_vp1"])
        S.dma("sp", d["VPd"][t * 128:(t + 1) * 128, :, :], vp[:], key="a_vpd", reads=["a_vp0", "a_vp1"], writes=[("VPd", t)])
        transposes(qaug, 8, 98, 5, stq, "a_stq", ["a_qaug_hi", "a_qaug_lo", "a_qaug_n", "a_qaug_r1", "a_qaug_r2"])
        S.dma("sp", d["QTd"][0:8, 0:98, t * 128:(t + 1) * 128].rearrange("h r t -> r h t"), stq[0:98, :, :], key="a_qtd", reads=["a_stq"], writes=[("QTd", t, 0)])
        transposes(kaug, 8, 98, 6, stk, "a_stk", ["a_kaug_n", "a_kaug_r", "a_kaug_one"])
        S.dma("sp", d["KTd"][0:8, 0:98, t * 128:(t + 1) * 128].rearrange("h r t -> r h t"), stk[0:98, :, :], key="a_ktd", reads=["a_stk"], writes=[("KTd", t, 0)])
        transposes(qaugf, 8, 66, 7, stqf, "a_stqf", ["a_qaugf_hi", "a_qaugf_lo", "a_qaugf_n"])
        S.dma("sp", d["QTd"][8:16, 0:66, t * 128:(t + 1) * 128].rearrange("h r t -> r h t"), stqf[0:66, :, :], key="a_qtdf", reads=["a_stqf"], writes=[("QTd", t, 1)])
        transposes(kaugf, 8, 66, 4, stkf, "a_stkf", ["a_kaugf_n", "a_kaugf_one"])
        S.dma("sp", d["KTd"][8:16, 0:66, t * 128:(t + 1) * 128].rearrange("h r t -> r h t"), stkf[0:66, :, :], key="a_ktdf", reads=["a_stkf"], writes=[("KTd", t, 1)])
    self.end_phase()


def _phase_odd_b(self, layer):
    S, nc, d = self.S, self.nc, self.d
    self.begin_phase()
    sb = self.sb
    PB = self.pb
    kb_all, ef_all = self.kb_all, self.ef_all
    QT = [sb(f"b_QT{i}", [128, S_TOK], BF16) for i in range(2)]
    KT = [sb(f"b_KT{i}", [128, S_TOK], BF16) for i in range(2)]
    VP = [sb(f"b_VP{i}", [128, NT, 64], BF16) for i in range(2)]
    W = [sb(f"b_W{i}", [128, NT, 64], BF16) for i in range(2)]
    pT = [sb(f"b_pT{i}", [128, 512], BF16) for i in range(3)]
    rl = [sb(f"b_rl{i}", [64, 512], F32) for i in range(2)]
    yo = [sb(f"b_yo{i}", [64, 512], BF16) for i in range(2)]
    mk = sb("b_mk", [128, 2, 128], F32)
    mkb = sb("b_mkb", [128, 2, 128], BF16)
    S.dma("sp", mk[:, 0, :], d["c_maskc"], key="b_mk0", writes=["b_mk0"])
    S.dma("sp", mk[:, 1, :], d["c_maskf"], key="b_mk1", writes=["b_mk1"])
    S.op("dve", lambda: nc.vector.tensor_copy(mkb[:], mk[:]), ["b_mk0", "b_mk1"], ["b_mkb"])
    units = []
    for hh in range(16):
        for j in range(S_TOK // 512):
            nkt = 4 * j + 4
            for kt in range(nkt):
                units.append((hh, j, kt, nkt))

    def head_loads(hh):
        b = hh % 2
        fox = 1 if hh >= 8 else 0
        da = 66 if fox else 98
        S.dma("sp", QT[b][0:da, :], d["QTd"][hh, 0:da, :], key=f"b_QT{b}", reads=[("QTd", t, fox) for t in range(NT)], writes=[f"b_QT{b}"])
        S.dma("sp", KT[b][0:da, :], d["KTd"][hh, 0:da, :], key=f"b_KT{b}", reads=[("KTd", t, fox) for t in range(NT)], writes=[f"b_KT{b}"])
        S.dma("sp", VP[b][:], d["VPd"][:, hh, :].rearrange("(kt p) c -> p kt c", p=128), key=f"b_VP{b}",
              reads=[("VPd", t) for t in range(NT)], writes=[f"b_VP{b}"])
        S.op("pool", lambda b=b, hh=hh: nc.gpsimd.tensor_copy(W[b][:], ef_all[:, :, hh].unsqueeze(2).to_broadcast([128, NT, 64])),
             [("ef", t, fox) for t in range(NT)], [f"b_W{b}"])

    def emit_qk(u, hh, j, kt, nkt):
        b = hh % 2
        fox = 1 if hh >= 8 else 0
        da = 66 if fox else 98
        qoff = 0 if kt < 4 * j else 128 * (kt - 4 * j)
        ncol = 512 - qoff
        sbk = u % 3
        S.op("pe", self.mm_group(PB[sbk][:, 0:ncol], [(KT[b][0:da, kt * 128:(kt + 1) * 128], QT[b][0:da, j * 512 + qoff:(j + 1) * 512])]),
             [f"b_KT{b}", f"b_QT{b}"], [f"pb{sbk}"])
        p = pT[sbk]
        self.act(p[:, 0:ncol], PB[sbk][:, 0:ncol], AF.Exp, [f"pb{sbk}", ("kb", kt, fox)], [f"b_pT{sbk}"],
                 bias=kb_all[:, kt, hh:hh + 1], scale=1.0)
        if kt >= 4 * j:
            self.tt("dve", p[:, 0:128], p[:, 0:128], mkb[:, fox, :], ALU.mult, [f"b_pT{sbk}", "b_mkb"], [f"b_pT{sbk}"])

    def emit_pv(u, hh, j, kt, nkt):
        b = hh % 2
        qoff = 0 if kt < 4 * j else 128 * (kt - 4 * j)
        ncol = 512 - qoff
        sbk = u % 3
        p = pT[sbk]
        ob = 4 + 2 * (j % 2)
        st, sp_ = (kt == 0), (kt == nkt - 1)

        def fpv(b=b, kt=kt, qoff=qoff, p=p, ncol=ncol, st=st, sp_=sp_, ob=ob):
            nc.tensor.matmul(PB[ob][0:64, qoff:512], VP[b][:, kt, :], p[:, 0:ncol], start=st, stop=sp_)
            return nc.tensor.matmul(PB[ob + 1][0:64, qoff:512], W[b][:, kt, :], p[:, 0:ncol], start=st, stop=sp_)
        S.op("pe", fpv, [f"b_pT{sbk}", f"b_VP{b}", f"b_W{b}"], [f"pb{ob}", f"pb{ob + 1}"])
        if sp_:
            r = j % 2
            S.op("dve", lambda r=r, ob=ob: nc.vector.reciprocal(rl[r][:], PB[ob + 1][0:64, :]), [f"pb{ob + 1}"], [f"b_rl{r}"])
            self.tt("dve", yo[r][:], PB[ob][0:64, :], rl[r][:], ALU.mult, [f"pb{ob}", f"b_rl{r}"], [f"b_yo{r}"])
            ck, hf = hh // 2, hh % 2
            S.dma("sp", d["mixTd"][hf * 64:(hf + 1) * 64, ck, j * 512:(j + 1) * 512], yo[r][:], key=f"b_yo{r}",
                  reads=[f"b_yo{r}"], writes=[("mixTd", j, hh)])

    NU = len(units)
    for u in range(NU + 1):
        if u < NU:
            hh, j, kt, nkt = units[u]
            if j == 0 and kt == 0:
                head_loads(hh)
            emit_qk(u, hh, j, kt, nkt)
        if u >= 1:
            emit_pv(u - 1, *units[u - 1])
    self.end_phase()


def _phase_odd_c(self, layer):
    S, nc, d = self.S, self.nc, self.d
    i = self.PI(layer)
    self.begin_phase()
    sb = self.sb
    PB = self.pb
    self.alloc_finish()
    self.load_ln_params("ln_mix_g", "ln_mix_b", layer, router_layer=layer)
    w_out = sb("c_wout", [128, KC, D], BF16)
    for k in range(KC):
        S.dma("pool", w_out[:, k, :], d["od_w_out"][i, k * 128:(k + 1) * 128, :], key="c_wout", writes=["c_wout"])
    mix = [sb(f"c_mix{b}", [128, KC, 512], BF16) for b in range(2)]
    z = sb("c_z", [128, D], F32)
    xsrc = d["x"] if self.single is not None else d["xres"]
    for tb in range(NT // 4):
        mb = mix[tb % 2]
        mn = f"c_mix{tb % 2}"
        S.dma("sp", mb[:], d["mixTd"][:, :, tb * 512:(tb + 1) * 512], key=mn, reads=[("mixTd", tb, hh) for hh in range(16)], writes=[mn])
        for tt in range(4):
            t = tb * 4 + tt
            tok = slice(tt * 128, (tt + 1) * 128)
            S.dma("sp", z[:], xsrc[t * 128:(t + 1) * 128, :], key="c_z", reads=[("res", t)], writes=["c_z"])
            for nb in range(2):
                bank = 5 + nb
                S.op("pe", self.mm_group(PB[bank][:], [(mb[:, k, tok], w_out[:, k, nb * 512:(nb + 1) * 512]) for k in range(KC)]),
                     reads=[mn, "c_wout"], writes=[f"pb{bank}"])
                self.stt("dve", z[:, nb * 512:(nb + 1) * 512], z[:, nb * 512:(nb + 1) * 512], DN_ALPHA, PB[bank][:],
                         ALU.mult, ALU.add, ["c_z", f"pb{bank}"], ["c_z"])
            self.finish_tile(z, "c_z", t, d["xres"], True, [7, 0, 1, 2])
    self.end_phase()


Prog.phase_odd = _phase_odd
Prog.phase_odd_a = _phase_odd_a
Prog.phase_odd_b = _phase_odd_b
Prog.phase_odd_c = _phase_odd_c
```
